# Optimizing a Trainium2 kernel written in Bass

```python
import math
import jax, jax.numpy as jnp
from jax import lax
import numpy as np

D_MODEL = 1024
BATCH = 2
SEQ = 16384
DEPTH = 4

SSM_WIDTH = D_MODEL // 4
SSM_GROUP_CH = 16
SSM_GROUPS = SSM_WIDTH // SSM_GROUP_CH
SSM_STATE = 64
RET_HEAD_DIM = 64
RET_WIDTH = D_MODEL // 2
RET_HEADS = RET_WIDTH // RET_HEAD_DIM
SB_HEAD_DIM = 64
SB_WIDTH = D_MODEL // 4
SB_HEADS = SB_WIDTH // SB_HEAD_DIM
MIX_WIDTH = SSM_WIDTH + RET_WIDTH + SB_WIDTH
IN_COLS = SSM_WIDTH + 4 * RET_WIDTH + 3 * SB_WIDTH
RET_CHUNK = 128
SB_BLOCK = 128
ROPE_BASE = 10000.0
D_FF = 64 * ((8 * D_MODEL // 3 + 63) // 64)
D_FF_EXPERT = D_FF // 4
N_EXPERTS = 8
TOP_K = 2
N_DENSE = (DEPTH + 1) // 2
N_MOE = DEPTH // 2
ALPHA = (2.0 * DEPTH) ** 0.25
BETA_INIT = (8.0 * DEPTH) ** -0.25
LN_EPS = 1e-5

kernel_name = "hybrid_s5_retention_stickbreaking_moe"

F32 = jnp.float32
HI = lax.Precision.HIGHEST


def layer_norm(x, w, b):
    xf = x.astype(F32)
    mu = jnp.mean(xf, -1, keepdims=True)
    var = jnp.mean(jnp.square(xf - mu), -1, keepdims=True)
    y = (xf - mu) * lax.rsqrt(var + LN_EPS) * w.astype(F32) + b.astype(F32)
    return y.astype(x.dtype)


def head_rms_norm(x, w):
    xf = x.astype(F32)
    y = xf * lax.rsqrt(jnp.mean(xf * xf, -1, keepdims=True) + LN_EPS)
    return y * w.astype(F32)


def s5_mixer(u, lam_re, lam_im, log_dt, b_re, b_im, c_re, c_im, d_skip, glu_w, glu_b, norm_w):
    bsz, seq, _ = u.shape
    G, H, P = SSM_GROUPS, SSM_GROUP_CH, SSM_STATE
    ug = u.astype(F32).reshape(bsz, seq, G, H)
    lam = lax.complex(lam_re.astype(F32), lam_im.astype(F32))
    dt = jnp.exp(log_dt.astype(F32))[:, None]
    lam_bar = jnp.exp(lam * dt)
    b = lax.complex(b_re.astype(F32), b_im.astype(F32))
    b_bar = ((lam_bar - 1.0) / lam)[..., None] * b
    c = lax.complex(c_re.astype(F32), c_im.astype(F32))
    bu = jnp.einsum('gph,bsgh->bsgp', b_bar, ug.astype(jnp.complex64))
    a = jnp.broadcast_to(lam_bar, bu.shape)

    def combine(e1, e2):
        a1, s1 = e1
        a2, s2 = e2
        return a1 * a2, a2 * s1 + s2

    _, states = lax.associative_scan(combine, (a, bu), axis=1)
    y = jnp.einsum('ghp,bsgp->bsgh', c, states).real + d_skip.astype(F32).reshape(G, H) * ug
    g = jax.nn.gelu(y.reshape(bsz, seq, SSM_WIDTH))
    out = g * jax.nn.sigmoid(g @ glu_w.astype(F32) + glu_b.astype(F32))
    out = head_rms_norm(out.reshape(bsz, seq, G, H), norm_w.reshape(G, H))
    return out.reshape(bsz, seq, SSM_WIDTH).astype(u.dtype)


def rope(x, pos):
    half = x.shape[-1] // 2
    inv = ROPE_BASE ** (-jnp.arange(half, dtype=F32) / half)
    ang = pos[:, None] * inv[None, :]
    cos = jnp.cos(ang)[:, None, :]
    sin = jnp.sin(ang)[:, None, :]
    x1, x2 = x[..., :half], x[..., half:]
    return jnp.concatenate([x1 * cos - x2 * sin, x1 * sin + x2 * cos], axis=-1)


def retention_mixer(q, k, v, g, gn_w, gn_b):
    bsz, seq, _ = q.shape
    H, dh, C = RET_HEADS, RET_HEAD_DIM, RET_CHUNK
    n = seq // C
    pos = jnp.arange(seq, dtype=F32)
    qh = rope(q.astype(F32).reshape(bsz, seq, H, dh), pos)
    kh = rope(k.astype(F32).reshape(bsz, seq, H, dh), pos) * (dh ** -0.5)
    vh = v.astype(F32).reshape(bsz, seq, H, dh)
    log_gamma = jnp.log(1.0 - 2.0 ** (-5.0 - jnp.arange(H, dtype=F32)))

    def chunk(t):
        return t.reshape(bsz, n, C, H, dh).transpose(1, 0, 3, 2, 4)

    qc, kc, vc = chunk(qh), chunk(kh), chunk(vh)
    idx = jnp.arange(C, dtype=F32)
    rel = idx[:, None] - idx[None, :]
    decay_mask = jnp.where(rel >= 0, jnp.exp(log_gamma[:, None, None] * jnp.maximum(rel, 0.0)), 0.0)
    scores = jnp.einsum('nbhqd,nbhkd->nbhqk', qc, kc) * decay_mask
    inner = jnp.einsum('nbhqk,nbhkd->nbhqd', scores, vc)
    q_decay = jnp.exp(log_gamma[:, None] * (idx[None, :] + 1.0))
    k_decay = jnp.exp(log_gamma[:, None] * (C - 1.0 - idx[None, :]))
    chunk_decay = jnp.exp(log_gamma * C)

    def step(state, inp):
        kc_i, vc_i = inp
        new = chunk_decay[None, :, None, None] * state + jnp.einsum(
            'bhkd,bhke->bhde', kc_i * k_decay[None, :, :, None], vc_i)
        return new, state

    state0 = jnp.zeros((bsz, H, dh, dh), F32)
    _, prev_states = lax.scan(step, state0, (kc, vc))
    cross = jnp.einsum('nbhqd,nbhde->nbhqe', qc, prev_states) * q_decay[None, None, :, :, None]
    o = (inner + cross).transpose(1, 0, 3, 2, 4).reshape(bsz, seq, H, dh)
    mu = jnp.mean(o, -1, keepdims=True)
    var = jnp.mean(jnp.square(o - mu), -1, keepdims=True)
    o = (o - mu) * lax.rsqrt(var + LN_EPS) * gn_w.astype(F32).reshape(H, dh) + gn_b.astype(F32).reshape(H, dh)
    out = jax.nn.silu(g.astype(F32)) * o.reshape(bsz, seq, RET_WIDTH)
    return out.astype(q.dtype)


def stick_breaking_mixer(q, k, v, norm_w):
    bsz, seq, _ = q.shape
    H, dh, Bq = SB_HEADS, SB_HEAD_DIM, SB_BLOCK
    nb = seq // Bq
    scale = dh ** -0.5

    def heads(t):
        return t.astype(F32).reshape(bsz, seq, H, dh).transpose(0, 2, 1, 3)

    qh, kh, vh = heads(q), heads(k), heads(v)
    idx = jnp.arange(Bq)
    rev_incl = (idx[:, None] >= idx[None, :]).astype(F32)
    outs = []
    for bi in range(nb):
        L = (bi + 1) * Bq
        nkb = bi + 1
        qb = qh[:, :, bi * Bq:L]
        kb = kh[:, :, :L]
        vb = vh[:, :, :L]
        z = jnp.einsum('bhqd,bhkd->bhqk', qb, kb) * scale
        mask = jnp.arange(L)[None, :] < (bi * Bq + idx)[:, None]
        log_beta = jax.nn.log_sigmoid(z)
        log_1m = jnp.where(mask, log_beta - z, 0.0)
        blk = log_1m.reshape(bsz, H, Bq, nkb, Bq)
        within = jnp.einsum('bhqmj,js->bhqms', blk, rev_incl, precision=HI)
        later_mat = (jnp.arange(nkb)[:, None] > jnp.arange(nkb)[None, :]).astype(F32)
        later = jnp.einsum('bhqn,nm->bhqm', within[..., 0], later_mat, precision=HI)
        after = (within + later[..., None]).reshape(bsz, H, Bq, L) - log_1m
        a = jnp.where(mask, jnp.exp(log_beta + after), 0.0)
        outs.append(jnp.einsum('bhqk,bhkd->bhqd', a, vb))
    o = jnp.concatenate(outs, axis=2)
    o = o.transpose(0, 2, 1, 3)
    o = head_rms_norm(o, norm_w.reshape(H, dh))
    return o.reshape(bsz, seq, SB_WIDTH).astype(q.dtype)


def swiglu(x, w_gate, w_up, w_down):
    return (jax.nn.silu(x @ w_gate) * (x @ w_up)) @ w_down


def moe_swiglu(x, w_router, e_gate, e_up, e_down):
    bsz, seq, d = x.shape
    xt = x.reshape(-1, d)
    logits = (xt @ w_router).astype(F32)
    top_val, top_idx = lax.top_k(logits, TOP_K)
    top_w = jax.nn.softmax(top_val, axis=-1)
    gates = jnp.sum(jax.nn.one_hot(top_idx, N_EXPERTS, dtype=F32) * top_w[..., None], axis=1)
    out = jnp.zeros(xt.shape, F32)
    for e in range(N_EXPERTS):
        out = out + gates[:, e:e + 1] * swiglu(xt, e_gate[e], e_up[e], e_down[e]).astype(F32)
    return out.astype(x.dtype).reshape(bsz, seq, d)


def setup_inputs(seed: int = 0) -> dict:
    key = jax.random.key(seed)
    ks = iter(jax.random.split(key, 40))
    nrm = lambda shape, s: jax.random.normal(next(ks), shape, F32) * s
    G, H, P = SSM_GROUPS, SSM_GROUP_CH, SSM_STATE
    d_in = D_MODEL ** -0.5
    lam_im = jnp.broadcast_to(jnp.pi * jnp.arange(P, dtype=F32), (DEPTH, G, P)) + nrm((DEPTH, G, P), 0.01)
    return {
        "x": nrm((BATCH, SEQ, D_MODEL), 1.0),
        "w_in": nrm((DEPTH, D_MODEL, IN_COLS), d_in),
        "ssm_lam_re": -0.5 + nrm((DEPTH, G, P), 0.01),
        "ssm_lam_im": lam_im,
        "ssm_log_dt": jax.random.uniform(next(ks), (DEPTH, G), F32, math.log(0.001), math.log(0.1)),
        "ssm_b_re": nrm((DEPTH, G, P, H), (2.0 * H) ** -0.5),
        "ssm_b_im": nrm((DEPTH, G, P, H), (2.0 * H) ** -0.5),
        "ssm_c_re": nrm((DEPTH, G, H, P), (2.0 * P) ** -0.5),
        "ssm_c_im": nrm((DEPTH, G, H, P), (2.0 * P) ** -0.5),
        "ssm_d": nrm((DEPTH, SSM_WIDTH), 1.0),
        "ssm_glu_w": nrm((DEPTH, SSM_WIDTH, SSM_WIDTH), SSM_WIDTH ** -0.5),
        "ssm_glu_b": nrm((DEPTH, SSM_WIDTH), 0.01),
        "ssm_norm_w": 1.0 + nrm((DEPTH, SSM_WIDTH), 0.02),
        "ret_gn_w": 1.0 + nrm((DEPTH, RET_WIDTH), 0.02),
        "ret_gn_b": nrm((DEPTH, RET_WIDTH), 0.01),
        "sb_norm_w": 1.0 + nrm((DEPTH, SB_WIDTH), 0.02),
        "w_out": nrm((DEPTH, MIX_WIDTH, D_MODEL), MIX_WIDTH ** -0.5 * BETA_INIT),
        "ln_mix_w": 1.0 + nrm((DEPTH, D_MODEL), 0.02),
        "ln_mix_b": nrm((DEPTH, D_MODEL), 0.01),
        "ffn_w_gate": nrm((N_DENSE, D_MODEL, D_FF), d_in),
        "ffn_w_up": nrm((N_DENSE, D_MODEL, D_FF), d_in),
        "ffn_w_down": nrm((N_DENSE, D_FF, D_MODEL), D_FF ** -0.5 * BETA_INIT),
        "moe_router": nrm((N_MOE, D_MODEL, N_EXPERTS), d_in),
        "moe_w_gate": nrm((N_MOE, N_EXPERTS, D_MODEL, D_FF_EXPERT), d_in),
        "moe_w_up": nrm((N_MOE, N_EXPERTS, D_MODEL, D_FF_EXPERT), d_in),
        "moe_w_down": nrm((N_MOE, N_EXPERTS, D_FF_EXPERT, D_MODEL), D_FF_EXPERT ** -0.5 * BETA_INIT),
        "ln_ffn_w": 1.0 + nrm((DEPTH, D_MODEL), 0.02),
        "ln_ffn_b": nrm((DEPTH, D_MODEL), 0.01),
    }


def reference(x, w_in, ssm_lam_re, ssm_lam_im, ssm_log_dt, ssm_b_re, ssm_b_im, ssm_c_re, ssm_c_im,
              ssm_d, ssm_glu_w, ssm_glu_b, ssm_norm_w, ret_gn_w, ret_gn_b, sb_norm_w, w_out,
              ln_mix_w, ln_mix_b, ffn_w_gate, ffn_w_up, ffn_w_down, moe_router, moe_w_gate,
              moe_w_up, moe_w_down, ln_ffn_w, ln_ffn_b):
    sizes = (SSM_WIDTH,) + (RET_WIDTH,) * 4 + (SB_WIDTH,) * 3
    split_points = np.cumsum(sizes)[:-1].tolist()
    for layer in range(DEPTH):
        proj = x @ w_in[layer]
        u, rq, rk, rv, rg, sq, sk, sv = jnp.split(proj, split_points, axis=-1)
        y_ssm = s5_mixer(u, ssm_lam_re[layer], ssm_lam_im[layer], ssm_log_dt[layer],
                         ssm_b_re[layer], ssm_b_im[layer], ssm_c_re[layer], ssm_c_im[layer],
                         ssm_d[layer], ssm_glu_w[layer], ssm_glu_b[layer], ssm_norm_w[layer])
        y_ret = retention_mixer(rq, rk, rv, rg, ret_gn_w[layer], ret_gn_b[layer])
        y_sb = stick_breaking_mixer(sq, sk, sv, sb_norm_w[layer])
        mix = jnp.concatenate([y_ssm, y_ret, y_sb], axis=-1) @ w_out[layer]
        x = layer_norm(ALPHA * x + mix, ln_mix_w[layer], ln_mix_b[layer])
        li = layer // 2
        if layer % 2 == 0:
            f = swiglu(x, ffn_w_gate[li], ffn_w_up[li], ffn_w_down[li])
        else:
            f = moe_swiglu(x, moe_router[li], moe_w_gate[li], moe_w_up[li], moe_w_down[li])
        x = layer_norm(ALPHA * x + f, ln_ffn_w[layer], ln_ffn_b[layer])
    return x
```

```python
import numpy as np
import ml_dtypes
import concourse.bass as bass
import concourse.mybir as mybir
from concourse.bass_utils import run_bass_kernel_spmd
from contextlib import ExitStack

F32 = mybir.dt.float32
BF16 = mybir.dt.bfloat16
AF = mybir.ActivationFunctionType
ALU = mybir.AluOpType
AX = mybir.AxisListType

D_MODEL = 1024
BATCH = 2
SEQ = 16384
DEPTH = 4
D_FF = 2752
D_FFE = 688
N_EXP = 8
ALPHA = (2.0 * DEPTH) ** 0.25
LN_EPS = 1e-5
MAGIC = 12582912.0
TWO_PI = float(2 * np.pi)


class Buf:
    __slots__ = ("w", "r")

    def __init__(self):
        self.w = None
        self.r = {}


class Sem:
    __slots__ = ("h", "val", "key")

    def __init__(self, h, key):
        self.h = h
        self.val = 0
        self.key = key


class Eng:
    def __init__(self, name, obj, sem):
        self.name = name
        self.obj = obj
        self.sem = sem
        self.waited = {}


_UID = [0]


class Emitter:
    def __init__(self, nc):
        self.nc = nc
        self.es = ExitStack()
        self.sems = {}
        self.nsem = 0
        self.handles = []
        self.engs = {}
        for name, obj in (("pe", nc.tensor), ("act", nc.scalar), ("dve", nc.vector),
                          ("pool", nc.gpsimd), ("sp", nc.sync)):
            self.engs[name] = Eng(name, obj, self.new_sem("e_" + name))
        self.n_inst = 0
        self.n_wait = 0
        self.nt = 0

    def new_sem(self, name="s"):
        _UID[0] += 1
        h = self.nc.alloc_semaphore(name=name + "_%d_%d" % (self.nsem, _UID[0]))
        self.handles.append(h)
        s = Sem(h, self.nsem)
        self.sems[s.key] = s
        self.nsem += 1
        return s

    def sbuf(self, shape, dtype, name=None):
        _UID[0] += 1
        return self.es.enter_context(self.nc.sbuf_tensor((name or "t") + "_%d" % _UID[0], list(shape), dtype))

    def psum(self, shape, dtype=F32, name=None):
        _UID[0] += 1
        return self.es.enter_context(self.nc.psum_tensor((name or "p") + "_%d" % _UID[0], list(shape), dtype))

    def _wait(self, E, deps):
        best = {}
        for (k, v) in deps:
            if v > best.get(k, 0):
                best[k] = v
        for k, v in best.items():
            if E.waited.get(k, 0) >= v:
                continue
            if E.name == "pe" and k == E.sem.key:
                continue
            E.obj.wait_ge(self.sems[k].h, v)
            E.waited[k] = v
            self.n_wait += 1

    @staticmethod
    def _deps(r, w):
        deps = []
        for b in r:
            if b.w is not None:
                deps.append(b.w)
        for b in w:
            if b.w is not None:
                deps.append(b.w)
            deps.extend(b.r.items())
        return deps

    @staticmethod
    def _mark(d, r, w):
        for b in r:
            if d[1] > b.r.get(d[0], 0):
                b.r[d[0]] = d[1]
        for b in w:
            b.w = d
            b.r = {}

    def do(self, eng, fn, r=(), w=()):
        E = self.engs[eng]
        self._wait(E, self._deps(r, w))
        ins = fn(E.obj)
        E.sem.val += 1
        ins.then_inc(E.sem.h, 1)
        self._mark((E.sem.key, E.sem.val), r, w)
        self.n_inst += 1
        return ins

    def dma(self, q, sem, out, in_, r=(), w=()):
        E = self.engs[q]
        self._wait(E, self._deps(r, w))
        ins = E.obj.dma_start(out=out, in_=in_)
        sem.val += 16
        ins.then_inc(sem.h, 16)
        self._mark((sem.key, sem.val), r, w)
        self.n_inst += 1
        return ins

    def dma_multi(self, q, sem, pairs, r=(), w=()):
        E = self.engs[q]
        self._wait(E, self._deps(r, w))
        for (out, in_) in pairs:
            ins = E.obj.dma_start(out=out, in_=in_)
            sem.val += 16
            ins.then_inc(sem.h, 16)
            self.n_inst += 1
        self._mark((sem.key, sem.val), r, w)

    def group_mark(self, sem, ts):
        for t in ts:
            t.b.w = (sem.key, sem.val)

    def finish(self, sems):
        E = self.engs["sp"]
        for s in sems:
            if s.val > 0:
                E.obj.wait_ge(s.h, s.val)

    def finish_all(self):
        E = self.engs["sp"]
        for s in self.sems.values():
            if s.val > 0:
                E.obj.wait_ge(s.h, s.val)

    def close(self):
        self.es.close()


class T:
    __slots__ = ("t", "b")

    def __init__(self, t):
        self.t = t
        self.b = Buf()


NFM = 704
NTM = 448


class _Stop(Exception):
    pass


def _end_phase(nc, em):
    em.finish_all()
    nc.all_engine_barrier()
    nc.clear_and_free_semaphores(em.handles)
    em.close()
    nc.all_engine_barrier()


def build_B(S, phases=('s5', 'ret', 'sb'), stop=None, io=None):
    try:
        return _build_B(S, phases, stop, io)
    except _Stop as e:
        em = e.args[0]
        em.finish_all()
        em.close()
        return em.nc, em


def _build_B(S, phases, stop, io):
    NT = S // 512
    nc = io.nc if io is not None else bass.Bass("TRN2", target_bir_lowering=False)

    def din(name, shape, dt=F32):
        if io is not None:
            return io.get("B", name)
        return nc.dram_tensor(name, list(shape), dt, kind="ExternalInput").ap()

    def dout(name, shape, dt=F32):
        if io is not None:
            return None
        return nc.dram_tensor(name, list(shape), dt, kind="ExternalOutput").ap()

    xT = din("xT", [1024, S], BF16) if io is None else None
    wfm_d = din("wfm", [128, 8, NFM])
    wtm_d = din("wtm", [128, 8, NTM])
    cosF_d = din("cosF", [128, S])
    sinF_d = din("sinF", [128, S])
    cosT_d = din("cosT", [S, 128])
    sinT_d = din("sinT", [S, 128])
    maskT_d = din("maskT", [128, 2, 512])
    qdec_d = din("qdec", [128, 512])
    kdec_d = din("kdec", [128, 2])
    gC_d = din("gC", [128, 1])
    s5p_d = din("s5p", [128, 2, 3])
    bt_d = din("bt", [128, 2, 2, 128])
    ct_d = din("ct", [128, 2, 2, 64])
    dm_d = din("dmat", [128, 64])
    iota_d = din("iota", [128, 513])
    m01_d = din("m01", [128, 128])
    mneg_d = din("mneg", [128, 128])
    ident_d = din("ident", [128, 128])

    yssm_o = dout("yssmT", [64, S])
    oret_o = dout("oretT", [128, S])
    osb_o = dout("osbT", [64, S])

    em = Emitter(nc)

    def CP(tag):
        if stop == tag:
            raise _Stop(em)
    sb = lambda shape, dt=F32: T(em.sbuf(shape, dt))
    ldw = em.new_sem("ldw"); ldh = em.new_sem("ldh")
    st_y = em.new_sem("sty"); st_r = [em.new_sem("str0"), em.new_sem("str1")]; st_s = em.new_sem("sts")

    wfm = sb([128, 8, NFM], BF16)
    wtm = sb([128, 8, NTM], BF16)
    for k in range(8):
        em.dma("pool", ldw, wfm.t[:, k, :], wfm_d[:, k, :])
        em.dma("pool", ldw, wtm.t[:, k, :], wtm_d[:, k, :])
    maskT = sb([128, 2, 512]); em.dma("sp", ldh, maskT.t[:], maskT_d)
    qdec = sb([128, 512]); em.dma("sp", ldh, qdec.t[:], qdec_d)
    kdec = sb([128, 2]); em.dma("sp", ldh, kdec.t[:], kdec_d)
    gC = sb([128, 1]); em.dma("sp", ldh, gC.t[:], gC_d)
    s5p = sb([128, 2, 3]); em.dma("sp", ldh, s5p.t[:], s5p_d)
    bt = sb([128, 2, 2, 128], BF16); em.dma("pool", ldw, bt.t[64:128], bt_d[64:128])
    ct = sb([128, 2, 2, 64], BF16); em.dma("pool", ldw, ct.t[:], ct_d)
    dmat = sb([128, 64], BF16); em.dma("pool", ldw, dmat.t[64:128], dm_d[64:128])
    iota = sb([128, 513]); em.dma("sp", ldh, iota.t[:], iota_d)
    m01 = sb([128, 128]); em.dma("sp", ldh, m01.t[:], m01_d)
    mneg = sb([128, 128]); em.dma("sp", ldh, mneg.t[:], mneg_d)
    ident = sb([128, 128], BF16); em.dma("pool", ldw, ident.t[:], ident_d)
    em.group_mark(ldw, [wfm, wtm, bt, ct, dmat, ident])
    em.group_mark(ldh, [maskT, qdec, kdec, gC, s5p, iota, m01, mneg])
    ones = sb([128, 513]); em.do("dve", lambda e: e.memset(ones.t[:], 1.0), w=[ones.b])

    CP("c1")
    m = [sb([128, 513]) for _ in range(4)]
    wre = sb([128, 513]); wim = sb([128, 513]); xr = sb([128, 513]); xi = sb([128, 513])
    sc = lambda: sb([128, 2])
    dtt = sc(); em.do("act", lambda e: e.activation(out=dtt.t[:], in_=s5p.t[:, :, 2], func=AF.Exp), r=[s5p.b], w=[dtt.b])
    aa = sc(); em.do("dve", lambda e: e.tensor_tensor(out=aa.t[:], in0=s5p.t[:, :, 0], in1=dtt.t[:], op=ALU.mult), r=[s5p.b, dtt.b], w=[aa.b])
    th = sc(); em.do("dve", lambda e: e.tensor_tensor(out=th.t[:], in0=s5p.t[:, :, 1], in1=dtt.t[:], op=ALU.mult), r=[s5p.b, dtt.b], w=[th.b])
    rr = sc(); em.do("act", lambda e: e.activation(out=rr.t[:], in_=aa.t[:], func=AF.Exp), r=[aa.b], w=[rr.b])

    CP("c2")

    def sincos(out_sin, out_cos, ang, shape):
        n = shape[1]
        A = lambda x_: ang.t[:, 0:n] if x_ is ang else x_.t[:, 0:n]
        for (o, shift) in ((out_sin, 0.0), (out_cos, 0.25)):
            t = m[0]; k = m[1]
            em.do("dve", lambda e: e.tensor_scalar(out=t.t[:, 0:n], in0=ang.t[:, 0:n], scalar1=1.0 / TWO_PI, scalar2=shift, op0=ALU.mult, op1=ALU.add), r=[ang.b], w=[t.b])
            em.do("dve", lambda e: e.tensor_scalar(out=k.t[:, 0:n], in0=t.t[:, 0:n], scalar1=MAGIC, scalar2=None, op0=ALU.add), r=[t.b], w=[k.b])
            em.do("dve", lambda e: e.tensor_scalar(out=k.t[:, 0:n], in0=k.t[:, 0:n], scalar1=MAGIC, scalar2=None, op0=ALU.subtract), r=[k.b], w=[k.b])
            em.do("dve", lambda e: e.tensor_tensor(out=t.t[:, 0:n], in0=t.t[:, 0:n], in1=k.t[:, 0:n], op=ALU.subtract), r=[t.b, k.b], w=[t.b])
            em.do("dve", lambda e: e.tensor_scalar(out=t.t[:, 0:n], in0=t.t[:, 0:n], scalar1=TWO_PI, scalar2=3.14159, op0=ALU.mult, op1=ALU.min), r=[t.b], w=[t.b])
            em.do("dve", lambda e: e.tensor_scalar(out=t.t[:, 0:n], in0=t.t[:, 0:n], scalar1=-3.14159, scalar2=None, op0=ALU.max), r=[t.b], w=[t.b])
            em.do("act", lambda e: e.activation(out=o.t[:, 0:n], in_=t.t[:, 0:n], func=AF.Sin), r=[t.b], w=[o.b])

    sn = sc(); cs_ = sc()
    sincos(sn, cs_, th, [128, 2])

    CP("c3")

    def tt(out, a, b, op, eng="dve"):
        em.do(eng, lambda e: e.tensor_tensor(out=out.t[:], in0=a.t[:], in1=b.t[:], op=op), r=[a.b, b.b], w=[out.b])

    nr = sc(); tt(nr, rr, cs_, ALU.mult)
    em.do("dve", lambda e: e.tensor_scalar(out=nr.t[:], in0=nr.t[:], scalar1=-1.0, scalar2=None, op0=ALU.add), r=[nr.b], w=[nr.b])
    ni = sc(); tt(ni, rr, sn, ALU.mult)
    lre = sc(); em.do("dve", lambda e: e.tensor_copy(out=lre.t[:], in_=s5p.t[:, :, 0]), r=[s5p.b], w=[lre.b])
    lim = sc(); em.do("dve", lambda e: e.tensor_copy(out=lim.t[:], in_=s5p.t[:, :, 1]), r=[s5p.b], w=[lim.b])
    den = sc(); t0 = sc()
    tt(den, lre, lre, ALU.mult); tt(t0, lim, lim, ALU.mult); tt(den, den, t0, ALU.add)
    rden = sc(); em.do("dve", lambda e: e.reciprocal(out=rden.t[:], in_=den.t[:]), r=[den.b], w=[rden.b])
    cre = sc(); cim = sc(); t1 = sc()
    tt(cre, nr, lre, ALU.mult); tt(t1, ni, lim, ALU.mult); tt(cre, cre, t1, ALU.add); tt(cre, cre, rden, ALU.mult)
    tt(cim, ni, lre, ALU.mult); tt(t1, nr, lim, ALU.mult); tt(cim, cim, t1, ALU.subtract); tt(cim, cim, rden, ALU.mult)

    CP("c4")
    T1re = [None, None]; T1im = [None, None]; T2re = [None, None]; T2im = [None, None]; Rfull = [None, None]
    Ere = sc(); Eim = sc()
    for rb in range(2):
        ang = m[2]
        em.do("dve", lambda e: e.tensor_scalar(out=ang.t[:], in0=iota.t[:], scalar1=th.t[:, rb:rb + 1], scalar2=None, op0=ALU.mult), r=[iota.b, th.b], w=[ang.b])
        sT = sb([128, 513]); cT = sb([128, 513])
        sincos(sT, cT, ang, [128, 513])
        T2re[rb] = cT; T2im[rb] = sT
        em.do("dve", lambda e: e.tensor_copy(out=Ere.t[:, rb:rb + 1], in_=cT.t[:, 512:513]), r=[cT.b], w=[Ere.b])
        em.do("dve", lambda e: e.tensor_copy(out=Eim.t[:, rb:rb + 1], in_=sT.t[:, 512:513]), r=[sT.b], w=[Eim.b])
        a1 = m[3]; a2 = wre; t1r = sb([128, 512]); t1i = sb([128, 512])
        em.do("dve", lambda e: e.tensor_scalar(out=a1.t[:, 0:512], in0=cT.t[:, 0:512], scalar1=cre.t[:, rb:rb + 1], scalar2=None, op0=ALU.mult), r=[cT.b, cre.b], w=[a1.b])
        em.do("dve", lambda e: e.scalar_tensor_tensor(out=t1r.t[:], in0=sT.t[:, 0:512], scalar=cim.t[:, rb:rb + 1], in1=a1.t[:, 0:512], op0=ALU.mult, op1=ALU.add), r=[sT.b, cim.b, a1.b], w=[t1r.b])
        em.do("dve", lambda e: e.tensor_scalar(out=a2.t[:, 0:512], in0=sT.t[:, 0:512], scalar1=cre.t[:, rb:rb + 1], scalar2=None, op0=ALU.mult), r=[sT.b, cre.b], w=[a2.b])
        em.do("dve", lambda e: e.scalar_tensor_tensor(out=t1i.t[:], in0=cT.t[:, 0:512], scalar=cim.t[:, rb:rb + 1], in1=a2.t[:, 0:512], op0=ALU.mult, op1=ALU.subtract), r=[cT.b, cim.b, a2.b], w=[t1i.b])
        T1re[rb] = t1r; T1im[rb] = t1i
        rf = sb([128, 512])
        em.do("dve", lambda e: e.tensor_scalar(out=rf.t[:], in0=ones.t[:, 0:512], scalar1=rr.t[:, rb:rb + 1], scalar2=None, op0=ALU.mult), r=[ones.b, rr.b], w=[rf.b])
        Rfull[rb] = rf
    ctn = sb([128, 2, 64], BF16)
    em.do("dve", lambda e: e.tensor_scalar(out=ctn.t[:], in0=ct.t[:, 1, :, :], scalar1=-1.0, scalar2=None, op0=ALU.mult), r=[ct.b], w=[ctn.b])
    car = [[sb([128, 1]) for _ in range(2)] for _ in range(2)]
    for rb in range(2):
        for c in range(2):
            em.do("dve", lambda e: e.memset(car[rb][c].t[:], 0.0), w=[car[rb][c].b])

    CP("c5")
    KT = em.sbuf([64, S], BF16)
    KTb = [Buf() for _ in range(NT)]
    VR = em.sbuf([128, S // 128, 64], BF16)
    VRb = [Buf() for _ in range(NT)]
    Sall = [sb([128, 5, 64]) for _ in range(2)]
    em.do("dve", lambda e: e.memset(Sall[0].t[:], 0.0), w=[Sall[0].b])
    em.do("dve", lambda e: e.memset(Sall[1].t[:], 0.0), w=[Sall[1].b])

    PB = [T(em.psum([128, 512])) for _ in range(7)]
    PT = T(em.psum([128, 1024], BF16))

    xt = [sb([128, 8, 512], BF16) for _ in range(2)]
    xsem = [[em.new_sem("x") for _ in range(5)] for _ in range(2)]
    xsem[1][1:] = xsem[0][1:]
    _cF = sb([128, 512]); _sF = sb([128, 512]); _cTm = sb([128, 4, 128]); _sTm = sb([128, 4, 128])
    cF = [_cF, _cF]; sF = [_sF, _sF]; cTm = [_cTm, _cTm]; sTm = [_sTm, _sTm]
    FU = [sb([128, 512], BF16) for _ in range(2)]
    qh = sb([128, 512], BF16); qtl = sb([128, 512], BF16); kh = sb([128, 512], BF16)
    qf = sb([128, 512])
    ktm = sb([128, 4, 128], BF16); vtm = sb([128, 4, 128], BF16); vdm = sb([128, 4, 128], BF16)
    g1 = sb([128, 128]); g2 = sb([128, 128])
    sTs = [sb([128, 512], BF16) for _ in range(2)]
    Sbf = sb([128, 4, 64], BF16)
    oret_t = [sb([64, 512]) for _ in range(2)]
    yss_t = sb([64, 512])
    osb_t = sb([64, 512])
    Xre = sb([128, 512], BF16); Xim = sb([128, 512], BF16)
    tA = sb([128, 1]); tB = sb([128, 1])
    NBUF = 2
    e_t = [sb([128, 512]) for _ in range(NBUF)]
    sp_t = [sb([128, 513]) for _ in range(NBUF)]
    cs_t = [sb([128, 513]) for _ in range(NBUF)]
    t_t = [sb([128, 512]) for _ in range(NBUF)]
    A_t = [sb([128, 512], BF16) for _ in range(NBUF)]
    AT_t = [sb([128, 512], BF16) for _ in range(NBUF)]
    for i in range(NBUF):
        em.do("dve", lambda e: e.memset(sp_t[i].t[:], 0.0), w=[sp_t[i].b])
    negR = [sb([128, 1]) for _ in range(4)]

    def load_tile(i):
        s = i % 2
        if io is None:
            em.dma("sp", xsem[s][0], xt[s].t[:], xT[:, i * 512:(i + 1) * 512].rearrange("(k p) t -> p k t", p=128), w=[xt[s].b])
        else:
            em.dma_multi("sp", xsem[s][0], [(xt[s].t[:, k, :], io.xT_tile(i, k)) for k in range(8)], w=[xt[s].b])

    def load_tabs(i):
        s = i % 2
        em.dma("sp", xsem[s][1], cF[s].t[:], cosF_d[:, i * 512:(i + 1) * 512], w=[cF[s].b])
        em.dma("sp", xsem[s][2], sF[s].t[:], sinF_d[:, i * 512:(i + 1) * 512], w=[sF[s].b])
        em.dma("sp", xsem[s][3], cTm[s].t[:], cosT_d[i * 512:(i + 1) * 512, :].rearrange("(c p) d -> p c d", p=128), w=[cTm[s].b])
        em.dma("sp", xsem[s][4], sTm[s].t[:], sinT_d[i * 512:(i + 1) * 512, :].rearrange("(c p) d -> p c d", p=128), w=[sTm[s].b])

    load_tile(0)
    load_tabs(0)
    sbit = 0
    for i in (range(NT) if 'noloop' not in phases else ()):
        s = i % 2
        if i + 1 < NT:
            load_tile(i + 1)
        X = xt[s]
        fu = FU[s]

        def fm_group(pb, c0, M):
            for k in range(8):
                em.do("pe", lambda e: e.matmul(pb.t[:M, :], wfm.t[:, k, c0:c0 + M], X.t[:, k, :], start=(k == 0), stop=(k == 7)),
                      r=[wfm.b, X.b], w=[pb.b])

        def rope_fm(pa, pb_, outf):
            em.do("dve", lambda e: e.tensor_tensor(out=m[0].t[:, 0:512], in0=pa.t[:], in1=cF[s].t[:], op=ALU.mult), r=[pa.b, cF[s].b], w=[m[0].b])
            em.do("dve", lambda e: e.tensor_tensor(out=m[1].t[:, 0:512], in0=pb_.t[:], in1=sF[s].t[:], op=ALU.mult), r=[pb_.b, sF[s].b], w=[m[1].b])
            em.do("pool", lambda e: e.tensor_tensor(out=outf.t[:], in0=m[0].t[:, 0:512], in1=m[1].t[:, 0:512], op=ALU.add), r=[m[0].b, m[1].b], w=[outf.b])

        fm_group(PB[0], 0, 128); fm_group(PB[1], 128, 128)
        rope_fm(PB[0], PB[1], qf)
        em.do("act", lambda e: e.activation(out=qh.t[:], in_=qf.t[:], func=AF.Copy), r=[qf.b], w=[qh.b])
        em.do("pool", lambda e: e.tensor_tensor(out=qtl.t[:], in0=qf.t[:], in1=qdec.t[:], op=ALU.mult), r=[qf.b, qdec.b], w=[qtl.b])
        fm_group(PB[0], 256, 128); fm_group(PB[1], 384, 128)
        rope_fm(PB[0], PB[1], kh)
        fm_group(PB[0], 512, 128)
        em.do("act", lambda e: e.activation(out=fu.t[:], in_=PB[0].t[:], func=AF.Copy), r=[PB[0].b], w=[fu.b])
        fm_group(PB[1], 640, 64)
        em.do("act", lambda e: e.activation(out=KT[:, i * 512:(i + 1) * 512], in_=PB[1].t[:64, :], func=AF.Copy), r=[PB[1].b], w=[KTb[i]])
        CP("p1")
        for blk in range(4):
            pb = PB[2 + (blk % 2)]
            for k in range(8):
                em.do("pe", lambda e: e.matmul(pb.t[:, :NTM], X.t[:, k, blk * 128:(blk + 1) * 128], wtm.t[:, k, :], start=(k == 0), stop=(k == 7)),
                      r=[wtm.b, X.b], w=[pb.b])
            CP("q1")
            em.do("dve", lambda e: e.tensor_tensor(out=g1.t[:], in0=pb.t[:, 0:128], in1=cTm[s].t[:, blk, :], op=ALU.mult), r=[pb.b, cTm[s].b], w=[g1.b])
            em.do("dve", lambda e: e.tensor_tensor(out=g2.t[:], in0=pb.t[:, 128:256], in1=sTm[s].t[:, blk, :], op=ALU.mult), r=[pb.b, sTm[s].b], w=[g2.b])
            em.do("pool", lambda e: e.tensor_tensor(out=ktm.t[:, blk, :], in0=g1.t[:], in1=g2.t[:], op=ALU.add), r=[g1.b, g2.b], w=[ktm.b])
            CP("q2")
            em.do("dve", lambda e: e.tensor_copy(out=vtm.t[:, blk, :], in_=pb.t[:, 256:384]), r=[pb.b], w=[vtm.b])
            CP("q3")
            for h in range(2):
                em.do("dve", lambda e: e.tensor_scalar(out=vdm.t[:, blk, h * 64:(h + 1) * 64], in0=pb.t[:, 256 + h * 64:256 + (h + 1) * 64],
                                                       scalar1=kdec.t[:, h:h + 1], scalar2=None, op0=ALU.mult), r=[pb.b, kdec.b], w=[vdm.b])
            CP("q4")
            em.do("dve", lambda e: e.tensor_copy(out=VR[:, i * 4 + blk, :], in_=pb.t[:, 384:448]), r=[pb.b], w=[VRb[i]])

        if i + 1 < NT:
            load_tabs(i + 1)
        CP("p2")
        for rb in (range(2) if 's5' in phases else ()):
            em.do("pe", lambda e: e.matmul(PB[0].t[:, :], bt.t[64:128, 0, rb, :], fu.t[64:128, :], start=True, stop=True), r=[bt.b, fu.b], w=[PB[0].b])
            em.do("pe", lambda e: e.matmul(PB[1].t[:, :], bt.t[64:128, 1, rb, :], fu.t[64:128, :], start=True, stop=True), r=[bt.b, fu.b], w=[PB[1].b])
            pr, pi = PB[0], PB[1]
            mm_ = lambda o, a, b_: em.do("dve", lambda e: e.tensor_tensor(out=o.t[:, 0:512], in0=a.t[:, 0:512], in1=b_.t[:, 0:512], op=ALU.mult), r=[a.b, b_.b], w=[o.b])
            tt5 = lambda o, a, b_, op, eng: em.do(eng, lambda e: e.tensor_tensor(out=o.t[:, 0:512], in0=a.t[:, 0:512], in1=b_.t[:, 0:512], op=op), r=[a.b, b_.b], w=[o.b])
            mm_(m[0], pr, T1re[rb]); mm_(m[1], pi, T1im[rb]); mm_(m[2], pi, T1re[rb]); mm_(m[3], pr, T1im[rb])
            tt5(wre, m[0], m[1], ALU.subtract, "pool"); tt5(wim, m[2], m[3], ALU.add, "pool")
            for (xo, wi, c) in ((xr, wre, 0), (xi, wim, 1)):
                em.do("dve", lambda e: e.tensor_tensor_scan(out=xo.t[:, 0:512], data0=Rfull[rb].t[:], data1=wi.t[:, 0:512], initial=car[rb][c].t[:, 0:1],
                                                            op0=ALU.mult, op1=ALU.add), r=[Rfull[rb].b, wi.b, car[rb][c].b], w=[xo.b])
            em.do("dve", lambda e: e.tensor_tensor(out=tA.t[:], in0=xi.t[:, 511:512], in1=Eim.t[:, rb:rb + 1], op=ALU.mult), r=[xi.b, Eim.b], w=[tA.b])
            em.do("dve", lambda e: e.scalar_tensor_tensor(out=car[rb][0].t[:], in0=xr.t[:, 511:512], scalar=Ere.t[:, rb:rb + 1], in1=tA.t[:], op0=ALU.mult, op1=ALU.subtract),
                  r=[xr.b, Ere.b, tA.b], w=[car[rb][0].b])
            em.do("dve", lambda e: e.tensor_tensor(out=tB.t[:], in0=xr.t[:, 511:512], in1=Eim.t[:, rb:rb + 1], op=ALU.mult), r=[xr.b, Eim.b], w=[tB.b])
            em.do("dve", lambda e: e.scalar_tensor_tensor(out=car[rb][1].t[:], in0=xi.t[:, 511:512], scalar=Ere.t[:, rb:rb + 1], in1=tB.t[:], op0=ALU.mult, op1=ALU.add),
                  r=[xi.b, Ere.b, tB.b], w=[car[rb][1].b])
            pm = lambda o, a, b_: em.do("pool", lambda e: e.tensor_tensor(out=o.t[:, 0:512], in0=a.t[:, 0:512], in1=b_.t[:, 0:512], op=ALU.mult), r=[a.b, b_.b], w=[o.b])
            pm(m[0], xr, T2re[rb]); pm(m[1], xi, T2im[rb]); mm_(m[2], xi, T2re[rb]); mm_(m[3], xr, T2im[rb])
            tt5(Xre, m[0], m[1], ALU.subtract, "pool"); tt5(Xim, m[2], m[3], ALU.add, "dve")
            em.do("pe", lambda e: e.matmul(PB[3].t[:64, :], ct.t[:, 0, rb, :], Xre.t[:], start=(rb == 0), stop=False), r=[ct.b, Xre.b], w=[PB[3].b])
            em.do("pe", lambda e: e.matmul(PB[3].t[:64, :], ctn.t[:, rb, :], Xim.t[:], start=False, stop=False), r=[ctn.b, Xim.b], w=[PB[3].b])
        if 's5' in phases:
            em.do("pe", lambda e: e.matmul(PB[3].t[:64, :], dmat.t[64:128, :], fu.t[64:128, :], start=False, stop=True), r=[dmat.b, fu.b], w=[PB[3].b])
            em.do("act", lambda e: e.activation(out=yss_t.t[:], in_=PB[3].t[:64, :], func=AF.Copy), r=[PB[3].b], w=[yss_t.b])
            em.dma("sp", st_y, yssm_o[:, i * 512:(i + 1) * 512] if io is None else io.mix_out(i, 0, 64), yss_t.t[:], r=[yss_t.b])

        CP("p3")
        for _once in ([0] if 'ret' in phases else []):
            Sc = Sall[i % 2]; Sn = Sall[(i + 1) % 2]
            for c in range(4):
                em.do("pe", lambda e: e.matmul(PB[5].t[:, c * 128:(c + 1) * 128], ktm.t[:, c, :], vdm.t[:, c, :], start=True, stop=True), r=[ktm.b, vdm.b], w=[PB[5].b])
            for c in range(4):
                for h in range(2):
                    P = slice(h * 64, (h + 1) * 64)
                    em.do("dve", lambda e: e.scalar_tensor_tensor(out=Sc.t[P, c + 1, :], in0=Sc.t[P, c, :], scalar=gC.t[P, 0:1],
                                                                  in1=PB[5].t[P, c * 128 + h * 64:c * 128 + (h + 1) * 64], op0=ALU.mult, op1=ALU.add),
                          r=[Sc.b, gC.b, PB[5].b], w=[Sc.b])
            em.do("act", lambda e: e.activation(out=Sbf.t[:], in_=Sc.t[:, 0:4, :], func=AF.Copy), r=[Sc.b], w=[Sbf.b])
            em.do("dve", lambda e: e.tensor_copy(out=Sn.t[:, 0, :], in_=Sc.t[:, 4, :]), r=[Sc.b], w=[Sn.b])
            for h in range(2):
                P = slice(h * 64, (h + 1) * 64)
                for c in range(4):
                    em.do("pe", lambda e: e.matmul(PB[4].t[:, c * 128:(c + 1) * 128], kh.t[P, c * 128:(c + 1) * 128], qh.t[P, c * 128:(c + 1) * 128], start=True, stop=True),
                          r=[kh.b, qh.b], w=[PB[4].b])
                em.do("dve", lambda e: e.tensor_tensor(out=sTs[h].t[:], in0=PB[4].t[:], in1=maskT.t[:, h, :], op=ALU.mult), r=[PB[4].b, maskT.b], w=[sTs[h].b])
                for c in range(4):
                    cs = slice(c * 128, (c + 1) * 128)
                    em.do("pe", lambda e: e.matmul(PB[6].t[:64, cs], vtm.t[:, c, h * 64:(h + 1) * 64], sTs[h].t[:, cs], start=True, stop=False),
                          r=[vtm.b, sTs[h].b], w=[PB[6].b])
                    em.do("pe", lambda e: e.matmul(PB[6].t[:64, cs], Sbf.t[P, c, :], qtl.t[P, cs], start=False, stop=True),
                          r=[Sbf.b, qtl.b], w=[PB[6].b])
                em.do("act", lambda e: e.activation(out=oret_t[h].t[:], in_=PB[6].t[:64, :], func=AF.Copy), r=[PB[6].b], w=[oret_t[h].b])
                em.dma("sp", st_r[h], oret_o[h * 64:(h + 1) * 64, i * 512:(i + 1) * 512] if io is None else io.mix_out(i, 64 + h * 64, 64), oret_t[h].t[:], r=[oret_t[h].b])

        CP("p4")
        for _once in ([0] if 'sb' in phases else []):
            for qb in range(4):
                nR = negR[qb]
                em.do("pool", lambda e: e.memset(nR.t[:], 0.0), w=[nR.b])
                qs = slice(qb * 128, (qb + 1) * 128)
                nmm = sum(((qb + 1) if kt == i else 4) for kt in range(i + 1))
                imm = 0
                for kt in range(i, -1, -1):
                    nblk = (qb + 1) if kt == i else 4
                    nco = nblk * 128
                    u = sbit % NBUF
                    zb = PB[sbit % 2]
                    sbit += 1
                    E_, SP, CS, TT, A_, AT = e_t[u], sp_t[u], cs_t[u], t_t[u], A_t[u], AT_t[u]
                    em.do("pe", lambda e: e.matmul(zb.t[:, :nco], fu.t[0:64, qs], KT[:, kt * 512:kt * 512 + nco], start=True, stop=True),
                          r=[fu.b, KTb[kt]], w=[zb.b])
                    em.do("act", lambda e: e.activation(out=E_.t[:, :nco], in_=zb.t[:, :nco], func=AF.Exp, scale=0.125), r=[zb.b], w=[E_.b])
                    em.do("act", lambda e: e.activation(out=SP.t[:, 1:1 + nco], in_=E_.t[:, :nco], func=AF.Ln, bias=1.0, scale=1.0), r=[E_.b], w=[SP.b])
                    if kt == i:
                        em.do("pool", lambda e: e.tensor_tensor(out=SP.t[:, 1 + nco - 128:1 + nco], in0=SP.t[:, 1 + nco - 128:1 + nco], in1=m01.t[:], op=ALU.mult),
                              r=[SP.b, m01.b], w=[SP.b])
                    em.do("dve", lambda e: e.tensor_tensor_scan(out=CS.t[:, 0:nco + 1], data0=ones.t[:, 0:nco + 1], data1=SP.t[:, 0:nco + 1], initial=0.0,
                                                                op0=ALU.mult, op1=ALU.add), r=[ones.b, SP.b], w=[CS.b])
                    em.do("dve", lambda e: e.tensor_tensor(out=nR.t[:], in0=nR.t[:], in1=CS.t[:, nco:nco + 1], op=ALU.subtract), r=[nR.b, CS.b], w=[nR.b])
                    em.do("dve", lambda e: e.scalar_tensor_tensor(out=TT.t[:, :nco], in0=zb.t[:, :nco], scalar=0.125, in1=CS.t[:, 0:nco], op0=ALU.mult, op1=ALU.add),
                          r=[zb.b, CS.b], w=[TT.b])
                    if kt == i:
                        em.do("pool", lambda e: e.tensor_tensor(out=TT.t[:, nco - 128:nco], in0=TT.t[:, nco - 128:nco], in1=mneg.t[:], op=ALU.add),
                              r=[TT.b, mneg.b], w=[TT.b])
                    em.do("act", lambda e: e.activation(out=A_.t[:, :nco], in_=TT.t[:, :nco], func=AF.Exp, bias=nR.t[:, 0:1], scale=1.0), r=[TT.b, nR.b], w=[A_.b])
                    for blk in range(nblk):
                        bs = slice(blk * 128, (blk + 1) * 128)
                        em.do("pe", lambda e: e.transpose(PT.t[:, bs], A_.t[:, bs], ident.t[:]), r=[A_.b, ident.b], w=[PT.b])
                    em.do("dve", lambda e: e.tensor_copy(out=AT.t[:, :nco], in_=PT.t[:, :nco]), r=[PT.b], w=[AT.b])
                    for blk in range(nblk):
                        bs = slice(blk * 128, (blk + 1) * 128)
                        em.do("pe", lambda e: e.matmul(PB[2].t[:64, qs], VR[:, kt * 4 + blk, :], AT.t[:, bs], start=(imm == 0), stop=(imm == nmm - 1)),
                              r=[VRb[kt], AT.b], w=[PB[2].b])
                        imm += 1
            em.do("act", lambda e: e.activation(out=osb_t.t[:], in_=PB[2].t[:64, :], func=AF.Copy), r=[PB[2].b], w=[osb_t.b])
            em.dma("sp", st_s, osb_o[:, i * 512:(i + 1) * 512] if io is None else io.mix_out(i, 192, 64), osb_t.t[:], r=[osb_t.b])

    if io is not None:
        _end_phase(nc, em)
        return nc, em
    em.finish([st_y, st_r[0], st_r[1], st_s])
    em.close()
    return nc, em


def _rope_tables(S):
    half = 32
    inv = (np.float32(10000.0) ** (-np.arange(half, dtype=np.float32) / np.float32(half))).astype(np.float32)
    pos = np.arange(S, dtype=np.float32)
    ang = (pos[:, None] * inv[None, :]).astype(np.float32)
    c = np.cos(ang).astype(np.float32)
    s = np.sin(ang).astype(np.float32)
    cos64 = np.concatenate([c, c], axis=1)
    sin64 = np.concatenate([-s, s], axis=1)
    cosT = np.concatenate([cos64, cos64], axis=1)
    sinT = np.concatenate([sin64, sin64], axis=1)
    return np.ascontiguousarray(cosT.T), np.ascontiguousarray(sinT.T), np.ascontiguousarray(cosT), np.ascontiguousarray(sinT)


def _ret_consts(j):
    C = 128
    idx = np.arange(C, dtype=np.float32)
    maskT = np.zeros((128, 2, 512), np.float32)
    qdec = np.zeros((128, 512), np.float32)
    kdec = np.zeros((128, 2), np.float32)
    gC = np.zeros((128, 1), np.float32)
    for hl in range(2):
        h = 2 * j + hl
        lg = np.log(np.float32(1.0) - np.float32(2.0) ** np.float32(-5.0 - h)).astype(np.float32)
        rel = idx[:, None] - idx[None, :]
        dm = np.where(rel >= 0, np.exp(lg * np.maximum(rel, 0.0)), 0.0).astype(np.float32)
        mT = (dm.T * np.float32(0.125)).astype(np.float32)
        maskT[:, hl, :] = np.tile(mT, (1, 4))
        qd = np.exp(lg * (idx + 1.0)).astype(np.float32)
        qdec[hl * 64:(hl + 1) * 64, :] = np.tile(qd[None, :], (64, 4))
        kdec[:, hl] = np.exp(lg * (C - 1.0 - idx)).astype(np.float32) * np.float32(0.125)
        gC[hl * 64:(hl + 1) * 64, 0] = np.exp(lg * np.float32(C))
    return maskT, qdec, kdec, gC


def _swap(cols):
    cols = np.asarray(cols).reshape(-1, 64)
    return np.concatenate([cols[:, 32:], cols[:, :32]], axis=1).reshape(-1)


def prep_B_inputs(layer, j, w_in, lam_re, lam_im, log_dt, b_re, b_im, c_re, c_im, d_skip, S, consts):
    u_cols = np.arange(64 * j, 64 * j + 64)
    rq = 256 + np.arange(128 * j, 128 * j + 128)
    rk = 768 + np.arange(128 * j, 128 * j + 128)
    rv = 1280 + np.arange(128 * j, 128 * j + 128)
    sq = 2304 + np.arange(64 * j, 64 * j + 64)
    sk = 2560 + np.arange(64 * j, 64 * j + 64)
    sv = 2816 + np.arange(64 * j, 64 * j + 64)
    fm_cols = np.concatenate([rq, _swap(rq), rk, _swap(rk), sq, u_cols, sk])
    tm_cols = np.concatenate([rk, _swap(rk), rv, sv])
    W = w_in[layer]
    wfm = np.ascontiguousarray(W[:, fm_cols].reshape(8, 128, NFM).transpose(1, 0, 2))
    wtm = np.ascontiguousarray(W[:, tm_cols].reshape(8, 128, NTM).transpose(1, 0, 2))
    G0 = 4 * j
    s5p = np.zeros((128, 2, 3), np.float32)
    bt = np.zeros((128, 2, 2, 128), np.float32)
    ct = np.zeros((128, 2, 2, 64), np.float32)
    for rb in range(2):
        for gl in range(2):
            g = G0 + 2 * rb + gl
            ps = slice(gl * 64, (gl + 1) * 64)
            s5p[ps, rb, 0] = lam_re[layer, g]
            s5p[ps, rb, 1] = lam_im[layer, g]
            s5p[ps, rb, 2] = log_dt[layer, g]
            chl = (2 * rb + gl) * 16
            bt[64 + chl:64 + chl + 16, 0, rb, ps] = b_re[layer, g].T
            bt[64 + chl:64 + chl + 16, 1, rb, ps] = b_im[layer, g].T
            ct[ps, 0, rb, chl:chl + 16] = c_re[layer, g].T
            ct[ps, 1, rb, chl:chl + 16] = c_im[layer, g].T
    dmat = np.zeros((128, 64), np.float32)
    dmat[64 + np.arange(64), np.arange(64)] = d_skip[layer, 64 * j:64 * j + 64]
    maskT, qdec, kdec, gC = _ret_consts(j)
    d = dict(wfm=wfm, wtm=wtm, s5p=s5p, bt=bt, ct=ct, dmat=dmat, maskT=maskT, qdec=qdec, kdec=kdec, gC=gC)
    d.update(consts)
    return d


def B_consts(S):
    cosF, sinF, cosT, sinT = _rope_tables(S)
    iota = np.tile(np.arange(513, dtype=np.float32)[None, :], (128, 1))
    qi = np.arange(128)
    m01 = (qi[None, :] < qi[:, None]).astype(np.float32)
    mneg = np.where(m01 > 0, 0.0, -30000.0).astype(np.float32)
    ident = np.eye(128, dtype=np.float32)
    return dict(cosF=cosF, sinF=sinF, cosT=cosT, sinT=sinT, iota=iota, m01=m01, mneg=mneg, ident=ident)


def build_P(TOK, io=None):
    if io is not None:
        nc = io.nc
        x_d = io.get("P", "x"); id_d = io.get("P", "ident"); xT_o = io.get("P", "xT")
    else:
        nc = bass.Bass("TRN2", target_bir_lowering=False)
        x_d = nc.dram_tensor("x", [TOK, 1024], F32, kind="ExternalInput").ap()
        id_d = nc.dram_tensor("ident", [128, 128], F32, kind="ExternalInput").ap()
        xT_o = nc.dram_tensor("xT", [1024, TOK], BF16, kind="ExternalOutput").ap()
    em = Emitter(nc)
    sb = lambda shape, dt=F32: T(em.sbuf(shape, dt))
    ldh = em.new_sem("ldh")
    ident = sb([128, 128]); em.dma("sp", ldh, ident.t[:], id_d); em.group_mark(ldh, [ident])
    xin = [sb([128, 1024]) for _ in range(2)]
    xs = [em.new_sem("x") for _ in range(2)]
    PTt = [T(em.psum([128, 1024])) for _ in range(2)]
    xo = [sb([128, 8, 128], BF16) for _ in range(2)]
    so = [em.new_sem("o") for _ in range(2)]
    nb = TOK // 128
    em.dma("sp", xs[0], xin[0].t[:], x_d[0:128, :], w=[xin[0].b])
    for blk in range(nb):
        s = blk % 2
        if blk + 1 < nb:
            em.dma("sp", xs[1 - s], xin[1 - s].t[:], x_d[(blk + 1) * 128:(blk + 2) * 128, :], w=[xin[1 - s].b])
        for k in range(8):
            em.do("pe", lambda e: e.transpose(PTt[s].t[:, k * 128:(k + 1) * 128], xin[s].t[:, k * 128:(k + 1) * 128], ident.t[:]),
                  r=[xin[s].b, ident.b], w=[PTt[s].b])
        em.do("dve" if s == 0 else "act",
              (lambda e: e.tensor_copy(out=xo[s].t[:], in_=PTt[s].t[:].rearrange("p (k t) -> p k t", k=8))) if s == 0 else
              (lambda e: e.activation(out=xo[s].t[:], in_=PTt[s].t[:].rearrange("p (k t) -> p k t", k=8), func=AF.Copy)),
              r=[PTt[s].b], w=[xo[s].b])
        em.dma("sp", so[s], xT_o[:, blk * 128:(blk + 1) * 128].rearrange("(k p) t -> p k t", p=128), xo[s].t[:], r=[xo[s].b])
    if io is not None:
        _end_phase(nc, em)
        return nc, em
    em.finish(so)
    em.close()
    return nc, em


FFC_DENSE = [128] * 21 + [64]
FFC_EXP = [128] * 5 + [48]


def build_C(TOK, kind, io=None):
    NTL = TOK // 512
    nc = io.nc if io is not None else bass.Bass("TRN2", target_bir_lowering=False)

    def din(name, shape, dt=F32):
        if io is not None:
            return io.get("C", name)
        return nc.dram_tensor(name, list(shape), dt, kind="ExternalInput").ap()

    def dout(name, shape, dt=F32):
        if io is not None:
            return io.get("C", name)
        return nc.dram_tensor(name, list(shape), dt, kind="ExternalOutput").ap()

    x_d = din("x", [TOK, 1024])
    xT_d = din("xT", [1024, TOK], BF16)
    if io is None:
        yss_d = din("yssmT", [256, TOK]); oret_d = din("oretT", [512, TOK]); osb_d = din("osbT", [256, TOK])
    wrg_d = din("wrg", [128, 8, 512]); gluw_d = din("gluw", [128, 2, 256]); wout_d = din("wout", [128, 8, 1024])
    vec_d = din("vecs", [128, 12])
    sbw_d = din("sbw", [128, 2])
    lnr_d = din("lnrows", [4, 128, 1024])
    blk16_d = din("blk16", [128, 128]); blk64_d = din("blk64", [128, 128]); id_d = din("ident", [128, 128])
    if kind == "dense":
        NF = 22; FFC = FFC_DENSE
        wg_d = din("wg", [NF, 128, 8, 128]); wu_d = din("wu", [NF, 128, 8, 128]); wd_d = din("wd", [128, NF, 1024])
    else:
        NF = 6; FFC = FFC_EXP
        wg_d = din("wg", [8, NF, 128, 8, 128]); wu_d = din("wu", [8, NF, 128, 8, 128]); wd_d = din("wd", [8, 128, NF, 1024])
        wr_d = din("wr", [128, 8, 8])
    x_o = dout("xo", [TOK, 1024])
    xT_o = dout("xTo", [1024, TOK], BF16)

    em = Emitter(nc)
    sb = lambda shape, dt=F32: T(em.sbuf(shape, dt))
    ldw = em.new_sem("ldw"); ldh = em.new_sem("ldh")
    wrg = sb([128, 8, 512], BF16); gluw = sb([128, 2, 256], BF16); wout = sb([128, 8, 1024], BF16)
    for k in range(8):
        em.dma("pool", ldw, wrg.t[:, k, :], wrg_d[:, k, :])
        em.dma("pool", ldw, wout.t[:, k, :], wout_d[:, k, :])
    em.dma("pool", ldw, gluw.t[:], gluw_d)
    blk16 = sb([128, 128], BF16); em.dma("pool", ldw, blk16.t[:], blk16_d)
    blk64 = sb([128, 128], BF16); em.dma("pool", ldw, blk64.t[:], blk64_d)
    gl = [wrg, gluw, wout, blk16, blk64]
    if kind == "dense":
        wd = sb([128, NF, 1024], BF16)
        for f in range(NF):
            em.dma("pool", ldw, wd.t[:, f, :], wd_d[:, f, :])
        gl.append(wd)
    em.group_mark(ldw, gl)
    vecs = sb([128, 12]); em.dma("sp", ldh, vecs.t[:], vec_d)
    sbw = sb([128, 2]); em.dma("sp", ldh, sbw.t[:], sbw_d)
    lnr = [sb([128, 1024]) for _ in range(4)]
    for q in range(4):
        em.dma("sp", ldh, lnr[q].t[:], lnr_d[q])
    ident = sb([128, 128]); em.dma("sp", ldh, ident.t[:], id_d)
    gh = [vecs, sbw, ident] + lnr
    if kind == "moe":
        wr = sb([128, 8, 8]); em.dma("sp", ldh, wr.t[:], wr_d); gh.append(wr)
    em.group_mark(ldh, gh)

    PF = [T(em.psum([128, 512])) for _ in range(6)]
    PTt = T(em.psum([128, 1024]))

    xTt = sb([128, 8, 512], BF16); s_xT = em.new_sem("xT")
    cin = [sb([128, 512]) for _ in range(3)]; s_cin = [em.new_sem("ci") for _ in range(3)]
    xtm = [sb([128, 1024]) for _ in range(2)]; s_xtm = [em.new_sem("xm") for _ in range(2)]
    NWB = 2
    wgb = [sb([128, 8, 128], BF16) for _ in range(NWB)]; wub = [sb([128, 8, 128], BF16) for _ in range(NWB)]
    s_wg = [em.new_sem("wg") for _ in range(NWB)]; s_wu = [em.new_sem("wu") for _ in range(NWB)]
    mixT = sb([128, 8, 512], BF16)
    X1 = [sb([128, 1024]) for _ in range(4)]
    x1T = sb([128, 8, 512], BF16)
    hT = sb([128, NF, 512], BF16)
    g32 = [sb([128, 512]) for _ in range(2)]; gb = [sb([128, 512], BF16) for _ in range(2)]
    tmpA = sb([128, 512]); tmpB = sb([128, 512]); tbf = sb([128, 512], BF16)
    rs = sb([128, 512])
    hbuf = sb([128, 1024])
    stats = sb([128, 2, 6]); mv = sb([128, 2]); sd1 = sb([128, 1]); rs1 = sb([128, 1])
    xo_t = [sb([128, 1024]) for _ in range(2)]; s_xo = [em.new_sem("xo") for _ in range(2)]
    xTo_t = [sb([128, 8, 128], BF16) for _ in range(2)]; s_xTo = [em.new_sem("xTo") for _ in range(2)]
    sg = [sb([128, 512]) for _ in range(2)]
    cin_i = [0]
    if kind == "moe":
        wdb = [sb([128, NF, 1024], BF16) for _ in range(2)]; s_wd = [em.new_sem("wd") for _ in range(2)]
        acc = [sb([128, 1024]) for _ in range(4)]
        x1T32 = sb([128, 8, 128])
        lg = sb([128, 8]); m8 = sb([128, 8]); nm1 = sb([128, 1]); sel = sb([128, 8]); ex = sb([128, 8]); den = sb([128, 1]); rden = sb([128, 1])
        G = [sb([128, 8]) for _ in range(4)]

    def load_chunk(which, idx, ts):
        u = cin_i[0] % 3
        cin_i[0] += 1
        if io is None:
            src = {"yss": yss_d, "oret": oret_d, "osb": osb_d}[which]
            em.dma("sp", s_cin[u], cin[u].t[:], src[idx * 128:(idx + 1) * 128, ts], w=[cin[u].b])
        else:
            em.dma_multi("sp", s_cin[u], [(cin[u].t[p0:p0 + np_, :], ap) for (p0, np_, ap) in io.mix_in(which, idx, ts)], w=[cin[u].b])
        return cin[u]

    def rstd_from_psum(ps, dst_rs):
        em.do("act", lambda e: e.activation(out=dst_rs.t[:], in_=ps.t[:], func=AF.Sqrt, bias=LN_EPS, scale=1.0), r=[ps.b], w=[dst_rs.b])
        em.do("dve", lambda e: e.reciprocal(out=dst_rs.t[:], in_=dst_rs.t[:]), r=[dst_rs.b], w=[dst_rs.b])

    def layer_norm_rows(h, wrow, brow, out):
        for c in range(2):
            em.do("dve", lambda e: e.bn_stats(out=stats.t[:, c, :], in_=h.t[:, c * 512:(c + 1) * 512]), r=[h.b], w=[stats.b])
        em.do("dve", lambda e: e.bn_aggr(out=mv.t[:], in_=stats.t[:]), r=[stats.b], w=[mv.b])
        em.do("act", lambda e: e.activation(out=sd1.t[:], in_=mv.t[:, 1:2], func=AF.Sqrt, bias=LN_EPS, scale=1.0), r=[mv.b], w=[sd1.b])
        em.do("dve", lambda e: e.reciprocal(out=rs1.t[:], in_=sd1.t[:]), r=[sd1.b], w=[rs1.b])
        em.do("dve", lambda e: e.tensor_scalar(out=h.t[:], in0=h.t[:], scalar1=mv.t[:, 0:1], scalar2=rs1.t[:, 0:1], op0=ALU.subtract, op1=ALU.mult),
              r=[h.b, mv.b, rs1.b], w=[h.b])
        em.do("dve", lambda e: e.tensor_tensor(out=h.t[:], in0=h.t[:], in1=wrow.t[:], op=ALU.mult), r=[h.b, wrow.b], w=[h.b])
        em.do("dve", lambda e: e.tensor_tensor(out=out.t[:], in0=h.t[:], in1=brow.t[:], op=ALU.add), r=[h.b, brow.b], w=[out.b])

    nwl = [0]

    def load_w(src_g, src_u):
        u = nwl[0] % NWB
        nwl[0] += 1
        em.dma("pool", s_wg[u], wgb[u].t[:], src_g, w=[wgb[u].b])
        em.dma("pool", s_wu[u], wub[u].t[:], src_u, w=[wub[u].b])
        return wgb[u], wub[u]

    gu_i = [0]

    def gate_up(wgt, wut, cw, dst_ap, dst_buf):
        p = (gu_i[0] % 2) * 2
        gu_i[0] += 1
        pg, pu = PF[p], PF[p + 1]
        sgt = sg[(gu_i[0]) % 2]
        for k in range(8):
            em.do("pe", lambda e: e.matmul(pg.t[:cw, :], wgt.t[:, k, :cw], x1T.t[:, k, :], start=(k == 0), stop=(k == 7)), r=[wgt.b, x1T.b], w=[pg.b])
        for k in range(8):
            em.do("pe", lambda e: e.matmul(pu.t[:cw, :], wut.t[:, k, :cw], x1T.t[:, k, :], start=(k == 0), stop=(k == 7)), r=[wut.b, x1T.b], w=[pu.b])
        em.do("act", lambda e: e.activation(out=sgt.t[:cw, :], in_=pg.t[:cw, :], func=AF.Silu), r=[pg.b], w=[sgt.b])
        em.do("dve", lambda e: e.tensor_tensor(out=dst_ap, in0=sgt.t[:cw, :], in1=pu.t[:cw, :], op=ALU.mult), r=[sgt.b, pu.b], w=[dst_buf])

    def transposes_to_bf16(src, dst_ap, dst_buf, also32=None):
        for k in range(8):
            em.do("pe", lambda e: e.transpose(PTt.t[:, k * 128:(k + 1) * 128], src.t[:, k * 128:(k + 1) * 128], ident.t[:]), r=[src.b, ident.b], w=[PTt.b])
        em.do("act", lambda e: e.activation(out=dst_ap, in_=PTt.t[:].rearrange("p (k t) -> p k t", k=8), func=AF.Copy), r=[PTt.b], w=[dst_buf])
        if also32 is not None:
            em.do("act", lambda e: e.activation(out=also32.t[:], in_=PTt.t[:].rearrange("p (k t) -> p k t", k=8), func=AF.Copy), r=[PTt.b], w=[also32.b])

    def finish_block(it, blk):
        u = (it * 4 + blk) % 2
        tok0 = it * 512 + blk * 128
        layer_norm_rows(hbuf, lnr[2], lnr[3], xo_t[u])
        em.dma("sp", s_xo[u], x_o[tok0:tok0 + 128, :], xo_t[u].t[:], r=[xo_t[u].b])
        transposes_to_bf16(xo_t[u], xTo_t[u].t[:], xTo_t[u].b)
        em.dma("sp", s_xTo[u], xT_o[:, tok0:tok0 + 128].rearrange("(k p) t -> p k t", p=128), xTo_t[u].t[:], r=[xTo_t[u].b])

    for it in range(NTL):
        t0 = it * 512
        ts = slice(t0, t0 + 512)
        em.dma("sp", s_xT, xTt.t[:], xT_d[:, ts].rearrange("(k p) t -> p k t", p=128), w=[xTt.b])
        for k in range(2):
            ci = load_chunk("yss", k, ts)
            em.do("act", lambda e: e.activation(out=g32[k].t[:], in_=ci.t[:], func=AF.Gelu), r=[ci.b], w=[g32[k].b])
            em.do("dve", lambda e: e.tensor_copy(out=gb[k].t[:], in_=g32[k].t[:]), r=[g32[k].b], w=[gb[k].b])
        for m in range(2):
            ps = PF[4 + m % 2]
            for k in range(2):
                em.do("pe", lambda e: e.matmul(ps.t[:], gluw.t[:, k, m * 128:(m + 1) * 128], gb[k].t[:], start=(k == 0), stop=(k == 1)), r=[gluw.b, gb[k].b], w=[ps.b])
            em.do("act", lambda e: e.activation(out=tmpA.t[:], in_=ps.t[:], func=AF.Sigmoid, bias=vecs.t[:, m:m + 1], scale=1.0), r=[ps.b, vecs.b], w=[tmpA.b])
            em.do("dve", lambda e: e.tensor_tensor(out=tmpB.t[:], in0=g32[m].t[:], in1=tmpA.t[:], op=ALU.mult), r=[g32[m].b, tmpA.b], w=[tmpB.b])
            em.do("dve", lambda e: e.tensor_tensor(out=tbf.t[:], in0=tmpB.t[:], in1=tmpB.t[:], op=ALU.mult), r=[tmpB.b], w=[tbf.b])
            em.do("pe", lambda e: e.matmul(ps.t[:], blk16.t[:], tbf.t[:], start=True, stop=True), r=[blk16.b, tbf.b], w=[ps.b])
            rstd_from_psum(ps, rs)
            em.do("dve", lambda e: e.scalar_tensor_tensor(out=mixT.t[:, m, :], in0=tmpB.t[:], scalar=vecs.t[:, 2 + m:3 + m], in1=rs.t[:], op0=ALU.mult, op1=ALU.mult),
                  r=[tmpB.b, vecs.b, rs.b], w=[mixT.b])
        for m in range(4):
            ci = load_chunk("oret", m, ts)
            ps = PF[4 + m % 2]
            em.do("dve", lambda e: e.tensor_copy(out=tbf.t[:], in_=ci.t[:]), r=[ci.b], w=[tbf.b])
            em.do("pe", lambda e: e.matmul(ps.t[:], blk64.t[:], tbf.t[:], start=True, stop=True), r=[blk64.b, tbf.b], w=[ps.b])
            em.do("dve", lambda e: e.tensor_tensor(out=tmpA.t[:], in0=ci.t[:], in1=ps.t[:], op=ALU.subtract), r=[ci.b, ps.b], w=[tmpA.b])
            em.do("dve", lambda e: e.tensor_tensor(out=tbf.t[:], in0=tmpA.t[:], in1=tmpA.t[:], op=ALU.mult), r=[tmpA.b], w=[tbf.b])
            em.do("pe", lambda e: e.matmul(ps.t[:], blk64.t[:], tbf.t[:], start=True, stop=True), r=[blk64.b, tbf.b], w=[ps.b])
            rstd_from_psum(ps, rs)
            em.do("dve", lambda e: e.scalar_tensor_tensor(out=tmpB.t[:], in0=tmpA.t[:], scalar=vecs.t[:, 4 + m:5 + m], in1=rs.t[:], op0=ALU.mult, op1=ALU.mult),
                  r=[tmpA.b, vecs.b, rs.b], w=[tmpB.b])
            for k in range(8):
                em.do("pe", lambda e: e.matmul(ps.t[:], wrg.t[:, k, m * 128:(m + 1) * 128], xTt.t[:, k, :], start=(k == 0), stop=(k == 7)), r=[wrg.b, xTt.b], w=[ps.b])
            em.do("act", lambda e: e.activation(out=sg[0].t[:], in_=ps.t[:], func=AF.Silu), r=[ps.b], w=[sg[0].b])
            em.do("dve", lambda e: e.scalar_tensor_tensor(out=mixT.t[:, 2 + m, :], in0=tmpB.t[:], scalar=vecs.t[:, 8 + m:9 + m], in1=sg[0].t[:], op0=ALU.add, op1=ALU.mult),
                  r=[tmpB.b, vecs.b, sg[0].b], w=[mixT.b])
        for m in range(2):
            ci = load_chunk("osb", m, ts)
            ps = PF[4 + m % 2]
            em.do("dve", lambda e: e.tensor_tensor(out=tbf.t[:], in0=ci.t[:], in1=ci.t[:], op=ALU.mult), r=[ci.b], w=[tbf.b])
            em.do("pe", lambda e: e.matmul(ps.t[:], blk64.t[:], tbf.t[:], start=True, stop=True), r=[blk64.b, tbf.b], w=[ps.b])
            rstd_from_psum(ps, rs)
            em.do("dve", lambda e: e.scalar_tensor_tensor(out=mixT.t[:, 6 + m, :], in0=ci.t[:], scalar=sbw.t[:, m:m + 1], in1=rs.t[:], op0=ALU.mult, op1=ALU.mult),
                  r=[ci.b, sbw.b, rs.b], w=[mixT.b])
        for blk in range(4):
            u = (it * 4 + blk) % 2
            em.dma("sp", s_xtm[u], xtm[u].t[:], x_d[t0 + blk * 128:t0 + (blk + 1) * 128, :], w=[xtm[u].b])
            for half in range(2):
                ps = PF[4 + half]
                hs = slice(half * 512, (half + 1) * 512)
                for k in range(8):
                    em.do("pe", lambda e: e.matmul(ps.t[:], mixT.t[:, k, blk * 128:(blk + 1) * 128], wout.t[:, k, hs], start=(k == 0), stop=(k == 7)),
                          r=[mixT.b, wout.b], w=[ps.b])
                em.do("dve", lambda e: e.scalar_tensor_tensor(out=hbuf.t[:, hs], in0=xtm[u].t[:, hs], scalar=ALPHA, in1=ps.t[:], op0=ALU.mult, op1=ALU.add),
                      r=[xtm[u].b, ps.b], w=[hbuf.b])
            layer_norm_rows(hbuf, lnr[0], lnr[1], X1[blk])
            if kind == "moe":
                transposes_to_bf16(X1[blk], x1T.t[:, :, blk * 128:(blk + 1) * 128], x1T.b, also32=x1T32)
                for k in range(8):
                    em.do("pe", lambda e: e.matmul(PF[4].t[:, 0:8], x1T32.t[:, k, :], wr.t[:, k, :], start=(k == 0), stop=(k == 7)), r=[x1T32.b, wr.b], w=[PF[4].b])
                em.do("dve", lambda e: e.tensor_copy(out=lg.t[:], in_=PF[4].t[:, 0:8]), r=[PF[4].b], w=[lg.b])
                em.do("dve", lambda e: e.max(out=m8.t[:], in_=lg.t[:]), r=[lg.b], w=[m8.b])
                em.do("dve", lambda e: e.tensor_scalar(out=nm1.t[:], in0=m8.t[:, 0:1], scalar1=-1.0, scalar2=None, op0=ALU.mult), r=[m8.b], w=[nm1.b])
                em.do("dve", lambda e: e.tensor_scalar(out=sel.t[:], in0=lg.t[:], scalar1=m8.t[:, 1:2], scalar2=None, op0=ALU.is_ge), r=[lg.b, m8.b], w=[sel.b])
                em.do("act", lambda e: e.activation(out=ex.t[:], in_=lg.t[:], func=AF.Exp, bias=nm1.t[:, 0:1], scale=1.0), r=[lg.b, nm1.b], w=[ex.b])
                em.do("dve", lambda e: e.tensor_tensor(out=ex.t[:], in0=ex.t[:], in1=sel.t[:], op=ALU.mult), r=[ex.b, sel.b], w=[ex.b])
                em.do("dve", lambda e: e.reduce_sum(out=den.t[:], in_=ex.t[:], axis=AX.X), r=[ex.b], w=[den.b])
                em.do("dve", lambda e: e.reciprocal(out=rden.t[:], in_=den.t[:]), r=[den.b], w=[rden.b])
                em.do("dve", lambda e: e.tensor_scalar(out=G[blk].t[:], in0=ex.t[:], scalar1=rden.t[:, 0:1], scalar2=None, op0=ALU.mult), r=[ex.b, rden.b], w=[G[blk].b])
            else:
                transposes_to_bf16(X1[blk], x1T.t[:, :, blk * 128:(blk + 1) * 128], x1T.b)
        if kind == "dense":
            nxt = load_w(wg_d[0], wu_d[0])
            for f in range(NF):
                cur = nxt
                if f + 1 < NF:
                    nxt = load_w(wg_d[f + 1], wu_d[f + 1])
                gate_up(cur[0], cur[1], FFC[f], hT.t[:FFC[f], f, :], hT.b)
            for blk in range(4):
                for half in range(2):
                    ps = PF[4 + half]
                    hs = slice(half * 512, (half + 1) * 512)
                    for f in range(NF):
                        cw = FFC[f]
                        em.do("pe", lambda e: e.matmul(ps.t[:], hT.t[:cw, f, blk * 128:(blk + 1) * 128], wd.t[:cw, f, hs], start=(f == 0), stop=(f == NF - 1)),
                              r=[hT.b, wd.b], w=[ps.b])
                    em.do("dve", lambda e: e.scalar_tensor_tensor(out=hbuf.t[:, hs], in0=X1[blk].t[:, hs], scalar=ALPHA, in1=ps.t[:], op0=ALU.mult, op1=ALU.add),
                          r=[X1[blk].b, ps.b], w=[hbuf.b])
                finish_block(it, blk)
        else:
            for ex_i in range(8):
                wdt = wdb[ex_i % 2]
                em.dma("pool", s_wd[ex_i % 2], wdt.t[:], wd_d[ex_i], w=[wdt.b])
                nxt = load_w(wg_d[ex_i, 0], wu_d[ex_i, 0])
                for f in range(NF):
                    cur = nxt
                    if f + 1 < NF:
                        nxt = load_w(wg_d[ex_i, f + 1], wu_d[ex_i, f + 1])
                    gate_up(cur[0], cur[1], FFC[f], hT.t[:FFC[f], f, :], hT.b)
                for blk in range(4):
                    for half in range(2):
                        ps = PF[4 + half]
                        hs = slice(half * 512, (half + 1) * 512)
                        for f in range(NF):
                            cw = FFC[f]
                            em.do("pe", lambda e: e.matmul(ps.t[:], hT.t[:cw, f, blk * 128:(blk + 1) * 128], wdt.t[:cw, f, hs], start=(f == 0), stop=(f == NF - 1)),
                                  r=[hT.b, wdt.b], w=[ps.b])
                        if ex_i == 0:
                            em.do("dve", lambda e: e.tensor_scalar(out=acc[blk].t[:, hs], in0=ps.t[:], scalar1=G[blk].t[:, 0:1], scalar2=None, op0=ALU.mult),
                                  r=[ps.b, G[blk].b], w=[acc[blk].b])
                        else:
                            em.do("dve", lambda e: e.scalar_tensor_tensor(out=acc[blk].t[:, hs], in0=ps.t[:], scalar=G[blk].t[:, ex_i:ex_i + 1], in1=acc[blk].t[:, hs],
                                                                          op0=ALU.mult, op1=ALU.add), r=[ps.b, G[blk].b, acc[blk].b], w=[acc[blk].b])
            for blk in range(4):
                em.do("dve", lambda e: e.scalar_tensor_tensor(out=hbuf.t[:], in0=X1[blk].t[:], scalar=ALPHA, in1=acc[blk].t[:], op0=ALU.mult, op1=ALU.add),
                      r=[X1[blk].b, acc[blk].b], w=[hbuf.b])
                finish_block(it, blk)

    if io is not None:
        _end_phase(nc, em)
        return nc, em
    em.finish(s_xo + s_xTo)
    em.close()
    return nc, em


def C_consts():
    p = np.arange(128)
    blk16 = ((p[:, None] // 16) == (p[None, :] // 16)).astype(np.float32) / np.float32(16.0)
    blk64 = ((p[:, None] // 64) == (p[None, :] // 64)).astype(np.float32) / np.float32(64.0)
    return dict(blk16=blk16, blk64=blk64, ident=np.eye(128, dtype=np.float32))


def _kchunk(w, ncols):
    K = w.shape[0] // 128
    return np.ascontiguousarray(w.reshape(K, 128, ncols).transpose(1, 0, 2))


def prep_C_weights(layer, inp, consts):
    d = dict(consts)
    d["wrg"] = _kchunk(inp["w_in"][layer][:, 1792:2304], 512)
    d["gluw"] = _kchunk(inp["ssm_glu_w"][layer], 256)
    d["wout"] = _kchunk(inp["w_out"][layer], 1024)
    vecs = np.zeros((128, 12), np.float32)
    vecs[:, 0:2] = inp["ssm_glu_b"][layer].reshape(2, 128).T
    vecs[:, 2:4] = inp["ssm_norm_w"][layer].reshape(2, 128).T
    vecs[:, 4:8] = inp["ret_gn_w"][layer].reshape(4, 128).T
    vecs[:, 8:12] = inp["ret_gn_b"][layer].reshape(4, 128).T
    d["vecs"] = vecs
    d["sbw"] = np.ascontiguousarray(inp["sb_norm_w"][layer].reshape(2, 128).T)
    rows = [inp["ln_mix_w"][layer], inp["ln_mix_b"][layer], inp["ln_ffn_w"][layer], inp["ln_ffn_b"][layer]]
    d["lnrows"] = np.ascontiguousarray(np.stack([np.broadcast_to(r[None, :], (128, 1024)) for r in rows]).astype(np.float32))
    li = layer // 2
    if layer % 2 == 0:
        def padc(w):
            o = np.zeros((1024, 2816), np.float32); o[:, :D_FF] = w; return o
        wgp = padc(inp["ffn_w_gate"][li]); wup = padc(inp["ffn_w_up"][li])
        d["wg"] = np.ascontiguousarray(wgp.reshape(8, 128, 22, 128).transpose(2, 1, 0, 3))
        d["wu"] = np.ascontiguousarray(wup.reshape(8, 128, 22, 128).transpose(2, 1, 0, 3))
        wdp = np.zeros((2816, 1024), np.float32); wdp[:D_FF] = inp["ffn_w_down"][li]
        d["wd"] = np.ascontiguousarray(wdp.reshape(22, 128, 1024).transpose(1, 0, 2))
    else:
        wg = np.zeros((8, 1024, 768), np.float32); wg[:, :, :D_FFE] = inp["moe_w_gate"][li]
        wu = np.zeros((8, 1024, 768), np.float32); wu[:, :, :D_FFE] = inp["moe_w_up"][li]
        d["wg"] = np.ascontiguousarray(wg.reshape(8, 8, 128, 6, 128).transpose(0, 3, 2, 1, 4))
        d["wu"] = np.ascontiguousarray(wu.reshape(8, 8, 128, 6, 128).transpose(0, 3, 2, 1, 4))
        wd = np.zeros((8, 768, 1024), np.float32); wd[:, :D_FFE] = inp["moe_w_down"][li]
        d["wd"] = np.ascontiguousarray(wd.reshape(8, 6, 128, 1024).transpose(0, 2, 1, 3))
        d["wr"] = _kchunk(inp["moe_router"][li], 8)
    return d


class _IO:
    def __init__(self, nc, S):
        self.nc = nc
        self.S = S
        self.TOK = S // 4
        self.layer = 0
        self.t = {}
        self.cur = {}

    def get(self, phase, name):
        return self.cur[(phase, name)]

    def xT_tile(self, i, k):
        r = (i * 512) // self.TOK
        off = i * 512 - r * self.TOK
        return self.t["xT_g"][k * 512 + r * 128:k * 512 + (r + 1) * 128, off:off + 512]

    def mix_out(self, i, row0, nrows):
        q = (i * 512) // self.TOK
        off = i * 512 - q * self.TOK
        return self.t["mix_loc"][q * 256 + row0:q * 256 + row0 + nrows, off:off + 512]

    def mix_in(self, which, idx, ts):
        gq = self.gq
        if which == "oret":
            return [(0, 64, gq[1 * 256 + idx * 64:1 * 256 + idx * 64 + 64, ts]),
                    (64, 64, gq[2 * 256 + idx * 64:2 * 256 + idx * 64 + 64, ts])]
        cb = 0 if which == "yss" else 3
        return [(0, 64, gq[cb * 256 + (2 * idx) * 64:cb * 256 + (2 * idx) * 64 + 64, ts]),
                (64, 64, gq[cb * 256 + (2 * idx + 1) * 64:cb * 256 + (2 * idx + 1) * 64 + 64, ts])]


_B_LAYER = ["wfm", "wtm", "s5p", "bt", "ct", "dmat"]
_B_CONST = ["cosF", "sinF", "cosT", "sinT", "maskT", "qdec", "kdec", "gC", "iota", "m01", "mneg", "ident"]
_C_LAYER = ["wrg", "gluw", "wout", "vecs", "sbw", "lnrows"]
_C_CONST = ["blk16", "blk64", "ident"]


def _collective(nc, kind, src, dst, nchunks):
    R = src.shape[0] // nchunks
    hs = []
    for c in range(nchunks):
        cs = nc.alloc_semaphore(name="cc_%d" % _next_uid())
        hs.append(cs)
        nc.gpsimd.collective_compute(kind, ALU.bypass, replica_groups=[[0, 1, 2, 3], [4, 5, 6, 7]],
                                     ins=[src[c * R:(c + 1) * R, :].opt()], outs=[dst[c * 4 * R:(c + 1) * 4 * R, :].opt()]).then_inc(cs, 1)
        nc.gpsimd.wait_ge(cs, 1)
    nc.all_engine_barrier()
    nc.clear_and_free_semaphores(hs)
    nc.all_engine_barrier()


def _next_uid():
    _UID[0] += 1
    return _UID[0]


def build_fused(S, depth=DEPTH, upto=None):
    TOK = S // 4
    nc = bass.Bass("TRN2", target_bir_lowering=False)
    io = _IO(nc, S)
    ein = lambda name, shape, dt=F32: nc.dram_tensor(name, list(shape), dt, kind="ExternalInput").ap()
    t = io.t
    t["x"] = ein("x", [TOK, 1024])
    shp = dict(wfm=[128, 8, NFM], wtm=[128, 8, NTM], s5p=[128, 2, 3], bt=[128, 2, 2, 128], ct=[128, 2, 2, 64], dmat=[128, 64])
    for n in _B_LAYER:
        t["B_" + n] = ein("B_" + n, [depth] + shp[n])
    cshp = dict(cosF=[128, S], sinF=[128, S], cosT=[S, 128], sinT=[S, 128], maskT=[128, 2, 512], qdec=[128, 512], kdec=[128, 2], gC=[128, 1],
                iota=[128, 513], m01=[128, 128], mneg=[128, 128], ident=[128, 128])
    for n in _B_CONST:
        t["K_" + n] = ein("K_" + n, cshp[n])
    shp = dict(wrg=[128, 8, 512], gluw=[128, 2, 256], wout=[128, 8, 1024], vecs=[128, 12], sbw=[128, 2], lnrows=[4, 128, 1024])
    for n in _C_LAYER:
        t["C_" + n] = ein("C_" + n, [depth] + shp[n])
    t["K_blk16"] = ein("K_blk16", [128, 128]); t["K_blk64"] = ein("K_blk64", [128, 128])
    nd = (depth + 1) // 2
    nm = depth // 2
    t["D_wg"] = ein("D_wg", [nd, 22, 128, 8, 128]); t["D_wu"] = ein("D_wu", [nd, 22, 128, 8, 128]); t["D_wd"] = ein("D_wd", [nd, 128, 22, 1024])
    if nm:
        t["M_wg"] = ein("M_wg", [nm, 8, 6, 128, 8, 128]); t["M_wu"] = ein("M_wu", [nm, 8, 6, 128, 8, 128]); t["M_wd"] = ein("M_wd", [nm, 8, 128, 6, 1024])
        t["M_wr"] = ein("M_wr", [nm, 128, 8, 8])
    t["xo"] = nc.dram_tensor("xo", [TOK, 1024], F32, kind="ExternalOutput").ap()
    t["xT_loc"] = [nc.dram_tensor("xT_loc%d" % i, [1024, TOK], BF16).ap() for i in range(2)]
    t["xT_g"] = nc.dram_tensor("xT_g", [4 * 1024, TOK], BF16).ap()
    t["mix_loc"] = nc.dram_tensor("mix_loc", [1024, TOK], F32).ap()
    t["mix_g"] = nc.dram_tensor("mix_g", [4 * 1024 + 128, TOK], F32).ap()
    t["mix_q"] = nc.dram_tensor("mix_q", [1024, TOK], F32).ap()
    t["xbuf"] = [nc.dram_tensor("xbuf%d" % i, [TOK, 1024], F32).ap() for i in range(2)]

    io.cur = {("P", "x"): t["x"], ("P", "ident"): t["K_ident"], ("P", "xT"): t["xT_loc"][0]}
    build_P(TOK, io=io)
    if upto == "P":
        return nc, []
    _collective(nc, "AllGather", t["xT_loc"][0], t["xT_g"], 8)
    if upto == "AG0":
        return nc, []
    stats = []
    for layer in range(depth):
        io.layer = layer
        cur = {}
        for n in _B_LAYER:
            cur[("B", n)] = t["B_" + n][layer]
        for n in _B_CONST:
            cur[("B", n)] = t["K_" + n]
        io.cur = cur
        _, emB = build_B(S, io=io)
        if upto == "B%d" % layer:
            return nc, []
        _collective(nc, "AllGather", t["mix_loc"], t["mix_g"], 16)
        if upto == "AGm%d" % layer:
            return nc, []
        gs = nc.alloc_semaphore(name="gq_%d" % _next_uid())
        nc.sync.dma_start(out=t["mix_q"], in_=t["mix_g"][bass.DynSlice((nc.partition_id() % 4) * 1024, 1024), :]).then_inc(gs, 16)
        nc.sync.wait_ge(gs, 16)
        nc.all_engine_barrier()
        nc.clear_and_free_semaphores([gs])
        nc.all_engine_barrier()
        io.gq = t["mix_q"]
        if upto == "Q%d" % layer:
            return nc, []
        cur = {}
        for n in _C_LAYER:
            cur[("C", n)] = t["C_" + n][layer]
        for n in _C_CONST:
            cur[("C", n)] = t["K_" + n]
        li = layer // 2
        kind = "dense" if layer % 2 == 0 else "moe"
        pre = "D_" if kind == "dense" else "M_"
        cur[("C", "wg")] = t[pre + "wg"][li]; cur[("C", "wu")] = t[pre + "wu"][li]; cur[("C", "wd")] = t[pre + "wd"][li]
        if kind == "moe":
            cur[("C", "wr")] = t["M_wr"][li]
        cur[("C", "x")] = t["x"] if layer == 0 else t["xbuf"][(layer - 1) % 2]
        cur[("C", "xT")] = t["xT_loc"][layer % 2]
        cur[("C", "xo")] = t["xo"] if layer == depth - 1 else t["xbuf"][layer % 2]
        cur[("C", "xTo")] = t["xT_loc"][(layer + 1) % 2]
        io.cur = cur
        _, emC = build_C(TOK, kind, io=io)
        stats.append((emB.n_inst, emB.n_wait, emC.n_inst, emC.n_wait))
        if upto == "C%d" % layer:
            return nc, []
        if layer + 1 < depth:
            _collective(nc, "AllGather", t["xT_loc"][(layer + 1) % 2], t["xT_g"], 8)
            if upto == "AGx%d" % layer:
                return nc, []
    return nc, stats


_CACHE = {}


def _get(name, fn):
    if name not in _CACHE:
        _CACHE[name] = fn()
    return _CACHE[name]


def fused_inputs(inp, S, depth=DEPTH):
    TOK = S // 4
    bcon = B_consts(S)
    ccon = C_consts()
    x = np.ascontiguousarray(inp["x"], dtype=np.float32)
    shared = {}
    for n in _B_CONST:
        if n not in ("maskT", "qdec", "kdec", "gC"):
            shared["K_" + n] = bcon[n]
    shared["K_blk16"] = ccon["blk16"]; shared["K_blk64"] = ccon["blk64"]
    cw = [prep_C_weights(l, inp, {}) for l in range(depth)]
    for n in _C_LAYER:
        shared["C_" + n] = np.ascontiguousarray(np.stack([cw[l][n] for l in range(depth)]))
    dl = [l for l in range(depth) if l % 2 == 0]
    ml = [l for l in range(depth) if l % 2 == 1]
    for n in ("wg", "wu", "wd"):
        shared["D_" + n] = np.ascontiguousarray(np.stack([cw[l][n] for l in dl]))
        if ml:
            shared["M_" + n] = np.ascontiguousarray(np.stack([cw[l][n] for l in ml]))
    if ml:
        shared["M_wr"] = np.ascontiguousarray(np.stack([cw[l]["wr"] for l in ml]))
    del cw
    perj = []
    for j in range(4):
        bl = [prep_B_inputs(l, j, inp["w_in"], inp["ssm_lam_re"], inp["ssm_lam_im"], inp["ssm_log_dt"], inp["ssm_b_re"], inp["ssm_b_im"],
                            inp["ssm_c_re"], inp["ssm_c_im"], inp["ssm_d"], S, {}) for l in range(depth)]
        d = {}
        for n in _B_LAYER:
            d["B_" + n] = np.ascontiguousarray(np.stack([bl[l][n] for l in range(depth)]))
        for n in ("maskT", "qdec", "kdec", "gC"):
            d["K_" + n] = bl[0][n]
        perj.append(d)
    maps = []
    for core in range(8):
        b, j = core // 4, core % 4
        d = dict(shared)
        d.update(perj[j])
        d["x"] = np.ascontiguousarray(x[b, j * TOK:(j + 1) * TOK])
        maps.append(d)
    return maps


def kernel(**inputs):
    inp = {k: np.asarray(v) for k, v in inputs.items()}
    S = inp["x"].shape[1]
    TOK = S // 4
    nc = _get(("F", S), lambda: build_fused(S)[0])
    maps = fused_inputs(inp, S)
    res = run_bass_kernel_spmd(nc, maps, core_ids=list(range(8))).results
    out = np.stack([np.concatenate([res[b * 4 + c]["xo"] for c in range(4)], axis=0) for b in range(2)])
    return np.ascontiguousarray(out, dtype=np.float32)
```

```python
import numpy as np
import ml_dtypes
import concourse.bass as bass
import concourse.mybir as mybir
from concourse.bass_utils import run_bass_kernel_spmd
from contextlib import ExitStack

F32 = mybir.dt.float32
BF16 = mybir.dt.bfloat16
AF = mybir.ActivationFunctionType
ALU = mybir.AluOpType
AX = mybir.AxisListType

D_MODEL = 1024
BATCH = 2
SEQ = 16384
DEPTH = 4
D_FF = 2752
D_FFE = 688
N_EXP = 8
ALPHA = (2.0 * DEPTH) ** 0.25
LN_EPS = 1e-5
MAGIC = 12582912.0
TWO_PI = float(2 * np.pi)


class Buf:
    __slots__ = ("w", "r")

    def __init__(self):
        self.w = None
        self.r = {}


class Sem:
    __slots__ = ("h", "val", "key")

    def __init__(self, h, key):
        self.h = h
        self.val = 0
        self.key = key


class Eng:
    def __init__(self, name, obj, sem):
        self.name = name
        self.obj = obj
        self.sem = sem
        self.waited = {}


_UID = [0]


class Emitter:
    def __init__(self, nc):
        self.nc = nc
        self.es = ExitStack()
        self.sems = {}
        self.nsem = 0
        self.handles = []
        self.engs = {}
        for name, obj in (("pe", nc.tensor), ("act", nc.scalar), ("dve", nc.vector),
                          ("pool", nc.gpsimd), ("sp", nc.sync)):
            self.engs[name] = Eng(name, obj, self.new_sem("e_" + name))
        self.n_inst = 0
        self.n_wait = 0
        self.nt = 0

    def new_sem(self, name="s"):
        _UID[0] += 1
        h = self.nc.alloc_semaphore(name=name + "_%d_%d" % (self.nsem, _UID[0]))
        self.handles.append(h)
        s = Sem(h, self.nsem)
        self.sems[s.key] = s
        self.nsem += 1
        return s

    def sbuf(self, shape, dtype, name=None):
        _UID[0] += 1
        return self.es.enter_context(self.nc.sbuf_tensor((name or "t") + "_%d" % _UID[0], list(shape), dtype))

    def psum(self, shape, dtype=F32, name=None):
        _UID[0] += 1
        return self.es.enter_context(self.nc.psum_tensor((name or "p") + "_%d" % _UID[0], list(shape), dtype))

    def _wait(self, E, deps):
        best = {}
        for (k, v) in deps:
            if v > best.get(k, 0):
                best[k] = v
        for k, v in best.items():
            if E.waited.get(k, 0) >= v:
                continue
            if E.name == "pe" and k == E.sem.key:
                continue
            E.obj.wait_ge(self.sems[k].h, v)
            E.waited[k] = v
            self.n_wait += 1

    @staticmethod
    def _deps(r, w):
        deps = []
        for b in r:
            if b.w is not None:
                deps.append(b.w)
        for b in w:
            if b.w is not None:
                deps.append(b.w)
            deps.extend(b.r.items())
        return deps

    @staticmethod
    def _mark(d, r, w):
        for b in r:
            if d[1] > b.r.get(d[0], 0):
                b.r[d[0]] = d[1]
        for b in w:
            b.w = d
            b.r = {}

    def do(self, eng, fn, r=(), w=()):
        E = self.engs[eng]
        self._wait(E, self._deps(r, w))
        ins = fn(E.obj)
        E.sem.val += 1
        ins.then_inc(E.sem.h, 1)
        self._mark((E.sem.key, E.sem.val), r, w)
        self.n_inst += 1
        return ins

    def dma(self, q, sem, out, in_, r=(), w=()):
        E = self.engs[q]
        self._wait(E, self._deps(r, w))
        ins = E.obj.dma_start(out=out, in_=in_)
        sem.val += 16
        ins.then_inc(sem.h, 16)
        self._mark((sem.key, sem.val), r, w)
        self.n_inst += 1
        return ins

    def dma_multi(self, q, sem, pairs, r=(), w=()):
        E = self.engs[q]
        self._wait(E, self._deps(r, w))
        for (out, in_) in pairs:
            ins = E.obj.dma_start(out=out, in_=in_)
            sem.val += 16
            ins.then_inc(sem.h, 16)
            self.n_inst += 1
        self._mark((sem.key, sem.val), r, w)

    def group_mark(self, sem, ts):
        for t in ts:
            t.b.w = (sem.key, sem.val)

    def finish(self, sems):
        E = self.engs["sp"]
        for s in sems:
            if s.val > 0:
                E.obj.wait_ge(s.h, s.val)

    def finish_all(self):
        E = self.engs["sp"]
        for s in self.sems.values():
            if s.val > 0:
                E.obj.wait_ge(s.h, s.val)

    def close(self):
        self.es.close()


class T:
    __slots__ = ("t", "b")

    def __init__(self, t):
        self.t = t
        self.b = Buf()


NFM = 704
NTM = 448


class _Stop(Exception):
    pass


def _end_phase(nc, em):
    em.finish_all()
    nc.all_engine_barrier()
    nc.clear_and_free_semaphores(em.handles)
    em.close()
    nc.all_engine_barrier()


def build_B(S, phases=('s5', 'ret', 'sb'), stop=None, io=None):
    try:
        return _build_B(S, phases, stop, io)
    except _Stop as e:
        em = e.args[0]
        em.finish_all()
        em.close()
        return em.nc, em


def _build_B(S, phases, stop, io):
    NT = S // 512
    nc = io.nc if io is not None else bass.Bass("TRN2", target_bir_lowering=False)

    def din(name, shape, dt=F32):
        if io is not None:
            return io.get("B", name)
        return nc.dram_tensor(name, list(shape), dt, kind="ExternalInput").ap()

    def dout(name, shape, dt=F32):
        if io is not None:
            return None
        return nc.dram_tensor(name, list(shape), dt, kind="ExternalOutput").ap()

    xT = din("xT", [1024, S], BF16) if io is None else None
    wfm_d = din("wfm", [128, 8, NFM])
    wtm_d = din("wtm", [128, 8, NTM])
    cosF_d = din("cosF", [128, S])
    sinF_d = din("sinF", [128, S])
    cosT_d = din("cosT", [S, 128])
    sinT_d = din("sinT", [S, 128])
    maskT_d = din("maskT", [128, 2, 512])
    qdec_d = din("qdec", [128, 512])
    kdec_d = din("kdec", [128, 2])
    gC_d = din("gC", [128, 1])
    s5p_d = din("s5p", [128, 2, 3])
    bt_d = din("bt", [128, 2, 2, 128])
    ct_d = din("ct", [128, 2, 2, 64])
    dm_d = din("dmat", [128, 64])
    iota_d = din("iota", [128, 513])
    m01_d = din("m01", [128, 128])
    mneg_d = din("mneg", [128, 128])
    ident_d = din("ident", [128, 128])

    yssm_o = dout("yssmT", [64, S])
    oret_o = dout("oretT", [128, S])
    osb_o = dout("osbT", [64, S])

    em = Emitter(nc)

    def CP(tag):
        if stop == tag:
            raise _Stop(em)
    sb = lambda shape, dt=F32: T(em.sbuf(shape, dt))
    ldw = em.new_sem("ldw"); ldh = em.new_sem("ldh")
    st_y = em.new_sem("sty"); st_r = [em.new_sem("str0"), em.new_sem("str1")]; st_s = em.new_sem("sts")

    wfm = sb([128, 8, NFM], BF16)
    wtm = sb([128, 8, NTM], BF16)
    for k in range(8):
        em.dma("pool", ldw, wfm.t[:, k, :], wfm_d[:, k, :])
        em.dma("pool", ldw, wtm.t[:, k, :], wtm_d[:, k, :])
    maskT = sb([128, 2, 512]); em.dma("sp", ldh, maskT.t[:], maskT_d)
    qdec = sb([128, 512]); em.dma("sp", ldh, qdec.t[:], qdec_d)
    kdec = sb([128, 2]); em.dma("sp", ldh, kdec.t[:], kdec_d)
    gC = sb([128, 1]); em.dma("sp", ldh, gC.t[:], gC_d)
    s5p = sb([128, 2, 3]); em.dma("sp", ldh, s5p.t[:], s5p_d)
    bt = sb([128, 2, 2, 128], BF16); em.dma("pool", ldw, bt.t[64:128], bt_d[64:128])
    ct = sb([128, 2, 2, 64], BF16); em.dma("pool", ldw, ct.t[:], ct_d)
    dmat = sb([128, 64], BF16); em.dma("pool", ldw, dmat.t[64:128], dm_d[64:128])
    iota = sb([128, 513]); em.dma("sp", ldh, iota.t[:], iota_d)
    m01 = sb([128, 128]); em.dma("sp", ldh, m01.t[:], m01_d)
    mneg = sb([128, 128]); em.dma("sp", ldh, mneg.t[:], mneg_d)
    ident = sb([128, 128], BF16); em.dma("pool", ldw, ident.t[:], ident_d)
    em.group_mark(ldw, [wfm, wtm, bt, ct, dmat, ident])
    em.group_mark(ldh, [maskT, qdec, kdec, gC, s5p, iota, m01, mneg])
    ones = sb([128, 513]); em.do("dve", lambda e: e.memset(ones.t[:], 1.0), w=[ones.b])

    CP("c1")
    m = [sb([128, 513]) for _ in range(4)]
    wre = sb([128, 513]); wim = sb([128, 513]); xr = sb([128, 513]); xi = sb([128, 513])
    sc = lambda: sb([128, 2])
    dtt = sc(); em.do("act", lambda e: e.activation(out=dtt.t[:], in_=s5p.t[:, :, 2], func=AF.Exp), r=[s5p.b], w=[dtt.b])
    aa = sc(); em.do("dve", lambda e: e.tensor_tensor(out=aa.t[:], in0=s5p.t[:, :, 0], in1=dtt.t[:], op=ALU.mult), r=[s5p.b, dtt.b], w=[aa.b])
    th = sc(); em.do("dve", lambda e: e.tensor_tensor(out=th.t[:], in0=s5p.t[:, :, 1], in1=dtt.t[:], op=ALU.mult), r=[s5p.b, dtt.b], w=[th.b])
    rr = sc(); em.do("act", lambda e: e.activation(out=rr.t[:], in_=aa.t[:], func=AF.Exp), r=[aa.b], w=[rr.b])

    CP("c2")

    def sincos(out_sin, out_cos, ang, shape):
        n = shape[1]
        A = lambda x_: ang.t[:, 0:n] if x_ is ang else x_.t[:, 0:n]
        for (o, shift) in ((out_sin, 0.0), (out_cos, 0.25)):
            t = m[0]; k = m[1]
            em.do("dve", lambda e: e.tensor_scalar(out=t.t[:, 0:n], in0=ang.t[:, 0:n], scalar1=1.0 / TWO_PI, scalar2=shift, op0=ALU.mult, op1=ALU.add), r=[ang.b], w=[t.b])
            em.do("dve", lambda e: e.tensor_scalar(out=k.t[:, 0:n], in0=t.t[:, 0:n], scalar1=MAGIC, scalar2=None, op0=ALU.add), r=[t.b], w=[k.b])
            em.do("dve", lambda e: e.tensor_scalar(out=k.t[:, 0:n], in0=k.t[:, 0:n], scalar1=MAGIC, scalar2=None, op0=ALU.subtract), r=[k.b], w=[k.b])
            em.do("dve", lambda e: e.tensor_tensor(out=t.t[:, 0:n], in0=t.t[:, 0:n], in1=k.t[:, 0:n], op=ALU.subtract), r=[t.b, k.b], w=[t.b])
            em.do("dve", lambda e: e.tensor_scalar(out=t.t[:, 0:n], in0=t.t[:, 0:n], scalar1=TWO_PI, scalar2=3.14159, op0=ALU.mult, op1=ALU.min), r=[t.b], w=[t.b])
            em.do("dve", lambda e: e.tensor_scalar(out=t.t[:, 0:n], in0=t.t[:, 0:n], scalar1=-3.14159, scalar2=None, op0=ALU.max), r=[t.b], w=[t.b])
            em.do("act", lambda e: e.activation(out=o.t[:, 0:n], in_=t.t[:, 0:n], func=AF.Sin), r=[t.b], w=[o.b])

    sn = sc(); cs_ = sc()
    sincos(sn, cs_, th, [128, 2])

    CP("c3")

    def tt(out, a, b, op, eng="dve"):
        em.do(eng, lambda e: e.tensor_tensor(out=out.t[:], in0=a.t[:], in1=b.t[:], op=op), r=[a.b, b.b], w=[out.b])

    nr = sc(); tt(nr, rr, cs_, ALU.mult)
    em.do("dve", lambda e: e.tensor_scalar(out=nr.t[:], in0=nr.t[:], scalar1=-1.0, scalar2=None, op0=ALU.add), r=[nr.b], w=[nr.b])
    ni = sc(); tt(ni, rr, sn, ALU.mult)
    lre = sc(); em.do("dve", lambda e: e.tensor_copy(out=lre.t[:], in_=s5p.t[:, :, 0]), r=[s5p.b], w=[lre.b])
    lim = sc(); em.do("dve", lambda e: e.tensor_copy(out=lim.t[:], in_=s5p.t[:, :, 1]), r=[s5p.b], w=[lim.b])
    den = sc(); t0 = sc()
    tt(den, lre, lre, ALU.mult); tt(t0, lim, lim, ALU.mult); tt(den, den, t0, ALU.add)
    rden = sc(); em.do("dve", lambda e: e.reciprocal(out=rden.t[:], in_=den.t[:]), r=[den.b], w=[rden.b])
    cre = sc(); cim = sc(); t1 = sc()
    tt(cre, nr, lre, ALU.mult); tt(t1, ni, lim, ALU.mult); tt(cre, cre, t1, ALU.add); tt(cre, cre, rden, ALU.mult)
    tt(cim, ni, lre, ALU.mult); tt(t1, nr, lim, ALU.mult); tt(cim, cim, t1, ALU.subtract); tt(cim, cim, rden, ALU.mult)

    CP("c4")
    T1re = [None, None]; T1im = [None, None]; T2re = [None, None]; T2im = [None, None]; Rfull = [None, None]
    Ere = sc(); Eim = sc()
    for rb in range(2):
        ang = m[2]
        em.do("dve", lambda e: e.tensor_scalar(out=ang.t[:], in0=iota.t[:], scalar1=th.t[:, rb:rb + 1], scalar2=None, op0=ALU.mult), r=[iota.b, th.b], w=[ang.b])
        sT = sb([128, 513]); cT = sb([128, 513])
        sincos(sT, cT, ang, [128, 513])
        T2re[rb] = cT; T2im[rb] = sT
        em.do("dve", lambda e: e.tensor_copy(out=Ere.t[:, rb:rb + 1], in_=cT.t[:, 512:513]), r=[cT.b], w=[Ere.b])
        em.do("dve", lambda e: e.tensor_copy(out=Eim.t[:, rb:rb + 1], in_=sT.t[:, 512:513]), r=[sT.b], w=[Eim.b])
        a1 = m[3]; a2 = wre; t1r = sb([128, 512]); t1i = sb([128, 512])
        em.do("dve", lambda e: e.tensor_scalar(out=a1.t[:, 0:512], in0=cT.t[:, 0:512], scalar1=cre.t[:, rb:rb + 1], scalar2=None, op0=ALU.mult), r=[cT.b, cre.b], w=[a1.b])
        em.do("dve", lambda e: e.scalar_tensor_tensor(out=t1r.t[:], in0=sT.t[:, 0:512], scalar=cim.t[:, rb:rb + 1], in1=a1.t[:, 0:512], op0=ALU.mult, op1=ALU.add), r=[sT.b, cim.b, a1.b], w=[t1r.b])
        em.do("dve", lambda e: e.tensor_scalar(out=a2.t[:, 0:512], in0=sT.t[:, 0:512], scalar1=cre.t[:, rb:rb + 1], scalar2=None, op0=ALU.mult), r=[sT.b, cre.b], w=[a2.b])
        em.do("dve", lambda e: e.scalar_tensor_tensor(out=t1i.t[:], in0=cT.t[:, 0:512], scalar=cim.t[:, rb:rb + 1], in1=a2.t[:, 0:512], op0=ALU.mult, op1=ALU.subtract), r=[cT.b, cim.b, a2.b], w=[t1i.b])
        T1re[rb] = t1r; T1im[rb] = t1i
        rf = sb([128, 512])
        em.do("dve", lambda e: e.tensor_scalar(out=rf.t[:], in0=ones.t[:, 0:512], scalar1=rr.t[:, rb:rb + 1], scalar2=None, op0=ALU.mult), r=[ones.b, rr.b], w=[rf.b])
        Rfull[rb] = rf
    ctn = sb([128, 2, 64], BF16)
    em.do("dve", lambda e: e.tensor_scalar(out=ctn.t[:], in0=ct.t[:, 1, :, :], scalar1=-1.0, scalar2=None, op0=ALU.mult), r=[ct.b], w=[ctn.b])
    car = [[sb([128, 1]) for _ in range(2)] for _ in range(2)]
    for rb in range(2):
        for c in range(2):
            em.do("dve", lambda e: e.memset(car[rb][c].t[:], 0.0), w=[car[rb][c].b])

    CP("c5")
    KT = em.sbuf([64, S], BF16)
    KTb = [Buf() for _ in range(NT)]
    VR = em.sbuf([128, S // 128, 64], BF16)
    VRb = [Buf() for _ in range(NT)]
    Sall = [sb([128, 5, 64]) for _ in range(2)]
    em.do("dve", lambda e: e.memset(Sall[0].t[:], 0.0), w=[Sall[0].b])
    em.do("dve", lambda e: e.memset(Sall[1].t[:], 0.0), w=[Sall[1].b])

    PB = [T(em.psum([128, 512])) for _ in range(7)]
    PT = T(em.psum([128, 1024], BF16))
    PTb = [Buf(), Buf()]

    xt = [sb([128, 8, 512], BF16) for _ in range(2)]
    xsem = [[em.new_sem("x") for _ in range(5)] for _ in range(2)]
    xsem[1][1:] = xsem[0][1:]
    _cF = sb([128, 512]); _sF = sb([128, 512]); _cTm = sb([128, 4, 128]); _sTm = sb([128, 4, 128])
    cF = [_cF, _cF]; sF = [_sF, _sF]; cTm = [_cTm, _cTm]; sTm = [_sTm, _sTm]
    FU = [sb([128, 512], BF16) for _ in range(2)]
    qh = sb([128, 512], BF16); qtl = sb([128, 512], BF16); kh = sb([128, 512], BF16)
    qf = sb([128, 512])
    ktm = sb([128, 4, 128], BF16); vtm = sb([128, 4, 128], BF16); vdm = sb([128, 4, 128], BF16)
    g1 = sb([128, 128]); g2 = sb([128, 128])
    sTs = [sb([128, 512], BF16) for _ in range(2)]
    Sbf = sb([128, 4, 64], BF16)
    oret_t = [sb([64, 512]) for _ in range(2)]
    yss_t = sb([64, 512])
    osb_t = sb([64, 512])
    Xre = sb([128, 512], BF16); Xim = sb([128, 512], BF16)
    tA = sb([128, 1]); tB = sb([128, 1])
    NBUF = 2
    e_t = [sb([128, 512]) for _ in range(NBUF)]
    sp_t = [sb([128, 513]) for _ in range(NBUF)]
    cs_t = [sb([128, 513]) for _ in range(NBUF)]
    t_t = [sb([128, 512]) for _ in range(NBUF)]
    A_t = [sb([128, 512], BF16) for _ in range(NBUF)]
    AT_t = [sb([128, 512], BF16) for _ in range(NBUF)]
    for i in range(NBUF):
        em.do("dve", lambda e: e.memset(sp_t[i].t[:], 0.0), w=[sp_t[i].b])
    negR = [sb([128, 1]) for _ in range(4)]

    def load_tile(i):
        s = i % 2
        if io is None:
            em.dma("sp", xsem[s][0], xt[s].t[:], xT[:, i * 512:(i + 1) * 512].rearrange("(k p) t -> p k t", p=128), w=[xt[s].b])
        else:
            em.dma_multi("sp", xsem[s][0], [(xt[s].t[:, k, :], io.xT_tile(i, k)) for k in range(8)], w=[xt[s].b])

    def load_tabs(i):
        s = i % 2
        em.dma("sp", xsem[s][1], cF[s].t[:], cosF_d[:, i * 512:(i + 1) * 512], w=[cF[s].b])
        em.dma("sp", xsem[s][2], sF[s].t[:], sinF_d[:, i * 512:(i + 1) * 512], w=[sF[s].b])
        em.dma("sp", xsem[s][3], cTm[s].t[:], cosT_d[i * 512:(i + 1) * 512, :].rearrange("(c p) d -> p c d", p=128), w=[cTm[s].b])
        em.dma("sp", xsem[s][4], sTm[s].t[:], sinT_d[i * 512:(i + 1) * 512, :].rearrange("(c p) d -> p c d", p=128), w=[sTm[s].b])

    load_tile(0)
    load_tabs(0)
    sbit = 0
    for i in (range(NT) if 'noloop' not in phases else ()):
        s = i % 2
        if i + 1 < NT:
            load_tile(i + 1)
        X = xt[s]
        fu = FU[s]

        def fm_group(pb, c0, M):
            for k in range(8):
                em.do("pe", lambda e: e.matmul(pb.t[:M, :], wfm.t[:, k, c0:c0 + M], X.t[:, k, :], start=(k == 0), stop=(k == 7)),
                      r=[wfm.b, X.b], w=[pb.b])

        def rope_fm(pa, pb_, outf):
            em.do("dve", lambda e: e.tensor_tensor(out=m[0].t[:, 0:512], in0=pa.t[:], in1=cF[s].t[:], op=ALU.mult), r=[pa.b, cF[s].b], w=[m[0].b])
            em.do("dve", lambda e: e.tensor_tensor(out=m[1].t[:, 0:512], in0=pb_.t[:], in1=sF[s].t[:], op=ALU.mult), r=[pb_.b, sF[s].b], w=[m[1].b])
            em.do("pool", lambda e: e.tensor_tensor(out=outf.t[:], in0=m[0].t[:, 0:512], in1=m[1].t[:, 0:512], op=ALU.add), r=[m[0].b, m[1].b], w=[outf.b])

        fm_group(PB[0], 0, 128); fm_group(PB[1], 128, 128)
        rope_fm(PB[0], PB[1], qf)
        em.do("act", lambda e: e.activation(out=qh.t[:], in_=qf.t[:], func=AF.Copy), r=[qf.b], w=[qh.b])
        em.do("pool", lambda e: e.tensor_tensor(out=qtl.t[:], in0=qf.t[:], in1=qdec.t[:], op=ALU.mult), r=[qf.b, qdec.b], w=[qtl.b])
        fm_group(PB[0], 256, 128); fm_group(PB[1], 384, 128)
        rope_fm(PB[0], PB[1], kh)
        fm_group(PB[0], 512, 128)
        em.do("act", lambda e: e.activation(out=fu.t[:], in_=PB[0].t[:], func=AF.Copy), r=[PB[0].b], w=[fu.b])
        fm_group(PB[1], 640, 64)
        em.do("act", lambda e: e.activation(out=KT[:, i * 512:(i + 1) * 512], in_=PB[1].t[:64, :], func=AF.Copy), r=[PB[1].b], w=[KTb[i]])
        CP("p1")
        for blk in range(4):
            pb = PB[2 + (blk % 2)]
            for k in range(8):
                em.do("pe", lambda e: e.matmul(pb.t[:, :NTM], X.t[:, k, blk * 128:(blk + 1) * 128], wtm.t[:, k, :], start=(k == 0), stop=(k == 7)),
                      r=[wtm.b, X.b], w=[pb.b])
            CP("q1")
            em.do("dve", lambda e: e.tensor_tensor(out=g1.t[:], in0=pb.t[:, 0:128], in1=cTm[s].t[:, blk, :], op=ALU.mult), r=[pb.b, cTm[s].b], w=[g1.b])
            em.do("dve", lambda e: e.tensor_tensor(out=g2.t[:], in0=pb.t[:, 128:256], in1=sTm[s].t[:, blk, :], op=ALU.mult), r=[pb.b, sTm[s].b], w=[g2.b])
            em.do("pool", lambda e: e.tensor_tensor(out=ktm.t[:, blk, :], in0=g1.t[:], in1=g2.t[:], op=ALU.add), r=[g1.b, g2.b], w=[ktm.b])
            CP("q2")
            em.do("dve", lambda e: e.tensor_copy(out=vtm.t[:, blk, :], in_=pb.t[:, 256:384]), r=[pb.b], w=[vtm.b])
            CP("q3")
            for h in range(2):
                em.do("dve", lambda e: e.tensor_scalar(out=vdm.t[:, blk, h * 64:(h + 1) * 64], in0=pb.t[:, 256 + h * 64:256 + (h + 1) * 64],
                                                       scalar1=kdec.t[:, h:h + 1], scalar2=None, op0=ALU.mult), r=[pb.b, kdec.b], w=[vdm.b])
            CP("q4")
            em.do("dve", lambda e: e.tensor_copy(out=VR[:, i * 4 + blk, :], in_=pb.t[:, 384:448]), r=[pb.b], w=[VRb[i]])

        if i + 1 < NT:
            load_tabs(i + 1)
        CP("p2")
        for rb in (range(2) if 's5' in phases else ()):
            em.do("pe", lambda e: e.matmul(PB[0].t[:, :], bt.t[64:128, 0, rb, :], fu.t[64:128, :], start=True, stop=True), r=[bt.b, fu.b], w=[PB[0].b])
            em.do("pe", lambda e: e.matmul(PB[1].t[:, :], bt.t[64:128, 1, rb, :], fu.t[64:128, :], start=True, stop=True), r=[bt.b, fu.b], w=[PB[1].b])
            pr, pi = PB[0], PB[1]
            mm_ = lambda o, a, b_: em.do("dve", lambda e: e.tensor_tensor(out=o.t[:, 0:512], in0=a.t[:, 0:512], in1=b_.t[:, 0:512], op=ALU.mult), r=[a.b, b_.b], w=[o.b])
            tt5 = lambda o, a, b_, op, eng: em.do(eng, lambda e: e.tensor_tensor(out=o.t[:, 0:512], in0=a.t[:, 0:512], in1=b_.t[:, 0:512], op=op), r=[a.b, b_.b], w=[o.b])
            mm_(m[0], pr, T1re[rb]); mm_(m[1], pi, T1im[rb]); mm_(m[2], pi, T1re[rb]); mm_(m[3], pr, T1im[rb])
            tt5(wre, m[0], m[1], ALU.subtract, "pool"); tt5(wim, m[2], m[3], ALU.add, "pool")
            for (xo, wi, c) in ((xr, wre, 0), (xi, wim, 1)):
                em.do("dve", lambda e: e.tensor_tensor_scan(out=xo.t[:, 0:512], data0=Rfull[rb].t[:], data1=wi.t[:, 0:512], initial=car[rb][c].t[:, 0:1],
                                                            op0=ALU.mult, op1=ALU.add), r=[Rfull[rb].b, wi.b, car[rb][c].b], w=[xo.b])
            em.do("dve", lambda e: e.tensor_tensor(out=tA.t[:], in0=xi.t[:, 511:512], in1=Eim.t[:, rb:rb + 1], op=ALU.mult), r=[xi.b, Eim.b], w=[tA.b])
            em.do("dve", lambda e: e.scalar_tensor_tensor(out=car[rb][0].t[:], in0=xr.t[:, 511:512], scalar=Ere.t[:, rb:rb + 1], in1=tA.t[:], op0=ALU.mult, op1=ALU.subtract),
                  r=[xr.b, Ere.b, tA.b], w=[car[rb][0].b])
            em.do("dve", lambda e: e.tensor_tensor(out=tB.t[:], in0=xr.t[:, 511:512], in1=Eim.t[:, rb:rb + 1], op=ALU.mult), r=[xr.b, Eim.b], w=[tB.b])
            em.do("dve", lambda e: e.scalar_tensor_tensor(out=car[rb][1].t[:], in0=xi.t[:, 511:512], scalar=Ere.t[:, rb:rb + 1], in1=tB.t[:], op0=ALU.mult, op1=ALU.add),
                  r=[xi.b, Ere.b, tB.b], w=[car[rb][1].b])
            pm = lambda o, a, b_: em.do("pool", lambda e: e.tensor_tensor(out=o.t[:, 0:512], in0=a.t[:, 0:512], in1=b_.t[:, 0:512], op=ALU.mult), r=[a.b, b_.b], w=[o.b])
            pm(m[0], xr, T2re[rb]); pm(m[1], xi, T2im[rb]); mm_(m[2], xi, T2re[rb]); mm_(m[3], xr, T2im[rb])
            tt5(Xre, m[0], m[1], ALU.subtract, "pool"); tt5(Xim, m[2], m[3], ALU.add, "dve")
            em.do("pe", lambda e: e.matmul(PB[3].t[:64, :], ct.t[:, 0, rb, :], Xre.t[:], start=(rb == 0), stop=False), r=[ct.b, Xre.b], w=[PB[3].b])
            em.do("pe", lambda e: e.matmul(PB[3].t[:64, :], ctn.t[:, rb, :], Xim.t[:], start=False, stop=False), r=[ctn.b, Xim.b], w=[PB[3].b])
        if 's5' in phases:
            em.do("pe", lambda e: e.matmul(PB[3].t[:64, :], dmat.t[64:128, :], fu.t[64:128, :], start=False, stop=True), r=[dmat.b, fu.b], w=[PB[3].b])
            em.do("act", lambda e: e.activation(out=yss_t.t[:], in_=PB[3].t[:64, :], func=AF.Copy), r=[PB[3].b], w=[yss_t.b])
            em.dma("sp", st_y, yssm_o[:, i * 512:(i + 1) * 512] if io is None else io.mix_out(i, 0, 64), yss_t.t[:], r=[yss_t.b])

        CP("p3")
        for _once in ([0] if 'ret' in phases else []):
            Sc = Sall[i % 2]; Sn = Sall[(i + 1) % 2]
            for c in range(4):
                em.do("pe", lambda e: e.matmul(PB[5].t[:, c * 128:(c + 1) * 128], ktm.t[:, c, :], vdm.t[:, c, :], start=True, stop=True), r=[ktm.b, vdm.b], w=[PB[5].b])
            for c in range(4):
                for h in range(2):
                    P = slice(h * 64, (h + 1) * 64)
                    em.do("dve", lambda e: e.scalar_tensor_tensor(out=Sc.t[P, c + 1, :], in0=Sc.t[P, c, :], scalar=gC.t[P, 0:1],
                                                                  in1=PB[5].t[P, c * 128 + h * 64:c * 128 + (h + 1) * 64], op0=ALU.mult, op1=ALU.add),
                          r=[Sc.b, gC.b, PB[5].b], w=[Sc.b])
            em.do("act", lambda e: e.activation(out=Sbf.t[:], in_=Sc.t[:, 0:4, :], func=AF.Copy), r=[Sc.b], w=[Sbf.b])
            em.do("dve", lambda e: e.tensor_copy(out=Sn.t[:, 0, :], in_=Sc.t[:, 4, :]), r=[Sc.b], w=[Sn.b])
            for h in range(2):
                P = slice(h * 64, (h + 1) * 64)
                for c in range(4):
                    em.do("pe", lambda e: e.matmul(PB[4].t[:, c * 128:(c + 1) * 128], kh.t[P, c * 128:(c + 1) * 128], qh.t[P, c * 128:(c + 1) * 128], start=True, stop=True),
                          r=[kh.b, qh.b], w=[PB[4].b])
                em.do("dve", lambda e: e.tensor_tensor(out=sTs[h].t[:], in0=PB[4].t[:], in1=maskT.t[:, h, :], op=ALU.mult), r=[PB[4].b, maskT.b], w=[sTs[h].b])
                for c in range(4):
                    cs = slice(c * 128, (c + 1) * 128)
                    em.do("pe", lambda e: e.matmul(PB[6].t[:64, cs], vtm.t[:, c, h * 64:(h + 1) * 64], sTs[h].t[:, cs], start=True, stop=False),
                          r=[vtm.b, sTs[h].b], w=[PB[6].b])
                    em.do("pe", lambda e: e.matmul(PB[6].t[:64, cs], Sbf.t[P, c, :], qtl.t[P, cs], start=False, stop=True),
                          r=[Sbf.b, qtl.b], w=[PB[6].b])
                em.do("act", lambda e: e.activation(out=oret_t[h].t[:], in_=PB[6].t[:64, :], func=AF.Copy), r=[PB[6].b], w=[oret_t[h].b])
                em.dma("sp", st_r[h], oret_o[h * 64:(h + 1) * 64, i * 512:(i + 1) * 512] if io is None else io.mix_out(i, 64 + h * 64, 64), oret_t[h].t[:], r=[oret_t[h].b])

        CP("p4")
        for _once in ([0] if 'sb' in phases else []):
            its = []
            for qb in range(4):
                nmm = sum(((qb + 1) if kt == i else 4) for kt in range(i + 1))
                imm = 0
                for kt in range(i, -1, -1):
                    nblk = (qb + 1) if kt == i else 4
                    its.append(dict(qb=qb, kt=kt, nblk=nblk, nco=nblk * 128, imm0=imm, nmm=nmm, first=(kt == i), u=sbit % NBUF))
                    sbit += 1
                    imm += nblk

            def S1(d):
                qb, kt, nco, u = d["qb"], d["kt"], d["nco"], d["u"]
                zb = PB[u]
                E_, SP = e_t[u], sp_t[u]
                qs = slice(qb * 128, (qb + 1) * 128)
                if d["first"]:
                    em.do("pool", lambda e: e.memset(negR[qb].t[:], 0.0), w=[negR[qb].b])
                em.do("pe", lambda e: e.matmul(zb.t[:, :nco], fu.t[0:64, qs], KT[:, kt * 512:kt * 512 + nco], start=True, stop=True),
                      r=[fu.b, KTb[kt]], w=[zb.b])
                em.do("act", lambda e: e.activation(out=E_.t[:, :nco], in_=zb.t[:, :nco], func=AF.Exp, scale=0.125), r=[zb.b], w=[E_.b])
                em.do("act", lambda e: e.activation(out=SP.t[:, 1:1 + nco], in_=E_.t[:, :nco], func=AF.Ln, bias=1.0, scale=1.0), r=[E_.b], w=[SP.b])
                if d["first"]:
                    em.do("pool", lambda e: e.tensor_tensor(out=SP.t[:, 1 + nco - 128:1 + nco], in0=SP.t[:, 1 + nco - 128:1 + nco], in1=m01.t[:], op=ALU.mult),
                          r=[SP.b, m01.b], w=[SP.b])

            def S2a(d):
                qb, nco, u = d["qb"], d["nco"], d["u"]
                zb = PB[u]
                SP, CS, TT, A_ = sp_t[u], cs_t[u], t_t[u], A_t[u]
                nR = negR[qb]
                em.do("dve", lambda e: e.tensor_tensor_scan(out=CS.t[:, 0:nco + 1], data0=ones.t[:, 0:nco + 1], data1=SP.t[:, 0:nco + 1], initial=0.0,
                                                            op0=ALU.mult, op1=ALU.add), r=[ones.b, SP.b], w=[CS.b])
                em.do("dve", lambda e: e.tensor_tensor(out=nR.t[:], in0=nR.t[:], in1=CS.t[:, nco:nco + 1], op=ALU.subtract), r=[nR.b, CS.b], w=[nR.b])
                em.do("dve", lambda e: e.scalar_tensor_tensor(out=TT.t[:, :nco], in0=zb.t[:, :nco], scalar=0.125, in1=CS.t[:, 0:nco], op0=ALU.mult, op1=ALU.add),
                      r=[zb.b, CS.b], w=[TT.b])
                if d["first"]:
                    em.do("pool", lambda e: e.tensor_tensor(out=TT.t[:, nco - 128:nco], in0=TT.t[:, nco - 128:nco], in1=mneg.t[:], op=ALU.add),
                          r=[TT.b, mneg.b], w=[TT.b])
                em.do("act", lambda e: e.activation(out=A_.t[:, :nco], in_=TT.t[:, :nco], func=AF.Exp, bias=nR.t[:, 0:1], scale=1.0), r=[TT.b, nR.b], w=[A_.b])

            def S2b(d):
                qb, kt, nco, nblk, u = d["qb"], d["kt"], d["nco"], d["nblk"], d["u"]
                A_, AT = A_t[u], AT_t[u]
                qs = slice(qb * 128, (qb + 1) * 128)
                po = u * 512
                for blk in range(nblk):
                    bs = slice(blk * 128, (blk + 1) * 128)
                    em.do("pe", lambda e: e.transpose(PT.t[:, po + blk * 128:po + (blk + 1) * 128], A_.t[:, bs], ident.t[:]), r=[A_.b, ident.b], w=[PTb[u]])
                em.do("dve", lambda e: e.tensor_copy(out=AT.t[:, :nco], in_=PT.t[:, po:po + nco]), r=[PTb[u]], w=[AT.b])
                for blk in range(nblk):
                    bs = slice(blk * 128, (blk + 1) * 128)
                    im = d["imm0"] + blk
                    em.do("pe", lambda e: e.matmul(PB[2].t[:64, qs], VR[:, kt * 4 + blk, :], AT.t[:, bs], start=(im == 0), stop=(im == d["nmm"] - 1)),
                          r=[VRb[kt], AT.b], w=[PB[2].b])

            N = len(its)
            for n in range(N + 2):
                if n < N:
                    S1(its[n])
                if 1 <= n <= N:
                    S2a(its[n - 1])
                if n >= 2:
                    S2b(its[n - 2])
            em.do("act", lambda e: e.activation(out=osb_t.t[:], in_=PB[2].t[:64, :], func=AF.Copy), r=[PB[2].b], w=[osb_t.b])
            em.dma("sp", st_s, osb_o[:, i * 512:(i + 1) * 512] if io is None else io.mix_out(i, 192, 64), osb_t.t[:], r=[osb_t.b])

    if io is not None:
        _end_phase(nc, em)
        return nc, em
    em.finish([st_y, st_r[0], st_r[1], st_s])
    em.close()
    return nc, em


def _rope_tables(S):
    half = 32
    inv = (np.float32(10000.0) ** (-np.arange(half, dtype=np.float32) / np.float32(half))).astype(np.float32)
    pos = np.arange(S, dtype=np.float32)
    ang = (pos[:, None] * inv[None, :]).astype(np.float32)
    c = np.cos(ang).astype(np.float32)
    s = np.sin(ang).astype(np.float32)
    cos64 = np.concatenate([c, c], axis=1)
    sin64 = np.concatenate([-s, s], axis=1)
    cosT = np.concatenate([cos64, cos64], axis=1)
    sinT = np.concatenate([sin64, sin64], axis=1)
    return np.ascontiguousarray(cosT.T), np.ascontiguousarray(sinT.T), np.ascontiguousarray(cosT), np.ascontiguousarray(sinT)


def _ret_consts(j):
    C = 128
    idx = np.arange(C, dtype=np.float32)
    maskT = np.zeros((128, 2, 512), np.float32)
    qdec = np.zeros((128, 512), np.float32)
    kdec = np.zeros((128, 2), np.float32)
    gC = np.zeros((128, 1), np.float32)
    for hl in range(2):
        h = 2 * j + hl
        lg = np.log(np.float32(1.0) - np.float32(2.0) ** np.float32(-5.0 - h)).astype(np.float32)
        rel = idx[:, None] - idx[None, :]
        dm = np.where(rel >= 0, np.exp(lg * np.maximum(rel, 0.0)), 0.0).astype(np.float32)
        mT = (dm.T * np.float32(0.125)).astype(np.float32)
        maskT[:, hl, :] = np.tile(mT, (1, 4))
        qd = np.exp(lg * (idx + 1.0)).astype(np.float32)
        qdec[hl * 64:(hl + 1) * 64, :] = np.tile(qd[None, :], (64, 4))
        kdec[:, hl] = np.exp(lg * (C - 1.0 - idx)).astype(np.float32) * np.float32(0.125)
        gC[hl * 64:(hl + 1) * 64, 0] = np.exp(lg * np.float32(C))
    return maskT, qdec, kdec, gC


def _swap(cols):
    cols = np.asarray(cols).reshape(-1, 64)
    return np.concatenate([cols[:, 32:], cols[:, :32]], axis=1).reshape(-1)


def prep_B_inputs(layer, j, w_in, lam_re, lam_im, log_dt, b_re, b_im, c_re, c_im, d_skip, S, consts):
    u_cols = np.arange(64 * j, 64 * j + 64)
    rq = 256 + np.arange(128 * j, 128 * j + 128)
    rk = 768 + np.arange(128 * j, 128 * j + 128)
    rv = 1280 + np.arange(128 * j, 128 * j + 128)
    sq = 2304 + np.arange(64 * j, 64 * j + 64)
    sk = 2560 + np.arange(64 * j, 64 * j + 64)
    sv = 2816 + np.arange(64 * j, 64 * j + 64)
    fm_cols = np.concatenate([rq, _swap(rq), rk, _swap(rk), sq, u_cols, sk])
    tm_cols = np.concatenate([rk, _swap(rk), rv, sv])
    W = w_in[layer]
    wfm = np.ascontiguousarray(W[:, fm_cols].reshape(8, 128, NFM).transpose(1, 0, 2))
    wtm = np.ascontiguousarray(W[:, tm_cols].reshape(8, 128, NTM).transpose(1, 0, 2))
    G0 = 4 * j
    s5p = np.zeros((128, 2, 3), np.float32)
    bt = np.zeros((128, 2, 2, 128), np.float32)
    ct = np.zeros((128, 2, 2, 64), np.float32)
    for rb in range(2):
        for gl in range(2):
            g = G0 + 2 * rb + gl
            ps = slice(gl * 64, (gl + 1) * 64)
            s5p[ps, rb, 0] = lam_re[layer, g]
            s5p[ps, rb, 1] = lam_im[layer, g]
            s5p[ps, rb, 2] = log_dt[layer, g]
            chl = (2 * rb + gl) * 16
            bt[64 + chl:64 + chl + 16, 0, rb, ps] = b_re[layer, g].T
            bt[64 + chl:64 + chl + 16, 1, rb, ps] = b_im[layer, g].T
            ct[ps, 0, rb, chl:chl + 16] = c_re[layer, g].T
            ct[ps, 1, rb, chl:chl + 16] = c_im[layer, g].T
    dmat = np.zeros((128, 64), np.float32)
    dmat[64 + np.arange(64), np.arange(64)] = d_skip[layer, 64 * j:64 * j + 64]
    maskT, qdec, kdec, gC = _ret_consts(j)
    d = dict(wfm=wfm, wtm=wtm, s5p=s5p, bt=bt, ct=ct, dmat=dmat, maskT=maskT, qdec=qdec, kdec=kdec, gC=gC)
    d.update(consts)
    return d


def B_consts(S):
    cosF, sinF, cosT, sinT = _rope_tables(S)
    iota = np.tile(np.arange(513, dtype=np.float32)[None, :], (128, 1))
    qi = np.arange(128)
    m01 = (qi[None, :] < qi[:, None]).astype(np.float32)
    mneg = np.where(m01 > 0, 0.0, -30000.0).astype(np.float32)
    ident = np.eye(128, dtype=np.float32)
    return dict(cosF=cosF, sinF=sinF, cosT=cosT, sinT=sinT, iota=iota, m01=m01, mneg=mneg, ident=ident)


def build_P(TOK, io=None):
    if io is not None:
        nc = io.nc
        x_d = io.get("P", "x"); id_d = io.get("P", "ident"); xT_o = io.get("P", "xT")
    else:
        nc = bass.Bass("TRN2", target_bir_lowering=False)
        x_d = nc.dram_tensor("x", [TOK, 1024], F32, kind="ExternalInput").ap()
        id_d = nc.dram_tensor("ident", [128, 128], F32, kind="ExternalInput").ap()
        xT_o = nc.dram_tensor("xT", [1024, TOK], BF16, kind="ExternalOutput").ap()
    em = Emitter(nc)
    sb = lambda shape, dt=F32: T(em.sbuf(shape, dt))
    ldh = em.new_sem("ldh")
    ident = sb([128, 128]); em.dma("sp", ldh, ident.t[:], id_d); em.group_mark(ldh, [ident])
    xin = [sb([128, 1024]) for _ in range(2)]
    xs = [em.new_sem("x") for _ in range(2)]
    PTt = [T(em.psum([128, 1024])) for _ in range(2)]
    xo = [sb([128, 8, 128], BF16) for _ in range(2)]
    so = [em.new_sem("o") for _ in range(2)]
    nb = TOK // 128
    em.dma("sp", xs[0], xin[0].t[:], x_d[0:128, :], w=[xin[0].b])
    for blk in range(nb):
        s = blk % 2
        if blk + 1 < nb:
            em.dma("sp", xs[1 - s], xin[1 - s].t[:], x_d[(blk + 1) * 128:(blk + 2) * 128, :], w=[xin[1 - s].b])
        for k in range(8):
            em.do("pe", lambda e: e.transpose(PTt[s].t[:, k * 128:(k + 1) * 128], xin[s].t[:, k * 128:(k + 1) * 128], ident.t[:]),
                  r=[xin[s].b, ident.b], w=[PTt[s].b])
        em.do("dve" if s == 0 else "act",
              (lambda e: e.tensor_copy(out=xo[s].t[:], in_=PTt[s].t[:].rearrange("p (k t) -> p k t", k=8))) if s == 0 else
              (lambda e: e.activation(out=xo[s].t[:], in_=PTt[s].t[:].rearrange("p (k t) -> p k t", k=8), func=AF.Copy)),
              r=[PTt[s].b], w=[xo[s].b])
        em.dma("sp", so[s], xT_o[:, blk * 128:(blk + 1) * 128].rearrange("(k p) t -> p k t", p=128), xo[s].t[:], r=[xo[s].b])
    if io is not None:
        _end_phase(nc, em)
        return nc, em
    em.finish(so)
    em.close()
    return nc, em


FFC_DENSE = [128] * 21 + [64]
FFC_EXP = [128] * 5 + [48]


def build_C(TOK, kind, io=None):
    NTL = TOK // 512
    nc = io.nc if io is not None else bass.Bass("TRN2", target_bir_lowering=False)

    def din(name, shape, dt=F32):
        if io is not None:
            return io.get("C", name)
        return nc.dram_tensor(name, list(shape), dt, kind="ExternalInput").ap()

    def dout(name, shape, dt=F32):
        if io is not None:
            return io.get("C", name)
        return nc.dram_tensor(name, list(shape), dt, kind="ExternalOutput").ap()

    x_d = din("x", [TOK, 1024])
    xT_d = din("xT", [1024, TOK], BF16)
    if io is None:
        yss_d = din("yssmT", [256, TOK]); oret_d = din("oretT", [512, TOK]); osb_d = din("osbT", [256, TOK])
    wrg_d = din("wrg", [128, 8, 512]); gluw_d = din("gluw", [128, 2, 256]); wout_d = din("wout", [128, 8, 1024])
    vec_d = din("vecs", [128, 12])
    sbw_d = din("sbw", [128, 2])
    lnr_d = din("lnrows", [4, 128, 1024])
    blk16_d = din("blk16", [128, 128]); blk64_d = din("blk64", [128, 128]); id_d = din("ident", [128, 128])
    if kind == "dense":
        NF = 22; FFC = FFC_DENSE
        wg_d = din("wg", [NF, 128, 8, 128]); wu_d = din("wu", [NF, 128, 8, 128]); wd_d = din("wd", [128, NF, 1024])
    else:
        NF = 6; FFC = FFC_EXP
        wg_d = din("wg", [8, NF, 128, 8, 128]); wu_d = din("wu", [8, NF, 128, 8, 128]); wd_d = din("wd", [8, 128, NF, 1024])
        wr_d = din("wr", [128, 8, 8])
    x_o = dout("xo", [TOK, 1024])
    xT_o = dout("xTo", [1024, TOK], BF16)

    em = Emitter(nc)
    sb = lambda shape, dt=F32: T(em.sbuf(shape, dt))
    ldw = em.new_sem("ldw"); ldh = em.new_sem("ldh")
    wrg = sb([128, 8, 512], BF16); gluw = sb([128, 2, 256], BF16); wout = sb([128, 8, 1024], BF16)
    for k in range(8):
        em.dma("pool", ldw, wrg.t[:, k, :], wrg_d[:, k, :])
        em.dma("pool", ldw, wout.t[:, k, :], wout_d[:, k, :])
    em.dma("pool", ldw, gluw.t[:], gluw_d)
    blk16 = sb([128, 128], BF16); em.dma("pool", ldw, blk16.t[:], blk16_d)
    blk64 = sb([128, 128], BF16); em.dma("pool", ldw, blk64.t[:], blk64_d)
    gl = [wrg, gluw, wout, blk16, blk64]
    if kind == "dense":
        wd = sb([128, NF, 1024], BF16)
        for f in range(NF):
            em.dma("pool", ldw, wd.t[:, f, :], wd_d[:, f, :])
        gl.append(wd)
    em.group_mark(ldw, gl)
    vecs = sb([128, 12]); em.dma("sp", ldh, vecs.t[:], vec_d)
    sbw = sb([128, 2]); em.dma("sp", ldh, sbw.t[:], sbw_d)
    lnr = [sb([128, 1024]) for _ in range(4)]
    for q in range(4):
        em.dma("sp", ldh, lnr[q].t[:], lnr_d[q])
    ident = sb([128, 128]); em.dma("sp", ldh, ident.t[:], id_d)
    gh = [vecs, sbw, ident] + lnr
    if kind == "moe":
        wr = sb([128, 8, 8]); em.dma("sp", ldh, wr.t[:], wr_d); gh.append(wr)
    em.group_mark(ldh, gh)

    PF = [T(em.psum([128, 512])) for _ in range(6)]
    PTt = T(em.psum([128, 1024]))

    xTt = sb([128, 8, 512], BF16); s_xT = em.new_sem("xT")
    cin = [sb([128, 512]) for _ in range(3)]; s_cin = [em.new_sem("ci") for _ in range(3)]
    xtm = [sb([128, 1024]) for _ in range(2)]; s_xtm = [em.new_sem("xm") for _ in range(2)]
    NWB = 2
    wgb = [sb([128, 8, 128], BF16) for _ in range(NWB)]; wub = [sb([128, 8, 128], BF16) for _ in range(NWB)]
    s_wg = [em.new_sem("wg") for _ in range(NWB)]; s_wu = [em.new_sem("wu") for _ in range(NWB)]
    mixT = sb([128, 8, 512], BF16)
    X1 = [sb([128, 1024]) for _ in range(4)]
    x1T = sb([128, 8, 512], BF16)
    hT = sb([128, NF, 512], BF16)
    g32 = [sb([128, 512]) for _ in range(2)]; gb = [sb([128, 512], BF16) for _ in range(2)]
    tmpA = sb([128, 512]); tmpB = sb([128, 512]); tbf = sb([128, 512], BF16)
    rs = sb([128, 512])
    hbuf = sb([128, 1024])
    stats = sb([128, 2, 6]); mv = sb([128, 2]); sd1 = sb([128, 1]); rs1 = sb([128, 1])
    xo_t = [sb([128, 1024]) for _ in range(2)]; s_xo = [em.new_sem("xo") for _ in range(2)]
    xTo_t = [sb([128, 8, 128], BF16) for _ in range(2)]; s_xTo = [em.new_sem("xTo") for _ in range(2)]
    sg = [sb([128, 512]) for _ in range(2)]
    cin_i = [0]
    if kind == "moe":
        wdb = [sb([128, NF, 1024], BF16) for _ in range(2)]; s_wd = [em.new_sem("wd") for _ in range(2)]
        acc = [sb([128, 1024]) for _ in range(4)]
        x1T32 = sb([128, 8, 128])
        lg = sb([128, 8]); m8 = sb([128, 8]); nm1 = sb([128, 1]); sel = sb([128, 8]); ex = sb([128, 8]); den = sb([128, 1]); rden = sb([128, 1])
        G = [sb([128, 8]) for _ in range(4)]

    def load_chunk(which, idx, ts):
        u = cin_i[0] % 3
        cin_i[0] += 1
        if io is None:
            src = {"yss": yss_d, "oret": oret_d, "osb": osb_d}[which]
            em.dma("sp", s_cin[u], cin[u].t[:], src[idx * 128:(idx + 1) * 128, ts], w=[cin[u].b])
        else:
            em.dma_multi("sp", s_cin[u], [(cin[u].t[p0:p0 + np_, :], ap) for (p0, np_, ap) in io.mix_in(which, idx, ts)], w=[cin[u].b])
        return cin[u]

    def rstd_from_psum(ps, dst_rs):
        em.do("act", lambda e: e.activation(out=dst_rs.t[:], in_=ps.t[:], func=AF.Sqrt, bias=LN_EPS, scale=1.0), r=[ps.b], w=[dst_rs.b])
        em.do("dve", lambda e: e.reciprocal(out=dst_rs.t[:], in_=dst_rs.t[:]), r=[dst_rs.b], w=[dst_rs.b])

    def layer_norm_rows(h, wrow, brow, out):
        for c in range(2):
            em.do("dve", lambda e: e.bn_stats(out=stats.t[:, c, :], in_=h.t[:, c * 512:(c + 1) * 512]), r=[h.b], w=[stats.b])
        em.do("dve", lambda e: e.bn_aggr(out=mv.t[:], in_=stats.t[:]), r=[stats.b], w=[mv.b])
        em.do("act", lambda e: e.activation(out=sd1.t[:], in_=mv.t[:, 1:2], func=AF.Sqrt, bias=LN_EPS, scale=1.0), r=[mv.b], w=[sd1.b])
        em.do("dve", lambda e: e.reciprocal(out=rs1.t[:], in_=sd1.t[:]), r=[sd1.b], w=[rs1.b])
        em.do("dve", lambda e: e.tensor_scalar(out=h.t[:], in0=h.t[:], scalar1=mv.t[:, 0:1], scalar2=rs1.t[:, 0:1], op0=ALU.subtract, op1=ALU.mult),
              r=[h.b, mv.b, rs1.b], w=[h.b])
        em.do("dve", lambda e: e.tensor_tensor(out=h.t[:], in0=h.t[:], in1=wrow.t[:], op=ALU.mult), r=[h.b, wrow.b], w=[h.b])
        em.do("dve", lambda e: e.tensor_tensor(out=out.t[:], in0=h.t[:], in1=brow.t[:], op=ALU.add), r=[h.b, brow.b], w=[out.b])

    nwl = [0]

    def load_w(src_g, src_u):
        u = nwl[0] % NWB
        nwl[0] += 1
        em.dma("pool", s_wg[u], wgb[u].t[:], src_g, w=[wgb[u].b])
        em.dma("pool", s_wu[u], wub[u].t[:], src_u, w=[wub[u].b])
        return wgb[u], wub[u]

    gu_i = [0]

    def gate_up(wgt, wut, cw, dst_ap, dst_buf):
        p = (gu_i[0] % 2) * 2
        gu_i[0] += 1
        pg, pu = PF[p], PF[p + 1]
        sgt = sg[(gu_i[0]) % 2]
        for k in range(8):
            em.do("pe", lambda e: e.matmul(pg.t[:cw, :], wgt.t[:, k, :cw], x1T.t[:, k, :], start=(k == 0), stop=(k == 7)), r=[wgt.b, x1T.b], w=[pg.b])
        for k in range(8):
            em.do("pe", lambda e: e.matmul(pu.t[:cw, :], wut.t[:, k, :cw], x1T.t[:, k, :], start=(k == 0), stop=(k == 7)), r=[wut.b, x1T.b], w=[pu.b])
        em.do("act", lambda e: e.activation(out=sgt.t[:cw, :], in_=pg.t[:cw, :], func=AF.Silu), r=[pg.b], w=[sgt.b])
        em.do("dve", lambda e: e.tensor_tensor(out=dst_ap, in0=sgt.t[:cw, :], in1=pu.t[:cw, :], op=ALU.mult), r=[sgt.b, pu.b], w=[dst_buf])

    def transposes_to_bf16(src, dst_ap, dst_buf, also32=None):
        for k in range(8):
            em.do("pe", lambda e: e.transpose(PTt.t[:, k * 128:(k + 1) * 128], src.t[:, k * 128:(k + 1) * 128], ident.t[:]), r=[src.b, ident.b], w=[PTt.b])
        em.do("act", lambda e: e.activation(out=dst_ap, in_=PTt.t[:].rearrange("p (k t) -> p k t", k=8), func=AF.Copy), r=[PTt.b], w=[dst_buf])
        if also32 is not None:
            em.do("act", lambda e: e.activation(out=also32.t[:], in_=PTt.t[:].rearrange("p (k t) -> p k t", k=8), func=AF.Copy), r=[PTt.b], w=[also32.b])

    def finish_block(it, blk):
        u = (it * 4 + blk) % 2
        tok0 = it * 512 + blk * 128
        layer_norm_rows(hbuf, lnr[2], lnr[3], xo_t[u])
        em.dma("sp", s_xo[u], x_o[tok0:tok0 + 128, :], xo_t[u].t[:], r=[xo_t[u].b])
        transposes_to_bf16(xo_t[u], xTo_t[u].t[:], xTo_t[u].b)
        em.dma("sp", s_xTo[u], xT_o[:, tok0:tok0 + 128].rearrange("(k p) t -> p k t", p=128), xTo_t[u].t[:], r=[xTo_t[u].b])

    for it in range(NTL):
        t0 = it * 512
        ts = slice(t0, t0 + 512)
        em.dma("sp", s_xT, xTt.t[:], xT_d[:, ts].rearrange("(k p) t -> p k t", p=128), w=[xTt.b])
        for k in range(2):
            ci = load_chunk("yss", k, ts)
            em.do("act", lambda e: e.activation(out=g32[k].t[:], in_=ci.t[:], func=AF.Gelu), r=[ci.b], w=[g32[k].b])
            em.do("dve", lambda e: e.tensor_copy(out=gb[k].t[:], in_=g32[k].t[:]), r=[g32[k].b], w=[gb[k].b])
        for m in range(2):
            ps = PF[4 + m % 2]
            for k in range(2):
                em.do("pe", lambda e: e.matmul(ps.t[:], gluw.t[:, k, m * 128:(m + 1) * 128], gb[k].t[:], start=(k == 0), stop=(k == 1)), r=[gluw.b, gb[k].b], w=[ps.b])
            em.do("act", lambda e: e.activation(out=tmpA.t[:], in_=ps.t[:], func=AF.Sigmoid, bias=vecs.t[:, m:m + 1], scale=1.0), r=[ps.b, vecs.b], w=[tmpA.b])
            em.do("dve", lambda e: e.tensor_tensor(out=tmpB.t[:], in0=g32[m].t[:], in1=tmpA.t[:], op=ALU.mult), r=[g32[m].b, tmpA.b], w=[tmpB.b])
            em.do("dve", lambda e: e.tensor_tensor(out=tbf.t[:], in0=tmpB.t[:], in1=tmpB.t[:], op=ALU.mult), r=[tmpB.b], w=[tbf.b])
            em.do("pe", lambda e: e.matmul(ps.t[:], blk16.t[:], tbf.t[:], start=True, stop=True), r=[blk16.b, tbf.b], w=[ps.b])
            rstd_from_psum(ps, rs)
            em.do("dve", lambda e: e.scalar_tensor_tensor(out=mixT.t[:, m, :], in0=tmpB.t[:], scalar=vecs.t[:, 2 + m:3 + m], in1=rs.t[:], op0=ALU.mult, op1=ALU.mult),
                  r=[tmpB.b, vecs.b, rs.b], w=[mixT.b])
        for m in range(4):
            ci = load_chunk("oret", m, ts)
            ps = PF[4 + m % 2]
            em.do("dve", lambda e: e.tensor_copy(out=tbf.t[:], in_=ci.t[:]), r=[ci.b], w=[tbf.b])
            em.do("pe", lambda e: e.matmul(ps.t[:], blk64.t[:], tbf.t[:], start=True, stop=True), r=[blk64.b, tbf.b], w=[ps.b])
            em.do("dve", lambda e: e.tensor_tensor(out=tmpA.t[:], in0=ci.t[:], in1=ps.t[:], op=ALU.subtract), r=[ci.b, ps.b], w=[tmpA.b])
            em.do("dve", lambda e: e.tensor_tensor(out=tbf.t[:], in0=tmpA.t[:], in1=tmpA.t[:], op=ALU.mult), r=[tmpA.b], w=[tbf.b])
            em.do("pe", lambda e: e.matmul(ps.t[:], blk64.t[:], tbf.t[:], start=True, stop=True), r=[blk64.b, tbf.b], w=[ps.b])
            rstd_from_psum(ps, rs)
            em.do("dve", lambda e: e.scalar_tensor_tensor(out=tmpB.t[:], in0=tmpA.t[:], scalar=vecs.t[:, 4 + m:5 + m], in1=rs.t[:], op0=ALU.mult, op1=ALU.mult),
                  r=[tmpA.b, vecs.b, rs.b], w=[tmpB.b])
            for k in range(8):
                em.do("pe", lambda e: e.matmul(ps.t[:], wrg.t[:, k, m * 128:(m + 1) * 128], xTt.t[:, k, :], start=(k == 0), stop=(k == 7)), r=[wrg.b, xTt.b], w=[ps.b])
            em.do("act", lambda e: e.activation(out=sg[0].t[:], in_=ps.t[:], func=AF.Silu), r=[ps.b], w=[sg[0].b])
            em.do("dve", lambda e: e.scalar_tensor_tensor(out=mixT.t[:, 2 + m, :], in0=tmpB.t[:], scalar=vecs.t[:, 8 + m:9 + m], in1=sg[0].t[:], op0=ALU.add, op1=ALU.mult),
                  r=[tmpB.b, vecs.b, sg[0].b], w=[mixT.b])
        for m in range(2):
            ci = load_chunk("osb", m, ts)
            ps = PF[4 + m % 2]
            em.do("dve", lambda e: e.tensor_tensor(out=tbf.t[:], in0=ci.t[:], in1=ci.t[:], op=ALU.mult), r=[ci.b], w=[tbf.b])
            em.do("pe", lambda e: e.matmul(ps.t[:], blk64.t[:], tbf.t[:], start=True, stop=True), r=[blk64.b, tbf.b], w=[ps.b])
            rstd_from_psum(ps, rs)
            em.do("dve", lambda e: e.scalar_tensor_tensor(out=mixT.t[:, 6 + m, :], in0=ci.t[:], scalar=sbw.t[:, m:m + 1], in1=rs.t[:], op0=ALU.mult, op1=ALU.mult),
                  r=[ci.b, sbw.b, rs.b], w=[mixT.b])
        for blk in range(4):
            u = (it * 4 + blk) % 2
            em.dma("sp", s_xtm[u], xtm[u].t[:], x_d[t0 + blk * 128:t0 + (blk + 1) * 128, :], w=[xtm[u].b])
            for half in range(2):
                ps = PF[4 + half]
                hs = slice(half * 512, (half + 1) * 512)
                for k in range(8):
                    em.do("pe", lambda e: e.matmul(ps.t[:], mixT.t[:, k, blk * 128:(blk + 1) * 128], wout.t[:, k, hs], start=(k == 0), stop=(k == 7)),
                          r=[mixT.b, wout.b], w=[ps.b])
                em.do("dve", lambda e: e.scalar_tensor_tensor(out=hbuf.t[:, hs], in0=xtm[u].t[:, hs], scalar=ALPHA, in1=ps.t[:], op0=ALU.mult, op1=ALU.add),
                      r=[xtm[u].b, ps.b], w=[hbuf.b])
            layer_norm_rows(hbuf, lnr[0], lnr[1], X1[blk])
            if kind == "moe":
                transposes_to_bf16(X1[blk], x1T.t[:, :, blk * 128:(blk + 1) * 128], x1T.b, also32=x1T32)
                for k in range(8):
                    em.do("pe", lambda e: e.matmul(PF[4].t[:, 0:8], x1T32.t[:, k, :], wr.t[:, k, :], start=(k == 0), stop=(k == 7)), r=[x1T32.b, wr.b], w=[PF[4].b])
                em.do("dve", lambda e: e.tensor_copy(out=lg.t[:], in_=PF[4].t[:, 0:8]), r=[PF[4].b], w=[lg.b])
                em.do("dve", lambda e: e.max(out=m8.t[:], in_=lg.t[:]), r=[lg.b], w=[m8.b])
                em.do("dve", lambda e: e.tensor_scalar(out=nm1.t[:], in0=m8.t[:, 0:1], scalar1=-1.0, scalar2=None, op0=ALU.mult), r=[m8.b], w=[nm1.b])
                em.do("dve", lambda e: e.tensor_scalar(out=sel.t[:], in0=lg.t[:], scalar1=m8.t[:, 1:2], scalar2=None, op0=ALU.is_ge), r=[lg.b, m8.b], w=[sel.b])
                em.do("act", lambda e: e.activation(out=ex.t[:], in_=lg.t[:], func=AF.Exp, bias=nm1.t[:, 0:1], scale=1.0), r=[lg.b, nm1.b], w=[ex.b])
                em.do("dve", lambda e: e.tensor_tensor(out=ex.t[:], in0=ex.t[:], in1=sel.t[:], op=ALU.mult), r=[ex.b, sel.b], w=[ex.b])
                em.do("dve", lambda e: e.reduce_sum(out=den.t[:], in_=ex.t[:], axis=AX.X), r=[ex.b], w=[den.b])
                em.do("dve", lambda e: e.reciprocal(out=rden.t[:], in_=den.t[:]), r=[den.b], w=[rden.b])
                em.do("dve", lambda e: e.tensor_scalar(out=G[blk].t[:], in0=ex.t[:], scalar1=rden.t[:, 0:1], scalar2=None, op0=ALU.mult), r=[ex.b, rden.b], w=[G[blk].b])
            else:
                transposes_to_bf16(X1[blk], x1T.t[:, :, blk * 128:(blk + 1) * 128], x1T.b)
        if kind == "dense":
            nxt = load_w(wg_d[0], wu_d[0])
            for f in range(NF):
                cur = nxt
                if f + 1 < NF:
                    nxt = load_w(wg_d[f + 1], wu_d[f + 1])
                gate_up(cur[0], cur[1], FFC[f], hT.t[:FFC[f], f, :], hT.b)
            for blk in range(4):
                for half in range(2):
                    ps = PF[4 + half]
                    hs = slice(half * 512, (half + 1) * 512)
                    for f in range(NF):
                        cw = FFC[f]
                        em.do("pe", lambda e: e.matmul(ps.t[:], hT.t[:cw, f, blk * 128:(blk + 1) * 128], wd.t[:cw, f, hs], start=(f == 0), stop=(f == NF - 1)),
                              r=[hT.b, wd.b], w=[ps.b])
                    em.do("dve", lambda e: e.scalar_tensor_tensor(out=hbuf.t[:, hs], in0=X1[blk].t[:, hs], scalar=ALPHA, in1=ps.t[:], op0=ALU.mult, op1=ALU.add),
                          r=[X1[blk].b, ps.b], w=[hbuf.b])
                finish_block(it, blk)
        else:
            for ex_i in range(8):
                wdt = wdb[ex_i % 2]
                em.dma("pool", s_wd[ex_i % 2], wdt.t[:], wd_d[ex_i], w=[wdt.b])
                nxt = load_w(wg_d[ex_i, 0], wu_d[ex_i, 0])
                for f in range(NF):
                    cur = nxt
                    if f + 1 < NF:
                        nxt = load_w(wg_d[ex_i, f + 1], wu_d[ex_i, f + 1])
                    gate_up(cur[0], cur[1], FFC[f], hT.t[:FFC[f], f, :], hT.b)
                for blk in range(4):
                    for half in range(2):
                        ps = PF[4 + half]
                        hs = slice(half * 512, (half + 1) * 512)
                        for f in range(NF):
                            cw = FFC[f]
                            em.do("pe", lambda e: e.matmul(ps.t[:], hT.t[:cw, f, blk * 128:(blk + 1) * 128], wdt.t[:cw, f, hs], start=(f == 0), stop=(f == NF - 1)),
                                  r=[hT.b, wdt.b], w=[ps.b])
                        if ex_i == 0:
                            em.do("dve", lambda e: e.tensor_scalar(out=acc[blk].t[:, hs], in0=ps.t[:], scalar1=G[blk].t[:, 0:1], scalar2=None, op0=ALU.mult),
                                  r=[ps.b, G[blk].b], w=[acc[blk].b])
                        else:
                            em.do("dve", lambda e: e.scalar_tensor_tensor(out=acc[blk].t[:, hs], in0=ps.t[:], scalar=G[blk].t[:, ex_i:ex_i + 1], in1=acc[blk].t[:, hs],
                                                                          op0=ALU.mult, op1=ALU.add), r=[ps.b, G[blk].b, acc[blk].b], w=[acc[blk].b])
            for blk in range(4):
                em.do("dve", lambda e: e.scalar_tensor_tensor(out=hbuf.t[:], in0=X1[blk].t[:], scalar=ALPHA, in1=acc[blk].t[:], op0=ALU.mult, op1=ALU.add),
                      r=[X1[blk].b, acc[blk].b], w=[hbuf.b])
                finish_block(it, blk)

    if io is not None:
        _end_phase(nc, em)
        return nc, em
    em.finish(s_xo + s_xTo)
    em.close()
    return nc, em


def C_consts():
    p = np.arange(128)
    blk16 = ((p[:, None] // 16) == (p[None, :] // 16)).astype(np.float32) / np.float32(16.0)
    blk64 = ((p[:, None] // 64) == (p[None, :] // 64)).astype(np.float32) / np.float32(64.0)
    return dict(blk16=blk16, blk64=blk64, ident=np.eye(128, dtype=np.float32))


def _kchunk(w, ncols):
    K = w.shape[0] // 128
    return np.ascontiguousarray(w.reshape(K, 128, ncols).transpose(1, 0, 2))


def prep_C_weights(layer, inp, consts):
    d = dict(consts)
    d["wrg"] = _kchunk(inp["w_in"][layer][:, 1792:2304], 512)
    d["gluw"] = _kchunk(inp["ssm_glu_w"][layer], 256)
    d["wout"] = _kchunk(inp["w_out"][layer], 1024)
    vecs = np.zeros((128, 12), np.float32)
    vecs[:, 0:2] = inp["ssm_glu_b"][layer].reshape(2, 128).T
    vecs[:, 2:4] = inp["ssm_norm_w"][layer].reshape(2, 128).T
    vecs[:, 4:8] = inp["ret_gn_w"][layer].reshape(4, 128).T
    vecs[:, 8:12] = inp["ret_gn_b"][layer].reshape(4, 128).T
    d["vecs"] = vecs
    d["sbw"] = np.ascontiguousarray(inp["sb_norm_w"][layer].reshape(2, 128).T)
    rows = [inp["ln_mix_w"][layer], inp["ln_mix_b"][layer], inp["ln_ffn_w"][layer], inp["ln_ffn_b"][layer]]
    d["lnrows"] = np.ascontiguousarray(np.stack([np.broadcast_to(r[None, :], (128, 1024)) for r in rows]).astype(np.float32))
    li = layer // 2
    if layer % 2 == 0:
        def padc(w):
            o = np.zeros((1024, 2816), np.float32); o[:, :D_FF] = w; return o
        wgp = padc(inp["ffn_w_gate"][li]); wup = padc(inp["ffn_w_up"][li])
        d["wg"] = np.ascontiguousarray(wgp.reshape(8, 128, 22, 128).transpose(2, 1, 0, 3))
        d["wu"] = np.ascontiguousarray(wup.reshape(8, 128, 22, 128).transpose(2, 1, 0, 3))
        wdp = np.zeros((2816, 1024), np.float32); wdp[:D_FF] = inp["ffn_w_down"][li]
        d["wd"] = np.ascontiguousarray(wdp.reshape(22, 128, 1024).transpose(1, 0, 2))
    else:
        wg = np.zeros((8, 1024, 768), np.float32); wg[:, :, :D_FFE] = inp["moe_w_gate"][li]
        wu = np.zeros((8, 1024, 768), np.float32); wu[:, :, :D_FFE] = inp["moe_w_up"][li]
        d["wg"] = np.ascontiguousarray(wg.reshape(8, 8, 128, 6, 128).transpose(0, 3, 2, 1, 4))
        d["wu"] = np.ascontiguousarray(wu.reshape(8, 8, 128, 6, 128).transpose(0, 3, 2, 1, 4))
        wd = np.zeros((8, 768, 1024), np.float32); wd[:, :D_FFE] = inp["moe_w_down"][li]
        d["wd"] = np.ascontiguousarray(wd.reshape(8, 6, 128, 1024).transpose(0, 2, 1, 3))
        d["wr"] = _kchunk(inp["moe_router"][li], 8)
    return d


class _IO:
    def __init__(self, nc, S):
        self.nc = nc
        self.S = S
        self.TOK = S // 4
        self.layer = 0
        self.t = {}
        self.cur = {}

    def get(self, phase, name):
        return self.cur[(phase, name)]

    def xT_tile(self, i, k):
        r = (i * 512) // self.TOK
        off = i * 512 - r * self.TOK
        return self.t["xT_g"][k * 512 + r * 128:k * 512 + (r + 1) * 128, off:off + 512]

    def mix_out(self, i, row0, nrows):
        q = (i * 512) // self.TOK
        off = i * 512 - q * self.TOK
        return self.t["mix_loc"][q * 256 + row0:q * 256 + row0 + nrows, off:off + 512]

    def mix_in(self, which, idx, ts):
        gq = self.gq
        if which == "oret":
            return [(0, 64, gq[1 * 256 + idx * 64:1 * 256 + idx * 64 + 64, ts]),
                    (64, 64, gq[2 * 256 + idx * 64:2 * 256 + idx * 64 + 64, ts])]
        cb = 0 if which == "yss" else 3
        return [(0, 64, gq[cb * 256 + (2 * idx) * 64:cb * 256 + (2 * idx) * 64 + 64, ts]),
                (64, 64, gq[cb * 256 + (2 * idx + 1) * 64:cb * 256 + (2 * idx + 1) * 64 + 64, ts])]


_B_LAYER = ["wfm", "wtm", "s5p", "bt", "ct", "dmat"]
_B_CONST = ["cosF", "sinF", "cosT", "sinT", "maskT", "qdec", "kdec", "gC", "iota", "m01", "mneg", "ident"]
_C_LAYER = ["wrg", "gluw", "wout", "vecs", "sbw", "lnrows"]
_C_CONST = ["blk16", "blk64", "ident"]


def _collective(nc, kind, src, dst, nchunks):
    R = src.shape[0] // nchunks
    hs = []
    for c in range(nchunks):
        cs = nc.alloc_semaphore(name="cc_%d" % _next_uid())
        hs.append(cs)
        nc.gpsimd.collective_compute(kind, ALU.bypass, replica_groups=[[0, 1, 2, 3], [4, 5, 6, 7]],
                                     ins=[src[c * R:(c + 1) * R, :].opt()], outs=[dst[c * 4 * R:(c + 1) * 4 * R, :].opt()]).then_inc(cs, 1)
        nc.gpsimd.wait_ge(cs, 1)
    nc.all_engine_barrier()
    nc.clear_and_free_semaphores(hs)
    nc.all_engine_barrier()


def _next_uid():
    _UID[0] += 1
    return _UID[0]


def build_fused(S, depth=DEPTH, upto=None):
    TOK = S // 4
    nc = bass.Bass("TRN2", target_bir_lowering=False)
    io = _IO(nc, S)
    ein = lambda name, shape, dt=F32: nc.dram_tensor(name, list(shape), dt, kind="ExternalInput").ap()
    t = io.t
    t["x"] = ein("x", [TOK, 1024])
    shp = dict(wfm=[128, 8, NFM], wtm=[128, 8, NTM], s5p=[128, 2, 3], bt=[128, 2, 2, 128], ct=[128, 2, 2, 64], dmat=[128, 64])
    for n in _B_LAYER:
        t["B_" + n] = ein("B_" + n, [depth] + shp[n])
    cshp = dict(cosF=[128, S], sinF=[128, S], cosT=[S, 128], sinT=[S, 128], maskT=[128, 2, 512], qdec=[128, 512], kdec=[128, 2], gC=[128, 1],
                iota=[128, 513], m01=[128, 128], mneg=[128, 128], ident=[128, 128])
    for n in _B_CONST:
        t["K_" + n] = ein("K_" + n, cshp[n])
    shp = dict(wrg=[128, 8, 512], gluw=[128, 2, 256], wout=[128, 8, 1024], vecs=[128, 12], sbw=[128, 2], lnrows=[4, 128, 1024])
    for n in _C_LAYER:
        t["C_" + n] = ein("C_" + n, [depth] + shp[n])
    t["K_blk16"] = ein("K_blk16", [128, 128]); t["K_blk64"] = ein("K_blk64", [128, 128])
    nd = (depth + 1) // 2
    nm = depth // 2
    t["D_wg"] = ein("D_wg", [nd, 22, 128, 8, 128]); t["D_wu"] = ein("D_wu", [nd, 22, 128, 8, 128]); t["D_wd"] = ein("D_wd", [nd, 128, 22, 1024])
    if nm:
        t["M_wg"] = ein("M_wg", [nm, 8, 6, 128, 8, 128]); t["M_wu"] = ein("M_wu", [nm, 8, 6, 128, 8, 128]); t["M_wd"] = ein("M_wd", [nm, 8, 128, 6, 1024])
        t["M_wr"] = ein("M_wr", [nm, 128, 8, 8])
    t["xo"] = nc.dram_tensor("xo", [TOK, 1024], F32, kind="ExternalOutput").ap()
    t["xT_loc"] = [nc.dram_tensor("xT_loc%d" % i, [1024, TOK], BF16).ap() for i in range(2)]
    t["xT_g"] = nc.dram_tensor("xT_g", [4 * 1024, TOK], BF16).ap()
    t["mix_loc"] = nc.dram_tensor("mix_loc", [1024, TOK], F32).ap()
    t["mix_g"] = nc.dram_tensor("mix_g", [4 * 1024 + 128, TOK], F32).ap()
    t["mix_q"] = nc.dram_tensor("mix_q", [1024, TOK], F32).ap()
    t["xbuf"] = [nc.dram_tensor("xbuf%d" % i, [TOK, 1024], F32).ap() for i in range(2)]

    io.cur = {("P", "x"): t["x"], ("P", "ident"): t["K_ident"], ("P", "xT"): t["xT_loc"][0]}
    build_P(TOK, io=io)
    if upto == "P":
        return nc, []
    _collective(nc, "AllGather", t["xT_loc"][0], t["xT_g"], 8)
    if upto == "AG0":
        return nc, []
    stats = []
    for layer in range(depth):
        io.layer = layer
        cur = {}
        for n in _B_LAYER:
            cur[("B", n)] = t["B_" + n][layer]
        for n in _B_CONST:
            cur[("B", n)] = t["K_" + n]
        io.cur = cur
        _, emB = build_B(S, io=io)
        if upto == "B%d" % layer:
            return nc, []
        _collective(nc, "AllGather", t["mix_loc"], t["mix_g"], 16)
        if upto == "AGm%d" % layer:
            return nc, []
        gs = nc.alloc_semaphore(name="gq_%d" % _next_uid())
        nc.sync.dma_start(out=t["mix_q"], in_=t["mix_g"][bass.DynSlice((nc.partition_id() % 4) * 1024, 1024), :]).then_inc(gs, 16)
        nc.sync.wait_ge(gs, 16)
        nc.all_engine_barrier()
        nc.clear_and_free_semaphores([gs])
        nc.all_engine_barrier()
        io.gq = t["mix_q"]
        if upto == "Q%d" % layer:
            return nc, []
        cur = {}
        for n in _C_LAYER:
            cur[("C", n)] = t["C_" + n][layer]
        for n in _C_CONST:
            cur[("C", n)] = t["K_" + n]
        li = layer // 2
        kind = "dense" if layer % 2 == 0 else "moe"
        pre = "D_" if kind == "dense" else "M_"
        cur[("C", "wg")] = t[pre + "wg"][li]; cur[("C", "wu")] = t[pre + "wu"][li]; cur[("C", "wd")] = t[pre + "wd"][li]
        if kind == "moe":
            cur[("C", "wr")] = t["M_wr"][li]
        cur[("C", "x")] = t["x"] if layer == 0 else t["xbuf"][(layer - 1) % 2]
        cur[("C", "xT")] = t["xT_loc"][layer % 2]
        cur[("C", "xo")] = t["xo"] if layer == depth - 1 else t["xbuf"][layer % 2]
        cur[("C", "xTo")] = t["xT_loc"][(layer + 1) % 2]
        io.cur = cur
        _, emC = build_C(TOK, kind, io=io)
        stats.append((emB.n_inst, emB.n_wait, emC.n_inst, emC.n_wait))
        if upto == "C%d" % layer:
            return nc, []
        if layer + 1 < depth:
            _collective(nc, "AllGather", t["xT_loc"][(layer + 1) % 2], t["xT_g"], 8)
            if upto == "AGx%d" % layer:
                return nc, []
    return nc, stats


_CACHE = {}


def _get(name, fn):
    if name not in _CACHE:
        _CACHE[name] = fn()
    return _CACHE[name]


def fused_inputs(inp, S, depth=DEPTH):
    TOK = S // 4
    bcon = B_consts(S)
    ccon = C_consts()
    x = np.ascontiguousarray(inp["x"], dtype=np.float32)
    shared = {}
    for n in _B_CONST:
        if n not in ("maskT", "qdec", "kdec", "gC"):
            shared["K_" + n] = bcon[n]
    shared["K_blk16"] = ccon["blk16"]; shared["K_blk64"] = ccon["blk64"]
    cw = [prep_C_weights(l, inp, {}) for l in range(depth)]
    for n in _C_LAYER:
        shared["C_" + n] = np.ascontiguousarray(np.stack([cw[l][n] for l in range(depth)]))
    dl = [l for l in range(depth) if l % 2 == 0]
    ml = [l for l in range(depth) if l % 2 == 1]
    for n in ("wg", "wu", "wd"):
        shared["D_" + n] = np.ascontiguousarray(np.stack([cw[l][n] for l in dl]))
        if ml:
            shared["M_" + n] = np.ascontiguousarray(np.stack([cw[l][n] for l in ml]))
    if ml:
        shared["M_wr"] = np.ascontiguousarray(np.stack([cw[l]["wr"] for l in ml]))
    del cw
    perj = []
    for j in range(4):
        bl = [prep_B_inputs(l, j, inp["w_in"], inp["ssm_lam_re"], inp["ssm_lam_im"], inp["ssm_log_dt"], inp["ssm_b_re"], inp["ssm_b_im"],
                            inp["ssm_c_re"], inp["ssm_c_im"], inp["ssm_d"], S, {}) for l in range(depth)]
        d = {}
        for n in _B_LAYER:
            d["B_" + n] = np.ascontiguousarray(np.stack([bl[l][n] for l in range(depth)]))
        for n in ("maskT", "qdec", "kdec", "gC"):
            d["K_" + n] = bl[0][n]
        perj.append(d)
    maps = []
    for core in range(8):
        b, j = core // 4, core % 4
        d = dict(shared)
        d.update(perj[j])
        d["x"] = np.ascontiguousarray(x[b, j * TOK:(j + 1) * TOK])
        maps.append(d)
    return maps


def kernel(**inputs):
    inp = {k: np.asarray(v) for k, v in inputs.items()}
    S = inp["x"].shape[1]
    TOK = S // 4
    nc = _get(("F", S), lambda: build_fused(S)[0])
    maps = fused_inputs(inp, S)
    res = run_bass_kernel_spmd(nc, maps, core_ids=list(range(8))).results
    out = np.stack([np.concatenate([res[b * 4 + c]["xo"] for c in range(4)], axis=0) for b in range(2)])
    return np.ascontiguousarray(out, dtype=np.float32)
```

```python
import numpy as np
import ml_dtypes
import concourse.bass as bass
import concourse.mybir as mybir
from concourse.bass_utils import run_bass_kernel_spmd
from contextlib import ExitStack

F32 = mybir.dt.float32
BF16 = mybir.dt.bfloat16
AF = mybir.ActivationFunctionType
ALU = mybir.AluOpType
AX = mybir.AxisListType

D_MODEL = 1024
BATCH = 2
SEQ = 16384
DEPTH = 4
D_FF = 2752
D_FFE = 688
N_EXP = 8
ALPHA = (2.0 * DEPTH) ** 0.25
LN_EPS = 1e-5
MAGIC = 12582912.0
TWO_PI = float(2 * np.pi)


class Buf:
    __slots__ = ("w", "r")

    def __init__(self):
        self.w = None
        self.r = {}


class Sem:
    __slots__ = ("h", "val", "key")

    def __init__(self, h, key):
        self.h = h
        self.val = 0
        self.key = key


class Eng:
    def __init__(self, name, obj, sem):
        self.name = name
        self.obj = obj
        self.sem = sem
        self.waited = {}


_UID = [0]


class Emitter:
    def __init__(self, nc):
        self.nc = nc
        self.es = ExitStack()
        self.sems = {}
        self.nsem = 0
        self.handles = []
        self.engs = {}
        for name, obj in (("pe", nc.tensor), ("act", nc.scalar), ("dve", nc.vector),
                          ("pool", nc.gpsimd), ("sp", nc.sync)):
            self.engs[name] = Eng(name, obj, self.new_sem("e_" + name))
        self.n_inst = 0
        self.n_wait = 0
        self.nt = 0

    def new_sem(self, name="s"):
        _UID[0] += 1
        h = self.nc.alloc_semaphore(name=name + "_%d_%d" % (self.nsem, _UID[0]))
        self.handles.append(h)
        s = Sem(h, self.nsem)
        self.sems[s.key] = s
        self.nsem += 1
        return s

    def sbuf(self, shape, dtype, name=None):
        _UID[0] += 1
        return self.es.enter_context(self.nc.sbuf_tensor((name or "t") + "_%d" % _UID[0], list(shape), dtype))

    def psum(self, shape, dtype=F32, name=None):
        _UID[0] += 1
        return self.es.enter_context(self.nc.psum_tensor((name or "p") + "_%d" % _UID[0], list(shape), dtype))

    def _wait(self, E, deps):
        best = {}
        for (k, v) in deps:
            if v > best.get(k, 0):
                best[k] = v
        for k, v in best.items():
            if E.waited.get(k, 0) >= v:
                continue
            if E.name == "pe" and k == E.sem.key:
                continue
            E.obj.wait_ge(self.sems[k].h, v)
            E.waited[k] = v
            self.n_wait += 1

    @staticmethod
    def _deps(r, w):
        deps = []
        for b in r:
            if b.w is not None:
                deps.append(b.w)
        for b in w:
            if b.w is not None:
                deps.append(b.w)
            deps.extend(b.r.items())
        return deps

    @staticmethod
    def _mark(d, r, w):
        for b in r:
            if d[1] > b.r.get(d[0], 0):
                b.r[d[0]] = d[1]
        for b in w:
            b.w = d
            b.r = {}

    def do(self, eng, fn, r=(), w=()):
        E = self.engs[eng]
        self._wait(E, self._deps(r, w))
        ins = fn(E.obj)
        E.sem.val += 1
        ins.then_inc(E.sem.h, 1)
        self._mark((E.sem.key, E.sem.val), r, w)
        self.n_inst += 1
        return ins

    def dma(self, q, sem, out, in_, r=(), w=()):
        E = self.engs[q]
        self._wait(E, self._deps(r, w))
        ins = E.obj.dma_start(out=out, in_=in_)
        sem.val += 16
        ins.then_inc(sem.h, 16)
        self._mark((sem.key, sem.val), r, w)
        self.n_inst += 1
        return ins

    def dma_multi(self, q, sem, pairs, r=(), w=()):
        E = self.engs[q]
        self._wait(E, self._deps(r, w))
        for (out, in_) in pairs:
            ins = E.obj.dma_start(out=out, in_=in_)
            sem.val += 16
            ins.then_inc(sem.h, 16)
            self.n_inst += 1
        self._mark((sem.key, sem.val), r, w)

    def group_mark(self, sem, ts):
        for t in ts:
            t.b.w = (sem.key, sem.val)

    def finish(self, sems):
        E = self.engs["sp"]
        for s in sems:
            if s.val > 0:
                E.obj.wait_ge(s.h, s.val)

    def finish_all(self):
        E = self.engs["sp"]
        for s in self.sems.values():
            if s.val > 0:
                E.obj.wait_ge(s.h, s.val)

    def close(self):
        self.es.close()


class T:
    __slots__ = ("t", "b")

    def __init__(self, t):
        self.t = t
        self.b = Buf()


NFM = 704
NTM = 448


class _Stop(Exception):
    pass


def _end_phase(nc, em):
    em.finish_all()
    nc.all_engine_barrier()
    nc.clear_and_free_semaphores(em.handles)
    em.close()
    nc.all_engine_barrier()


def build_B(S, phases=('s5', 'ret', 'sb'), stop=None, io=None):
    try:
        return _build_B(S, phases, stop, io)
    except _Stop as e:
        em = e.args[0]
        em.finish_all()
        em.close()
        return em.nc, em


def _build_B(S, phases, stop, io):
    NT = S // 512
    nc = io.nc if io is not None else bass.Bass("TRN2", target_bir_lowering=False)

    def din(name, shape, dt=F32):
        if io is not None:
            return io.get("B", name)
        return nc.dram_tensor(name, list(shape), dt, kind="ExternalInput").ap()

    def dout(name, shape, dt=F32):
        if io is not None:
            return None
        return nc.dram_tensor(name, list(shape), dt, kind="ExternalOutput").ap()

    xT = din("xT", [1024, S], BF16) if io is None else None
    wfm_d = din("wfm", [128, 8, NFM])
    wtm_d = din("wtm", [128, 8, NTM])
    cosF_d = din("cosF", [128, S])
    sinF_d = din("sinF", [128, S])
    cosT_d = din("cosT", [S, 128])
    sinT_d = din("sinT", [S, 128])
    maskT_d = din("maskT", [128, 2, 512])
    qdec_d = din("qdec", [128, 512])
    kdec_d = din("kdec", [128, 2])
    gC_d = din("gC", [128, 1])
    s5p_d = din("s5p", [128, 2, 3])
    bt_d = din("bt", [128, 2, 2, 128])
    ct_d = din("ct", [128, 2, 2, 64])
    dm_d = din("dmat", [128, 64])
    iota_d = din("iota", [128, 513])
    m01_d = din("m01", [128, 128])
    mneg_d = din("mneg", [128, 128])
    ident_d = din("ident", [128, 128])

    yssm_o = dout("yssmT", [64, S])
    oret_o = dout("oretT", [128, S])
    osb_o = dout("osbT", [64, S])

    em = Emitter(nc)

    def CP(tag):
        if stop == tag:
            raise _Stop(em)
    sb = lambda shape, dt=F32: T(em.sbuf(shape, dt))
    ldw = em.new_sem("ldw"); ldh = em.new_sem("ldh")
    st_y = em.new_sem("sty"); st_r = [em.new_sem("str0"), em.new_sem("str1")]; st_s = em.new_sem("sts")

    wfm = sb([128, 8, NFM], BF16)
    wtm = sb([128, 8, NTM], BF16)
    for k in range(8):
        em.dma("pool", ldw, wfm.t[:, k, :], wfm_d[:, k, :])
        em.dma("pool", ldw, wtm.t[:, k, :], wtm_d[:, k, :])
    maskT = sb([128, 2, 512]); em.dma("sp", ldh, maskT.t[:], maskT_d)
    qdec = sb([128, 512]); em.dma("sp", ldh, qdec.t[:], qdec_d)
    kdec = sb([128, 2]); em.dma("sp", ldh, kdec.t[:], kdec_d)
    gC = sb([128, 1]); em.dma("sp", ldh, gC.t[:], gC_d)
    s5p = sb([128, 2, 3]); em.dma("sp", ldh, s5p.t[:], s5p_d)
    bt = sb([128, 2, 2, 128], BF16); em.dma("pool", ldw, bt.t[64:128], bt_d[64:128])
    ct = sb([128, 2, 2, 64], BF16); em.dma("pool", ldw, ct.t[:], ct_d)
    dmat = sb([128, 64], BF16); em.dma("pool", ldw, dmat.t[64:128], dm_d[64:128])
    iota = sb([128, 513]); em.dma("sp", ldh, iota.t[:], iota_d)
    m01 = sb([128, 128]); em.dma("sp", ldh, m01.t[:], m01_d)
    mneg = sb([128, 128]); em.dma("sp", ldh, mneg.t[:], mneg_d)
    ident = sb([128, 128], BF16); em.dma("pool", ldw, ident.t[:], ident_d)
    em.group_mark(ldw, [wfm, wtm, bt, ct, dmat, ident])
    em.group_mark(ldh, [maskT, qdec, kdec, gC, s5p, iota, m01, mneg])
    ones = sb([128, 513]); em.do("dve", lambda e: e.memset(ones.t[:], 1.0), w=[ones.b])

    CP("c1")
    m = [sb([128, 513]) for _ in range(4)]
    wre = sb([128, 513]); wim = sb([128, 513]); xr = sb([128, 513]); xi = sb([128, 513])
    sc = lambda: sb([128, 2])
    dtt = sc(); em.do("act", lambda e: e.activation(out=dtt.t[:], in_=s5p.t[:, :, 2], func=AF.Exp), r=[s5p.b], w=[dtt.b])
    aa = sc(); em.do("dve", lambda e: e.tensor_tensor(out=aa.t[:], in0=s5p.t[:, :, 0], in1=dtt.t[:], op=ALU.mult), r=[s5p.b, dtt.b], w=[aa.b])
    th = sc(); em.do("dve", lambda e: e.tensor_tensor(out=th.t[:], in0=s5p.t[:, :, 1], in1=dtt.t[:], op=ALU.mult), r=[s5p.b, dtt.b], w=[th.b])
    rr = sc(); em.do("act", lambda e: e.activation(out=rr.t[:], in_=aa.t[:], func=AF.Exp), r=[aa.b], w=[rr.b])

    CP("c2")

    def sincos(out_sin, out_cos, ang, shape):
        n = shape[1]
        A = lambda x_: ang.t[:, 0:n] if x_ is ang else x_.t[:, 0:n]
        for (o, shift) in ((out_sin, 0.0), (out_cos, 0.25)):
            t = m[0]; k = m[1]
            em.do("dve", lambda e: e.tensor_scalar(out=t.t[:, 0:n], in0=ang.t[:, 0:n], scalar1=1.0 / TWO_PI, scalar2=shift, op0=ALU.mult, op1=ALU.add), r=[ang.b], w=[t.b])
            em.do("dve", lambda e: e.tensor_scalar(out=k.t[:, 0:n], in0=t.t[:, 0:n], scalar1=MAGIC, scalar2=None, op0=ALU.add), r=[t.b], w=[k.b])
            em.do("dve", lambda e: e.tensor_scalar(out=k.t[:, 0:n], in0=k.t[:, 0:n], scalar1=MAGIC, scalar2=None, op0=ALU.subtract), r=[k.b], w=[k.b])
            em.do("dve", lambda e: e.tensor_tensor(out=t.t[:, 0:n], in0=t.t[:, 0:n], in1=k.t[:, 0:n], op=ALU.subtract), r=[t.b, k.b], w=[t.b])
            em.do("dve", lambda e: e.tensor_scalar(out=t.t[:, 0:n], in0=t.t[:, 0:n], scalar1=TWO_PI, scalar2=3.14159, op0=ALU.mult, op1=ALU.min), r=[t.b], w=[t.b])
            em.do("dve", lambda e: e.tensor_scalar(out=t.t[:, 0:n], in0=t.t[:, 0:n], scalar1=-3.14159, scalar2=None, op0=ALU.max), r=[t.b], w=[t.b])
            em.do("act", lambda e: e.activation(out=o.t[:, 0:n], in_=t.t[:, 0:n], func=AF.Sin), r=[t.b], w=[o.b])

    sn = sc(); cs_ = sc()
    sincos(sn, cs_, th, [128, 2])

    CP("c3")

    def tt(out, a, b, op, eng="dve"):
        em.do(eng, lambda e: e.tensor_tensor(out=out.t[:], in0=a.t[:], in1=b.t[:], op=op), r=[a.b, b.b], w=[out.b])

    nr = sc(); tt(nr, rr, cs_, ALU.mult)
    em.do("dve", lambda e: e.tensor_scalar(out=nr.t[:], in0=nr.t[:], scalar1=-1.0, scalar2=None, op0=ALU.add), r=[nr.b], w=[nr.b])
    ni = sc(); tt(ni, rr, sn, ALU.mult)
    lre = sc(); em.do("dve", lambda e: e.tensor_copy(out=lre.t[:], in_=s5p.t[:, :, 0]), r=[s5p.b], w=[lre.b])
    lim = sc(); em.do("dve", lambda e: e.tensor_copy(out=lim.t[:], in_=s5p.t[:, :, 1]), r=[s5p.b], w=[lim.b])
    den = sc(); t0 = sc()
    tt(den, lre, lre, ALU.mult); tt(t0, lim, lim, ALU.mult); tt(den, den, t0, ALU.add)
    rden = sc(); em.do("dve", lambda e: e.reciprocal(out=rden.t[:], in_=den.t[:]), r=[den.b], w=[rden.b])
    cre = sc(); cim = sc(); t1 = sc()
    tt(cre, nr, lre, ALU.mult); tt(t1, ni, lim, ALU.mult); tt(cre, cre, t1, ALU.add); tt(cre, cre, rden, ALU.mult)
    tt(cim, ni, lre, ALU.mult); tt(t1, nr, lim, ALU.mult); tt(cim, cim, t1, ALU.subtract); tt(cim, cim, rden, ALU.mult)

    CP("c4")
    T1re = [None, None]; T1im = [None, None]; T2re = [None, None]; T2im = [None, None]; Rfull = [None, None]
    Ere = sc(); Eim = sc()
    for rb in range(2):
        ang = m[2]
        em.do("dve", lambda e: e.tensor_scalar(out=ang.t[:], in0=iota.t[:], scalar1=th.t[:, rb:rb + 1], scalar2=None, op0=ALU.mult), r=[iota.b, th.b], w=[ang.b])
        sT = sb([128, 513]); cT = sb([128, 513])
        sincos(sT, cT, ang, [128, 513])
        T2re[rb] = cT; T2im[rb] = sT
        em.do("dve", lambda e: e.tensor_copy(out=Ere.t[:, rb:rb + 1], in_=cT.t[:, 512:513]), r=[cT.b], w=[Ere.b])
        em.do("dve", lambda e: e.tensor_copy(out=Eim.t[:, rb:rb + 1], in_=sT.t[:, 512:513]), r=[sT.b], w=[Eim.b])
        a1 = m[3]; a2 = wre; t1r = sb([128, 512]); t1i = sb([128, 512])
        em.do("dve", lambda e: e.tensor_scalar(out=a1.t[:, 0:512], in0=cT.t[:, 0:512], scalar1=cre.t[:, rb:rb + 1], scalar2=None, op0=ALU.mult), r=[cT.b, cre.b], w=[a1.b])
        em.do("dve", lambda e: e.scalar_tensor_tensor(out=t1r.t[:], in0=sT.t[:, 0:512], scalar=cim.t[:, rb:rb + 1], in1=a1.t[:, 0:512], op0=ALU.mult, op1=ALU.add), r=[sT.b, cim.b, a1.b], w=[t1r.b])
        em.do("dve", lambda e: e.tensor_scalar(out=a2.t[:, 0:512], in0=sT.t[:, 0:512], scalar1=cre.t[:, rb:rb + 1], scalar2=None, op0=ALU.mult), r=[sT.b, cre.b], w=[a2.b])
        em.do("dve", lambda e: e.scalar_tensor_tensor(out=t1i.t[:], in0=cT.t[:, 0:512], scalar=cim.t[:, rb:rb + 1], in1=a2.t[:, 0:512], op0=ALU.mult, op1=ALU.subtract), r=[cT.b, cim.b, a2.b], w=[t1i.b])
        T1re[rb] = t1r; T1im[rb] = t1i
        rf = sb([128, 512])
        em.do("dve", lambda e: e.tensor_scalar(out=rf.t[:], in0=ones.t[:, 0:512], scalar1=rr.t[:, rb:rb + 1], scalar2=None, op0=ALU.mult), r=[ones.b, rr.b], w=[rf.b])
        Rfull[rb] = rf
    ctn = sb([128, 2, 64], BF16)
    em.do("dve", lambda e: e.tensor_scalar(out=ctn.t[:], in0=ct.t[:, 1, :, :], scalar1=-1.0, scalar2=None, op0=ALU.mult), r=[ct.b], w=[ctn.b])
    car = [[sb([128, 1]) for _ in range(2)] for _ in range(2)]
    for rb in range(2):
        for c in range(2):
            em.do("dve", lambda e: e.memset(car[rb][c].t[:], 0.0), w=[car[rb][c].b])

    CP("c5")
    KT = em.sbuf([64, S], BF16)
    KTb = [Buf() for _ in range(NT)]
    VR = em.sbuf([128, S // 128, 64], BF16)
    VRb = [Buf() for _ in range(NT)]
    Sall = [sb([128, 5, 64]) for _ in range(2)]
    em.do("dve", lambda e: e.memset(Sall[0].t[:], 0.0), w=[Sall[0].b])
    em.do("dve", lambda e: e.memset(Sall[1].t[:], 0.0), w=[Sall[1].b])

    PB = [T(em.psum([128, 512])) for _ in range(7)]
    PT = T(em.psum([128, 1024], BF16))
    PTb = [Buf(), Buf()]

    xt = [sb([128, 8, 512], BF16) for _ in range(2)]
    xsem = [[em.new_sem("x") for _ in range(5)] for _ in range(2)]
    xsem[1][1:] = xsem[0][1:]
    _cF = sb([128, 512]); _sF = sb([128, 512]); _cTm = sb([128, 4, 128]); _sTm = sb([128, 4, 128])
    cF = [_cF, _cF]; sF = [_sF, _sF]; cTm = [_cTm, _cTm]; sTm = [_sTm, _sTm]
    FU = [sb([128, 512], BF16) for _ in range(2)]
    qh = sb([128, 512], BF16); qtl = sb([128, 512], BF16); kh = sb([128, 512], BF16)
    qf = sb([128, 512])
    ktm = sb([128, 4, 128], BF16); vtm = sb([128, 4, 128], BF16); vdm = sb([128, 4, 128], BF16)
    g1 = sb([128, 128]); g2 = sb([128, 128])
    sTs = [sb([128, 512], BF16) for _ in range(2)]
    Sbf = sb([128, 4, 64], BF16)
    oret_t = [sb([64, 512]) for _ in range(2)]
    yss_t = sb([64, 512])
    osb_t = sb([64, 512])
    Xre = sb([128, 512], BF16); Xim = sb([128, 512], BF16)
    tA = sb([128, 1]); tB = sb([128, 1])
    NBUF = 2
    e_t = [sb([128, 512]) for _ in range(NBUF)]
    sp_t = [sb([128, 513]) for _ in range(NBUF)]
    cs_t = [sb([128, 513]) for _ in range(NBUF)]
    t_t = [sb([128, 512]) for _ in range(NBUF)]
    A_t = [sb([128, 512], BF16) for _ in range(NBUF)]
    AT_t = [sb([128, 512], BF16) for _ in range(NBUF)]
    for i in range(NBUF):
        em.do("dve", lambda e: e.memset(sp_t[i].t[:], 0.0), w=[sp_t[i].b])
    negR = [sb([128, 1]) for _ in range(4)]

    def load_tile(i):
        s = i % 2
        if io is None:
            em.dma("sp", xsem[s][0], xt[s].t[:], xT[:, i * 512:(i + 1) * 512].rearrange("(k p) t -> p k t", p=128), w=[xt[s].b])
        else:
            em.dma_multi("sp", xsem[s][0], [(xt[s].t[:, k, :], io.xT_tile(i, k)) for k in range(8)], w=[xt[s].b])

    def load_tabs(i):
        s = i % 2
        em.dma("sp", xsem[s][1], cF[s].t[:], cosF_d[:, i * 512:(i + 1) * 512], w=[cF[s].b])
        em.dma("sp", xsem[s][2], sF[s].t[:], sinF_d[:, i * 512:(i + 1) * 512], w=[sF[s].b])
        em.dma("sp", xsem[s][3], cTm[s].t[:], cosT_d[i * 512:(i + 1) * 512, :].rearrange("(c p) d -> p c d", p=128), w=[cTm[s].b])
        em.dma("sp", xsem[s][4], sTm[s].t[:], sinT_d[i * 512:(i + 1) * 512, :].rearrange("(c p) d -> p c d", p=128), w=[sTm[s].b])

    load_tile(0)
    load_tabs(0)
    sbit = 0
    for i in (range(NT) if 'noloop' not in phases else ()):
        s = i % 2
        if i + 1 < NT:
            load_tile(i + 1)
        X = xt[s]
        fu = FU[s]

        def fm_group(pb, c0, M):
            for k in range(8):
                em.do("pe", lambda e: e.matmul(pb.t[:M, :], wfm.t[:, k, c0:c0 + M], X.t[:, k, :], start=(k == 0), stop=(k == 7)),
                      r=[wfm.b, X.b], w=[pb.b])

        def rope_fm(pa, pb_, outf):
            em.do("dve", lambda e: e.tensor_tensor(out=m[0].t[:, 0:512], in0=pa.t[:], in1=cF[s].t[:], op=ALU.mult), r=[pa.b, cF[s].b], w=[m[0].b])
            em.do("dve", lambda e: e.tensor_tensor(out=m[1].t[:, 0:512], in0=pb_.t[:], in1=sF[s].t[:], op=ALU.mult), r=[pb_.b, sF[s].b], w=[m[1].b])
            em.do("pool", lambda e: e.tensor_tensor(out=outf.t[:], in0=m[0].t[:, 0:512], in1=m[1].t[:, 0:512], op=ALU.add), r=[m[0].b, m[1].b], w=[outf.b])

        fm_group(PB[0], 0, 128); fm_group(PB[1], 128, 128)
        rope_fm(PB[0], PB[1], qf)
        em.do("act", lambda e: e.activation(out=qh.t[:], in_=qf.t[:], func=AF.Copy), r=[qf.b], w=[qh.b])
        em.do("pool", lambda e: e.tensor_tensor(out=qtl.t[:], in0=qf.t[:], in1=qdec.t[:], op=ALU.mult), r=[qf.b, qdec.b], w=[qtl.b])
        fm_group(PB[0], 256, 128); fm_group(PB[1], 384, 128)
        rope_fm(PB[0], PB[1], kh)
        fm_group(PB[0], 512, 128)
        em.do("act", lambda e: e.activation(out=fu.t[:], in_=PB[0].t[:], func=AF.Copy), r=[PB[0].b], w=[fu.b])
        fm_group(PB[1], 640, 64)
        em.do("act", lambda e: e.activation(out=KT[:, i * 512:(i + 1) * 512], in_=PB[1].t[:64, :], func=AF.Copy), r=[PB[1].b], w=[KTb[i]])
        CP("p1")
        for blk in range(4):
            pb = PB[2 + (blk % 2)]
            for k in range(8):
                em.do("pe", lambda e: e.matmul(pb.t[:, :NTM], X.t[:, k, blk * 128:(blk + 1) * 128], wtm.t[:, k, :], start=(k == 0), stop=(k == 7)),
                      r=[wtm.b, X.b], w=[pb.b])
            CP("q1")
            em.do("dve", lambda e: e.tensor_tensor(out=g1.t[:], in0=pb.t[:, 0:128], in1=cTm[s].t[:, blk, :], op=ALU.mult), r=[pb.b, cTm[s].b], w=[g1.b])
            em.do("dve", lambda e: e.tensor_tensor(out=g2.t[:], in0=pb.t[:, 128:256], in1=sTm[s].t[:, blk, :], op=ALU.mult), r=[pb.b, sTm[s].b], w=[g2.b])
            em.do("pool", lambda e: e.tensor_tensor(out=ktm.t[:, blk, :], in0=g1.t[:], in1=g2.t[:], op=ALU.add), r=[g1.b, g2.b], w=[ktm.b])
            CP("q2")
            em.do("dve", lambda e: e.tensor_copy(out=vtm.t[:, blk, :], in_=pb.t[:, 256:384]), r=[pb.b], w=[vtm.b])
            CP("q3")
            for h in range(2):
                em.do("dve", lambda e: e.tensor_scalar(out=vdm.t[:, blk, h * 64:(h + 1) * 64], in0=pb.t[:, 256 + h * 64:256 + (h + 1) * 64],
                                                       scalar1=kdec.t[:, h:h + 1], scalar2=None, op0=ALU.mult), r=[pb.b, kdec.b], w=[vdm.b])
            CP("q4")
            em.do("dve", lambda e: e.tensor_copy(out=VR[:, i * 4 + blk, :], in_=pb.t[:, 384:448]), r=[pb.b], w=[VRb[i]])

        if i + 1 < NT:
            load_tabs(i + 1)
        CP("p2")
        for rb in (range(2) if 's5' in phases else ()):
            em.do("pe", lambda e: e.matmul(PB[0].t[:, :], bt.t[64:128, 0, rb, :], fu.t[64:128, :], start=True, stop=True), r=[bt.b, fu.b], w=[PB[0].b])
            em.do("pe", lambda e: e.matmul(PB[1].t[:, :], bt.t[64:128, 1, rb, :], fu.t[64:128, :], start=True, stop=True), r=[bt.b, fu.b], w=[PB[1].b])
            pr, pi = PB[0], PB[1]
            mm_ = lambda o, a, b_: em.do("dve", lambda e: e.tensor_tensor(out=o.t[:, 0:512], in0=a.t[:, 0:512], in1=b_.t[:, 0:512], op=ALU.mult), r=[a.b, b_.b], w=[o.b])
            tt5 = lambda o, a, b_, op, eng: em.do(eng, lambda e: e.tensor_tensor(out=o.t[:, 0:512], in0=a.t[:, 0:512], in1=b_.t[:, 0:512], op=op), r=[a.b, b_.b], w=[o.b])
            mm_(m[0], pr, T1re[rb]); mm_(m[1], pi, T1im[rb]); mm_(m[2], pi, T1re[rb]); mm_(m[3], pr, T1im[rb])
            tt5(wre, m[0], m[1], ALU.subtract, "pool"); tt5(wim, m[2], m[3], ALU.add, "pool")
            for (xo, wi, c) in ((xr, wre, 0), (xi, wim, 1)):
                em.do("dve", lambda e: e.tensor_tensor_scan(out=xo.t[:, 0:512], data0=Rfull[rb].t[:], data1=wi.t[:, 0:512], initial=car[rb][c].t[:, 0:1],
                                                            op0=ALU.mult, op1=ALU.add), r=[Rfull[rb].b, wi.b, car[rb][c].b], w=[xo.b])
            em.do("dve", lambda e: e.tensor_tensor(out=tA.t[:], in0=xi.t[:, 511:512], in1=Eim.t[:, rb:rb + 1], op=ALU.mult), r=[xi.b, Eim.b], w=[tA.b])
            em.do("dve", lambda e: e.scalar_tensor_tensor(out=car[rb][0].t[:], in0=xr.t[:, 511:512], scalar=Ere.t[:, rb:rb + 1], in1=tA.t[:], op0=ALU.mult, op1=ALU.subtract),
                  r=[xr.b, Ere.b, tA.b], w=[car[rb][0].b])
            em.do("dve", lambda e: e.tensor_tensor(out=tB.t[:], in0=xr.t[:, 511:512], in1=Eim.t[:, rb:rb + 1], op=ALU.mult), r=[xr.b, Eim.b], w=[tB.b])
            em.do("dve", lambda e: e.scalar_tensor_tensor(out=car[rb][1].t[:], in0=xi.t[:, 511:512], scalar=Ere.t[:, rb:rb + 1], in1=tB.t[:], op0=ALU.mult, op1=ALU.add),
                  r=[xi.b, Ere.b, tB.b], w=[car[rb][1].b])
            pm = lambda o, a, b_: em.do("pool", lambda e: e.tensor_tensor(out=o.t[:, 0:512], in0=a.t[:, 0:512], in1=b_.t[:, 0:512], op=ALU.mult), r=[a.b, b_.b], w=[o.b])
            pm(m[0], xr, T2re[rb]); pm(m[1], xi, T2im[rb]); mm_(m[2], xi, T2re[rb]); mm_(m[3], xr, T2im[rb])
            tt5(Xre, m[0], m[1], ALU.subtract, "pool"); tt5(Xim, m[2], m[3], ALU.add, "dve")
            em.do("pe", lambda e: e.matmul(PB[3].t[:64, :], ct.t[:, 0, rb, :], Xre.t[:], start=(rb == 0), stop=False), r=[ct.b, Xre.b], w=[PB[3].b])
            em.do("pe", lambda e: e.matmul(PB[3].t[:64, :], ctn.t[:, rb, :], Xim.t[:], start=False, stop=False), r=[ctn.b, Xim.b], w=[PB[3].b])
        if 's5' in phases:
            em.do("pe", lambda e: e.matmul(PB[3].t[:64, :], dmat.t[64:128, :], fu.t[64:128, :], start=False, stop=True), r=[dmat.b, fu.b], w=[PB[3].b])
            em.do("act", lambda e: e.activation(out=yss_t.t[:], in_=PB[3].t[:64, :], func=AF.Copy), r=[PB[3].b], w=[yss_t.b])
            em.dma("sp", st_y, yssm_o[:, i * 512:(i + 1) * 512] if io is None else io.mix_out(i, 0, 64), yss_t.t[:], r=[yss_t.b])

        CP("p3")
        for _once in ([0] if 'ret' in phases else []):
            Sc = Sall[i % 2]; Sn = Sall[(i + 1) % 2]
            for c in range(4):
                em.do("pe", lambda e: e.matmul(PB[5].t[:, c * 128:(c + 1) * 128], ktm.t[:, c, :], vdm.t[:, c, :], start=True, stop=True), r=[ktm.b, vdm.b], w=[PB[5].b])
            for c in range(4):
                for h in range(2):
                    P = slice(h * 64, (h + 1) * 64)
                    em.do("dve", lambda e: e.scalar_tensor_tensor(out=Sc.t[P, c + 1, :], in0=Sc.t[P, c, :], scalar=gC.t[P, 0:1],
                                                                  in1=PB[5].t[P, c * 128 + h * 64:c * 128 + (h + 1) * 64], op0=ALU.mult, op1=ALU.add),
                          r=[Sc.b, gC.b, PB[5].b], w=[Sc.b])
            em.do("act", lambda e: e.activation(out=Sbf.t[:], in_=Sc.t[:, 0:4, :], func=AF.Copy), r=[Sc.b], w=[Sbf.b])
            em.do("dve", lambda e: e.tensor_copy(out=Sn.t[:, 0, :], in_=Sc.t[:, 4, :]), r=[Sc.b], w=[Sn.b])
            for h in range(2):
                P = slice(h * 64, (h + 1) * 64)
                for c in range(4):
                    em.do("pe", lambda e: e.matmul(PB[4].t[:, c * 128:(c + 1) * 128], kh.t[P, c * 128:(c + 1) * 128], qh.t[P, c * 128:(c + 1) * 128], start=True, stop=True),
                          r=[kh.b, qh.b], w=[PB[4].b])
                em.do("dve", lambda e: e.tensor_tensor(out=sTs[h].t[:], in0=PB[4].t[:], in1=maskT.t[:, h, :], op=ALU.mult), r=[PB[4].b, maskT.b], w=[sTs[h].b])
                for c in range(4):
                    cs = slice(c * 128, (c + 1) * 128)
                    em.do("pe", lambda e: e.matmul(PB[6].t[:64, cs], vtm.t[:, c, h * 64:(h + 1) * 64], sTs[h].t[:, cs], start=True, stop=False),
                          r=[vtm.b, sTs[h].b], w=[PB[6].b])
                    em.do("pe", lambda e: e.matmul(PB[6].t[:64, cs], Sbf.t[P, c, :], qtl.t[P, cs], start=False, stop=True),
                          r=[Sbf.b, qtl.b], w=[PB[6].b])
                em.do("act", lambda e: e.activation(out=oret_t[h].t[:], in_=PB[6].t[:64, :], func=AF.Copy), r=[PB[6].b], w=[oret_t[h].b])
                em.dma("sp", st_r[h], oret_o[h * 64:(h + 1) * 64, i * 512:(i + 1) * 512] if io is None else io.mix_out(i, 64 + h * 64, 64), oret_t[h].t[:], r=[oret_t[h].b])

        CP("p4")
        for _once in ([0] if 'sb' in phases else []):
            its = []
            for qb in range(4):
                nmm = sum(((qb + 1) if kt == i else 4) for kt in range(i + 1))
                imm = 0
                for kt in range(i, -1, -1):
                    nblk = (qb + 1) if kt == i else 4
                    its.append(dict(qb=qb, kt=kt, nblk=nblk, nco=nblk * 128, imm0=imm, nmm=nmm, first=(kt == i), u=sbit % NBUF))
                    sbit += 1
                    imm += nblk

            def S1(d):
                qb, kt, nco, u = d["qb"], d["kt"], d["nco"], d["u"]
                zb = PB[u]
                E_, SP = e_t[u], sp_t[u]
                qs = slice(qb * 128, (qb + 1) * 128)
                if d["first"]:
                    em.do("pool", lambda e: e.memset(negR[qb].t[:], 0.0), w=[negR[qb].b])
                em.do("pe", lambda e: e.matmul(zb.t[:, :nco], fu.t[0:64, qs], KT[:, kt * 512:kt * 512 + nco], start=True, stop=True),
                      r=[fu.b, KTb[kt]], w=[zb.b])
                em.do("act", lambda e: e.activation(out=E_.t[:, :nco], in_=zb.t[:, :nco], func=AF.Exp, scale=0.125), r=[zb.b], w=[E_.b])
                em.do("act", lambda e: e.activation(out=SP.t[:, 1:1 + nco], in_=E_.t[:, :nco], func=AF.Ln, bias=1.0, scale=1.0), r=[E_.b], w=[SP.b])
                if d["first"]:
                    em.do("pool", lambda e: e.tensor_tensor(out=SP.t[:, 1 + nco - 128:1 + nco], in0=SP.t[:, 1 + nco - 128:1 + nco], in1=m01.t[:], op=ALU.mult),
                          r=[SP.b, m01.b], w=[SP.b])

            def S2a(d):
                qb, nco, u = d["qb"], d["nco"], d["u"]
                zb = PB[u]
                SP, CS, TT, A_ = sp_t[u], cs_t[u], t_t[u], A_t[u]
                nR = negR[qb]
                em.do("dve", lambda e: e.tensor_tensor_scan(out=CS.t[:, 0:nco + 1], data0=ones.t[:, 0:nco + 1], data1=SP.t[:, 0:nco + 1], initial=0.0,
                                                            op0=ALU.mult, op1=ALU.add), r=[ones.b, SP.b], w=[CS.b])
                em.do("dve", lambda e: e.tensor_tensor(out=nR.t[:], in0=nR.t[:], in1=CS.t[:, nco:nco + 1], op=ALU.subtract), r=[nR.b, CS.b], w=[nR.b])
                em.do("dve", lambda e: e.scalar_tensor_tensor(out=TT.t[:, :nco], in0=zb.t[:, :nco], scalar=0.125, in1=CS.t[:, 0:nco], op0=ALU.mult, op1=ALU.add),
                      r=[zb.b, CS.b], w=[TT.b])
                if d["first"]:
                    em.do("pool", lambda e: e.tensor_tensor(out=TT.t[:, nco - 128:nco], in0=TT.t[:, nco - 128:nco], in1=mneg.t[:], op=ALU.add),
                          r=[TT.b, mneg.b], w=[TT.b])
                em.do("act", lambda e: e.activation(out=A_.t[:, :nco], in_=TT.t[:, :nco], func=AF.Exp, bias=nR.t[:, 0:1], scale=1.0), r=[TT.b, nR.b], w=[A_.b])

            def S2b(d):
                qb, kt, nco, nblk, u = d["qb"], d["kt"], d["nco"], d["nblk"], d["u"]
                A_, AT = A_t[u], AT_t[u]
                qs = slice(qb * 128, (qb + 1) * 128)
                po = u * 512
                for blk in range(nblk):
                    bs = slice(blk * 128, (blk + 1) * 128)
                    em.do("pe", lambda e: e.transpose(PT.t[:, po + blk * 128:po + (blk + 1) * 128], A_.t[:, bs], ident.t[:]), r=[A_.b, ident.b], w=[PTb[u]])
                em.do("dve", lambda e: e.tensor_copy(out=AT.t[:, :nco], in_=PT.t[:, po:po + nco]), r=[PTb[u]], w=[AT.b])
                for blk in range(nblk):
                    bs = slice(blk * 128, (blk + 1) * 128)
                    im = d["imm0"] + blk
                    em.do("pe", lambda e: e.matmul(PB[2].t[:64, qs], VR[:, kt * 4 + blk, :], AT.t[:, bs], start=(im == 0), stop=(im == d["nmm"] - 1)),
                          r=[VRb[kt], AT.b], w=[PB[2].b])

            N = len(its)
            for n in range(N + 2):
                if n < N:
                    S1(its[n])
                if 1 <= n <= N:
                    S2a(its[n - 1])
                if n >= 2:
                    S2b(its[n - 2])
            em.do("act", lambda e: e.activation(out=osb_t.t[:], in_=PB[2].t[:64, :], func=AF.Copy), r=[PB[2].b], w=[osb_t.b])
            em.dma("sp", st_s, osb_o[:, i * 512:(i + 1) * 512] if io is None else io.mix_out(i, 192, 64), osb_t.t[:], r=[osb_t.b])

    if io is not None:
        _end_phase(nc, em)
        return nc, em
    em.finish([st_y, st_r[0], st_r[1], st_s])
    em.close()
    return nc, em


def _rope_tables(S):
    half = 32
    inv = (np.float32(10000.0) ** (-np.arange(half, dtype=np.float32) / np.float32(half))).astype(np.float32)
    pos = np.arange(S, dtype=np.float32)
    ang = (pos[:, None] * inv[None, :]).astype(np.float32)
    c = np.cos(ang).astype(np.float32)
    s = np.sin(ang).astype(np.float32)
    cos64 = np.concatenate([c, c], axis=1)
    sin64 = np.concatenate([-s, s], axis=1)
    cosT = np.concatenate([cos64, cos64], axis=1)
    sinT = np.concatenate([sin64, sin64], axis=1)
    return np.ascontiguousarray(cosT.T), np.ascontiguousarray(sinT.T), np.ascontiguousarray(cosT), np.ascontiguousarray(sinT)


def _ret_consts(j):
    C = 128
    idx = np.arange(C, dtype=np.float32)
    maskT = np.zeros((128, 2, 512), np.float32)
    qdec = np.zeros((128, 512), np.float32)
    kdec = np.zeros((128, 2), np.float32)
    gC = np.zeros((128, 1), np.float32)
    for hl in range(2):
        h = 2 * j + hl
        lg = np.log(np.float32(1.0) - np.float32(2.0) ** np.float32(-5.0 - h)).astype(np.float32)
        rel = idx[:, None] - idx[None, :]
        dm = np.where(rel >= 0, np.exp(lg * np.maximum(rel, 0.0)), 0.0).astype(np.float32)
        mT = (dm.T * np.float32(0.125)).astype(np.float32)
        maskT[:, hl, :] = np.tile(mT, (1, 4))
        qd = np.exp(lg * (idx + 1.0)).astype(np.float32)
        qdec[hl * 64:(hl + 1) * 64, :] = np.tile(qd[None, :], (64, 4))
        kdec[:, hl] = np.exp(lg * (C - 1.0 - idx)).astype(np.float32) * np.float32(0.125)
        gC[hl * 64:(hl + 1) * 64, 0] = np.exp(lg * np.float32(C))
    return maskT, qdec, kdec, gC


def _swap(cols):
    cols = np.asarray(cols).reshape(-1, 64)
    return np.concatenate([cols[:, 32:], cols[:, :32]], axis=1).reshape(-1)


def prep_B_inputs(layer, j, w_in, lam_re, lam_im, log_dt, b_re, b_im, c_re, c_im, d_skip, S, consts):
    u_cols = np.arange(64 * j, 64 * j + 64)
    rq = 256 + np.arange(128 * j, 128 * j + 128)
    rk = 768 + np.arange(128 * j, 128 * j + 128)
    rv = 1280 + np.arange(128 * j, 128 * j + 128)
    sq = 2304 + np.arange(64 * j, 64 * j + 64)
    sk = 2560 + np.arange(64 * j, 64 * j + 64)
    sv = 2816 + np.arange(64 * j, 64 * j + 64)
    fm_cols = np.concatenate([rq, _swap(rq), rk, _swap(rk), sq, u_cols, sk])
    tm_cols = np.concatenate([rk, _swap(rk), rv, sv])
    W = w_in[layer]
    wfm = np.ascontiguousarray(W[:, fm_cols].reshape(8, 128, NFM).transpose(1, 0, 2))
    wtm = np.ascontiguousarray(W[:, tm_cols].reshape(8, 128, NTM).transpose(1, 0, 2))
    G0 = 4 * j
    s5p = np.zeros((128, 2, 3), np.float32)
    bt = np.zeros((128, 2, 2, 128), np.float32)
    ct = np.zeros((128, 2, 2, 64), np.float32)
    for rb in range(2):
        for gl in range(2):
            g = G0 + 2 * rb + gl
            ps = slice(gl * 64, (gl + 1) * 64)
            s5p[ps, rb, 0] = lam_re[layer, g]
            s5p[ps, rb, 1] = lam_im[layer, g]
            s5p[ps, rb, 2] = log_dt[layer, g]
            chl = (2 * rb + gl) * 16
            bt[64 + chl:64 + chl + 16, 0, rb, ps] = b_re[layer, g].T
            bt[64 + chl:64 + chl + 16, 1, rb, ps] = b_im[layer, g].T
            ct[ps, 0, rb, chl:chl + 16] = c_re[layer, g].T
            ct[ps, 1, rb, chl:chl + 16] = c_im[layer, g].T
    dmat = np.zeros((128, 64), np.float32)
    dmat[64 + np.arange(64), np.arange(64)] = d_skip[layer, 64 * j:64 * j + 64]
    maskT, qdec, kdec, gC = _ret_consts(j)
    d = dict(wfm=wfm, wtm=wtm, s5p=s5p, bt=bt, ct=ct, dmat=dmat, maskT=maskT, qdec=qdec, kdec=kdec, gC=gC)
    d.update(consts)
    return d


def B_consts(S):
    cosF, sinF, cosT, sinT = _rope_tables(S)
    iota = np.tile(np.arange(513, dtype=np.float32)[None, :], (128, 1))
    qi = np.arange(128)
    m01 = (qi[None, :] < qi[:, None]).astype(np.float32)
    mneg = np.where(m01 > 0, 0.0, -30000.0).astype(np.float32)
    ident = np.eye(128, dtype=np.float32)
    return dict(cosF=cosF, sinF=sinF, cosT=cosT, sinT=sinT, iota=iota, m01=m01, mneg=mneg, ident=ident)


def build_P(TOK, io=None):
    if io is not None:
        nc = io.nc
        x_d = io.get("P", "x"); id_d = io.get("P", "ident"); xT_o = io.get("P", "xT")
    else:
        nc = bass.Bass("TRN2", target_bir_lowering=False)
        x_d = nc.dram_tensor("x", [TOK, 1024], F32, kind="ExternalInput").ap()
        id_d = nc.dram_tensor("ident", [128, 128], F32, kind="ExternalInput").ap()
        xT_o = nc.dram_tensor("xT", [1024, TOK], BF16, kind="ExternalOutput").ap()
    em = Emitter(nc)
    sb = lambda shape, dt=F32: T(em.sbuf(shape, dt))
    ldh = em.new_sem("ldh")
    ident = sb([128, 128]); em.dma("sp", ldh, ident.t[:], id_d); em.group_mark(ldh, [ident])
    xin = [sb([128, 1024]) for _ in range(2)]
    xs = [em.new_sem("x") for _ in range(2)]
    PTt = [T(em.psum([128, 1024])) for _ in range(2)]
    xo = [sb([128, 8, 128], BF16) for _ in range(2)]
    so = [em.new_sem("o") for _ in range(2)]
    nb = TOK // 128
    em.dma("sp", xs[0], xin[0].t[:], x_d[0:128, :], w=[xin[0].b])
    for blk in range(nb):
        s = blk % 2
        if blk + 1 < nb:
            em.dma("sp", xs[1 - s], xin[1 - s].t[:], x_d[(blk + 1) * 128:(blk + 2) * 128, :], w=[xin[1 - s].b])
        for k in range(8):
            em.do("pe", lambda e: e.transpose(PTt[s].t[:, k * 128:(k + 1) * 128], xin[s].t[:, k * 128:(k + 1) * 128], ident.t[:]),
                  r=[xin[s].b, ident.b], w=[PTt[s].b])
        em.do("dve" if s == 0 else "act",
              (lambda e: e.tensor_copy(out=xo[s].t[:], in_=PTt[s].t[:].rearrange("p (k t) -> p k t", k=8))) if s == 0 else
              (lambda e: e.activation(out=xo[s].t[:], in_=PTt[s].t[:].rearrange("p (k t) -> p k t", k=8), func=AF.Copy)),
              r=[PTt[s].b], w=[xo[s].b])
        em.dma("sp", so[s], xT_o[:, blk * 128:(blk + 1) * 128].rearrange("(k p) t -> p k t", p=128), xo[s].t[:], r=[xo[s].b])
    if io is not None:
        _end_phase(nc, em)
        return nc, em
    em.finish(so)
    em.close()
    return nc, em


FFC_DENSE = [128] * 21 + [64]
FFC_EXP = [128] * 5 + [48]


def build_C(TOK, kind, io=None):
    NTL = TOK // 512
    nc = io.nc if io is not None else bass.Bass("TRN2", target_bir_lowering=False)

    def din(name, shape, dt=F32):
        if io is not None:
            return io.get("C", name)
        return nc.dram_tensor(name, list(shape), dt, kind="ExternalInput").ap()

    def dout(name, shape, dt=F32):
        if io is not None:
            return io.get("C", name)
        return nc.dram_tensor(name, list(shape), dt, kind="ExternalOutput").ap()

    x_d = din("x", [TOK, 1024])
    xT_d = din("xT", [1024, TOK], BF16)
    if io is None:
        yss_d = din("yssmT", [256, TOK]); oret_d = din("oretT", [512, TOK]); osb_d = din("osbT", [256, TOK])
    wrg_d = din("wrg", [128, 8, 512]); gluw_d = din("gluw", [128, 2, 256]); wout_d = din("wout", [128, 8, 1024])
    vec_d = din("vecs", [128, 12])
    sbw_d = din("sbw", [128, 2])
    lnr_d = din("lnrows", [4, 128, 1024])
    blk16_d = din("blk16", [128, 128]); blk64_d = din("blk64", [128, 128]); id_d = din("ident", [128, 128])
    if kind == "dense":
        NF = 22; FFC = FFC_DENSE
        wg_d = din("wg", [NF, 128, 8, 128]); wu_d = din("wu", [NF, 128, 8, 128]); wd_d = din("wd", [128, NF, 1024])
    else:
        NF = 6; FFC = FFC_EXP
        wg_d = din("wg", [8, NF, 128, 8, 128]); wu_d = din("wu", [8, NF, 128, 8, 128]); wd_d = din("wd", [8, 128, NF, 1024])
        wr_d = din("wr", [128, 8, 8])
    x_o = dout("xo", [TOK, 1024])
    xT_o = dout("xTo", [1024, TOK], BF16)

    em = Emitter(nc)
    sb = lambda shape, dt=F32: T(em.sbuf(shape, dt))
    ldw = em.new_sem("ldw"); ldh = em.new_sem("ldh")
    wrg = sb([128, 8, 512], BF16); gluw = sb([128, 2, 256], BF16); wout = sb([128, 8, 1024], BF16)
    for k in range(8):
        em.dma("pool", ldw, wrg.t[:, k, :], wrg_d[:, k, :])
        em.dma("pool", ldw, wout.t[:, k, :], wout_d[:, k, :])
    em.dma("pool", ldw, gluw.t[:], gluw_d)
    blk16 = sb([128, 128], BF16); em.dma("pool", ldw, blk16.t[:], blk16_d)
    blk64 = sb([128, 128], BF16); em.dma("pool", ldw, blk64.t[:], blk64_d)
    gl = [wrg, gluw, wout, blk16, blk64]
    if kind == "dense":
        wd = sb([128, NF, 1024], BF16)
        for f in range(NF):
            em.dma("pool", ldw, wd.t[:, f, :], wd_d[:, f, :])
        gl.append(wd)
    em.group_mark(ldw, gl)
    vecs = sb([128, 12]); em.dma("sp", ldh, vecs.t[:], vec_d)
    sbw = sb([128, 2]); em.dma("sp", ldh, sbw.t[:], sbw_d)
    lnr = [sb([128, 1024]) for _ in range(4)]
    for q in range(4):
        em.dma("sp", ldh, lnr[q].t[:], lnr_d[q])
    ident = sb([128, 128]); em.dma("sp", ldh, ident.t[:], id_d)
    gh = [vecs, sbw, ident] + lnr
    if kind == "moe":
        wr = sb([128, 8, 8]); em.dma("sp", ldh, wr.t[:], wr_d); gh.append(wr)
    em.group_mark(ldh, gh)

    PF = [T(em.psum([128, 512])) for _ in range(6)]
    PTt = T(em.psum([128, 1024]))

    xTt = sb([128, 8, 512], BF16); s_xT = em.new_sem("xT")
    cin = [sb([128, 512]) for _ in range(3)]; s_cin = [em.new_sem("ci") for _ in range(3)]
    xtm = [sb([128, 1024]) for _ in range(2)]; s_xtm = [em.new_sem("xm") for _ in range(2)]
    NWB = 2
    wgb = [sb([128, 8, 128], BF16) for _ in range(NWB)]; wub = [sb([128, 8, 128], BF16) for _ in range(NWB)]
    s_wg = [em.new_sem("wg") for _ in range(NWB)]; s_wu = [em.new_sem("wu") for _ in range(NWB)]
    mixT = sb([128, 8, 512], BF16)
    X1 = [sb([128, 1024]) for _ in range(4)]
    x1T = sb([128, 8, 512], BF16)
    hT = sb([128, NF, 512], BF16)
    g32 = [sb([128, 512]) for _ in range(2)]; gb = [sb([128, 512], BF16) for _ in range(2)]
    tmpA = sb([128, 512]); tmpB = sb([128, 512]); tbf = sb([128, 512], BF16)
    rs = sb([128, 512])
    hbuf = sb([128, 1024])
    stats = sb([128, 2, 6]); mv = sb([128, 2]); sd1 = sb([128, 1]); rs1 = sb([128, 1])
    xo_t = [sb([128, 1024]) for _ in range(2)]; s_xo = [em.new_sem("xo") for _ in range(2)]
    xTo_t = [sb([128, 8, 128], BF16) for _ in range(2)]; s_xTo = [em.new_sem("xTo") for _ in range(2)]
    sg = [sb([128, 512]) for _ in range(2)]
    cin_i = [0]
    if kind == "moe":
        wdb = [sb([128, NF, 1024], BF16) for _ in range(2)]; s_wd = [em.new_sem("wd") for _ in range(2)]
        acc = [sb([128, 1024]) for _ in range(4)]
        x1T32 = sb([128, 8, 128])
        lg = sb([128, 8]); m8 = sb([128, 8]); nm1 = sb([128, 1]); sel = sb([128, 8]); ex = sb([128, 8]); den = sb([128, 1]); rden = sb([128, 1])
        G = [sb([128, 8]) for _ in range(4)]

    def load_chunk(which, idx, ts):
        u = cin_i[0] % 3
        cin_i[0] += 1
        if io is None:
            src = {"yss": yss_d, "oret": oret_d, "osb": osb_d}[which]
            em.dma("sp", s_cin[u], cin[u].t[:], src[idx * 128:(idx + 1) * 128, ts], w=[cin[u].b])
        else:
            em.dma_multi("sp", s_cin[u], [(cin[u].t[p0:p0 + np_, :], ap) for (p0, np_, ap) in io.mix_in(which, idx, ts)], w=[cin[u].b])
        return cin[u]

    def rstd_from_psum(ps, dst_rs):
        em.do("act", lambda e: e.activation(out=dst_rs.t[:], in_=ps.t[:], func=AF.Sqrt, bias=LN_EPS, scale=1.0), r=[ps.b], w=[dst_rs.b])
        em.do("dve", lambda e: e.reciprocal(out=dst_rs.t[:], in_=dst_rs.t[:]), r=[dst_rs.b], w=[dst_rs.b])

    def layer_norm_rows(h, wrow, brow, out):
        for c in range(2):
            em.do("dve", lambda e: e.bn_stats(out=stats.t[:, c, :], in_=h.t[:, c * 512:(c + 1) * 512]), r=[h.b], w=[stats.b])
        em.do("dve", lambda e: e.bn_aggr(out=mv.t[:], in_=stats.t[:]), r=[stats.b], w=[mv.b])
        em.do("act", lambda e: e.activation(out=sd1.t[:], in_=mv.t[:, 1:2], func=AF.Sqrt, bias=LN_EPS, scale=1.0), r=[mv.b], w=[sd1.b])
        em.do("dve", lambda e: e.reciprocal(out=rs1.t[:], in_=sd1.t[:]), r=[sd1.b], w=[rs1.b])
        em.do("dve", lambda e: e.tensor_scalar(out=h.t[:], in0=h.t[:], scalar1=mv.t[:, 0:1], scalar2=rs1.t[:, 0:1], op0=ALU.subtract, op1=ALU.mult),
              r=[h.b, mv.b, rs1.b], w=[h.b])
        em.do("dve", lambda e: e.tensor_tensor(out=h.t[:], in0=h.t[:], in1=wrow.t[:], op=ALU.mult), r=[h.b, wrow.b], w=[h.b])
        em.do("dve", lambda e: e.tensor_tensor(out=out.t[:], in0=h.t[:], in1=brow.t[:], op=ALU.add), r=[h.b, brow.b], w=[out.b])

    nwl = [0]

    def load_w(src_g, src_u):
        u = nwl[0] % NWB
        nwl[0] += 1
        em.dma("pool", s_wg[u], wgb[u].t[:], src_g, w=[wgb[u].b])
        em.dma("pool", s_wu[u], wub[u].t[:], src_u, w=[wub[u].b])
        return wgb[u], wub[u]

    gu_i = [0]

    def gate_up(wgt, wut, cw, dst_ap, dst_buf):
        p = (gu_i[0] % 2) * 2
        gu_i[0] += 1
        pg, pu = PF[p], PF[p + 1]
        sgt = sg[(gu_i[0]) % 2]
        for k in range(8):
            em.do("pe", lambda e: e.matmul(pg.t[:cw, :], wgt.t[:, k, :cw], x1T.t[:, k, :], start=(k == 0), stop=(k == 7)), r=[wgt.b, x1T.b], w=[pg.b])
        for k in range(8):
            em.do("pe", lambda e: e.matmul(pu.t[:cw, :], wut.t[:, k, :cw], x1T.t[:, k, :], start=(k == 0), stop=(k == 7)), r=[wut.b, x1T.b], w=[pu.b])
        em.do("act", lambda e: e.activation(out=sgt.t[:cw, :], in_=pg.t[:cw, :], func=AF.Silu), r=[pg.b], w=[sgt.b])
        em.do("dve", lambda e: e.tensor_tensor(out=dst_ap, in0=sgt.t[:cw, :], in1=pu.t[:cw, :], op=ALU.mult), r=[sgt.b, pu.b], w=[dst_buf])

    def transposes_to_bf16(src, dst_ap, dst_buf, also32=None):
        for k in range(8):
            em.do("pe", lambda e: e.transpose(PTt.t[:, k * 128:(k + 1) * 128], src.t[:, k * 128:(k + 1) * 128], ident.t[:]), r=[src.b, ident.b], w=[PTt.b])
        em.do("act", lambda e: e.activation(out=dst_ap, in_=PTt.t[:].rearrange("p (k t) -> p k t", k=8), func=AF.Copy), r=[PTt.b], w=[dst_buf])
        if also32 is not None:
            em.do("act", lambda e: e.activation(out=also32.t[:], in_=PTt.t[:].rearrange("p (k t) -> p k t", k=8), func=AF.Copy), r=[PTt.b], w=[also32.b])

    def finish_block(it, blk):
        u = (it * 4 + blk) % 2
        tok0 = it * 512 + blk * 128
        layer_norm_rows(hbuf, lnr[2], lnr[3], xo_t[u])
        em.dma("sp", s_xo[u], x_o[tok0:tok0 + 128, :], xo_t[u].t[:], r=[xo_t[u].b])
        transposes_to_bf16(xo_t[u], xTo_t[u].t[:], xTo_t[u].b)
        em.dma("sp", s_xTo[u], xT_o[:, tok0:tok0 + 128].rearrange("(k p) t -> p k t", p=128), xTo_t[u].t[:], r=[xTo_t[u].b])

    for it in range(NTL):
        t0 = it * 512
        ts = slice(t0, t0 + 512)
        em.dma("sp", s_xT, xTt.t[:], xT_d[:, ts].rearrange("(k p) t -> p k t", p=128), w=[xTt.b])
        for k in range(2):
            ci = load_chunk("yss", k, ts)
            em.do("act", lambda e: e.activation(out=g32[k].t[:], in_=ci.t[:], func=AF.Gelu), r=[ci.b], w=[g32[k].b])
            em.do("dve", lambda e: e.tensor_copy(out=gb[k].t[:], in_=g32[k].t[:]), r=[g32[k].b], w=[gb[k].b])
        for m in range(2):
            ps = PF[4 + m % 2]
            for k in range(2):
                em.do("pe", lambda e: e.matmul(ps.t[:], gluw.t[:, k, m * 128:(m + 1) * 128], gb[k].t[:], start=(k == 0), stop=(k == 1)), r=[gluw.b, gb[k].b], w=[ps.b])
            em.do("act", lambda e: e.activation(out=tmpA.t[:], in_=ps.t[:], func=AF.Sigmoid, bias=vecs.t[:, m:m + 1], scale=1.0), r=[ps.b, vecs.b], w=[tmpA.b])
            em.do("dve", lambda e: e.tensor_tensor(out=tmpB.t[:], in0=g32[m].t[:], in1=tmpA.t[:], op=ALU.mult), r=[g32[m].b, tmpA.b], w=[tmpB.b])
            em.do("dve", lambda e: e.tensor_tensor(out=tbf.t[:], in0=tmpB.t[:], in1=tmpB.t[:], op=ALU.mult), r=[tmpB.b], w=[tbf.b])
            em.do("pe", lambda e: e.matmul(ps.t[:], blk16.t[:], tbf.t[:], start=True, stop=True), r=[blk16.b, tbf.b], w=[ps.b])
            rstd_from_psum(ps, rs)
            em.do("dve", lambda e: e.scalar_tensor_tensor(out=mixT.t[:, m, :], in0=tmpB.t[:], scalar=vecs.t[:, 2 + m:3 + m], in1=rs.t[:], op0=ALU.mult, op1=ALU.mult),
                  r=[tmpB.b, vecs.b, rs.b], w=[mixT.b])
        for m in range(4):
            ci = load_chunk("oret", m, ts)
            ps = PF[4 + m % 2]
            em.do("dve", lambda e: e.tensor_copy(out=tbf.t[:], in_=ci.t[:]), r=[ci.b], w=[tbf.b])
            em.do("pe", lambda e: e.matmul(ps.t[:], blk64.t[:], tbf.t[:], start=True, stop=True), r=[blk64.b, tbf.b], w=[ps.b])
            em.do("dve", lambda e: e.tensor_tensor(out=tmpA.t[:], in0=ci.t[:], in1=ps.t[:], op=ALU.subtract), r=[ci.b, ps.b], w=[tmpA.b])
            em.do("dve", lambda e: e.tensor_tensor(out=tbf.t[:], in0=tmpA.t[:], in1=tmpA.t[:], op=ALU.mult), r=[tmpA.b], w=[tbf.b])
            em.do("pe", lambda e: e.matmul(ps.t[:], blk64.t[:], tbf.t[:], start=True, stop=True), r=[blk64.b, tbf.b], w=[ps.b])
            rstd_from_psum(ps, rs)
            em.do("dve", lambda e: e.scalar_tensor_tensor(out=tmpB.t[:], in0=tmpA.t[:], scalar=vecs.t[:, 4 + m:5 + m], in1=rs.t[:], op0=ALU.mult, op1=ALU.mult),
                  r=[tmpA.b, vecs.b, rs.b], w=[tmpB.b])
            for k in range(8):
                em.do("pe", lambda e: e.matmul(ps.t[:], wrg.t[:, k, m * 128:(m + 1) * 128], xTt.t[:, k, :], start=(k == 0), stop=(k == 7)), r=[wrg.b, xTt.b], w=[ps.b])
            em.do("act", lambda e: e.activation(out=sg[0].t[:], in_=ps.t[:], func=AF.Silu), r=[ps.b], w=[sg[0].b])
            em.do("dve", lambda e: e.scalar_tensor_tensor(out=mixT.t[:, 2 + m, :], in0=tmpB.t[:], scalar=vecs.t[:, 8 + m:9 + m], in1=sg[0].t[:], op0=ALU.add, op1=ALU.mult),
                  r=[tmpB.b, vecs.b, sg[0].b], w=[mixT.b])
        for m in range(2):
            ci = load_chunk("osb", m, ts)
            ps = PF[4 + m % 2]
            em.do("dve", lambda e: e.tensor_tensor(out=tbf.t[:], in0=ci.t[:], in1=ci.t[:], op=ALU.mult), r=[ci.b], w=[tbf.b])
            em.do("pe", lambda e: e.matmul(ps.t[:], blk64.t[:], tbf.t[:], start=True, stop=True), r=[blk64.b, tbf.b], w=[ps.b])
            rstd_from_psum(ps, rs)
            em.do("dve", lambda e: e.scalar_tensor_tensor(out=mixT.t[:, 6 + m, :], in0=ci.t[:], scalar=sbw.t[:, m:m + 1], in1=rs.t[:], op0=ALU.mult, op1=ALU.mult),
                  r=[ci.b, sbw.b, rs.b], w=[mixT.b])
        for blk in range(4):
            u = (it * 4 + blk) % 2
            em.dma("sp", s_xtm[u], xtm[u].t[:], x_d[t0 + blk * 128:t0 + (blk + 1) * 128, :], w=[xtm[u].b])
            for half in range(2):
                ps = PF[4 + half]
                hs = slice(half * 512, (half + 1) * 512)
                for k in range(8):
                    em.do("pe", lambda e: e.matmul(ps.t[:], mixT.t[:, k, blk * 128:(blk + 1) * 128], wout.t[:, k, hs], start=(k == 0), stop=(k == 7)),
                          r=[mixT.b, wout.b], w=[ps.b])
                em.do("dve", lambda e: e.scalar_tensor_tensor(out=hbuf.t[:, hs], in0=xtm[u].t[:, hs], scalar=ALPHA, in1=ps.t[:], op0=ALU.mult, op1=ALU.add),
                      r=[xtm[u].b, ps.b], w=[hbuf.b])
            layer_norm_rows(hbuf, lnr[0], lnr[1], X1[blk])
            if kind == "moe":
                transposes_to_bf16(X1[blk], x1T.t[:, :, blk * 128:(blk + 1) * 128], x1T.b, also32=x1T32)
                for k in range(8):
                    em.do("pe", lambda e: e.matmul(PF[4].t[:, 0:8], x1T32.t[:, k, :], wr.t[:, k, :], start=(k == 0), stop=(k == 7)), r=[x1T32.b, wr.b], w=[PF[4].b])
                em.do("dve", lambda e: e.tensor_copy(out=lg.t[:], in_=PF[4].t[:, 0:8]), r=[PF[4].b], w=[lg.b])
                em.do("dve", lambda e: e.max(out=m8.t[:], in_=lg.t[:]), r=[lg.b], w=[m8.b])
                em.do("dve", lambda e: e.tensor_scalar(out=nm1.t[:], in0=m8.t[:, 0:1], scalar1=-1.0, scalar2=None, op0=ALU.mult), r=[m8.b], w=[nm1.b])
                em.do("dve", lambda e: e.tensor_scalar(out=sel.t[:], in0=lg.t[:], scalar1=m8.t[:, 1:2], scalar2=None, op0=ALU.is_ge), r=[lg.b, m8.b], w=[sel.b])
                em.do("act", lambda e: e.activation(out=ex.t[:], in_=lg.t[:], func=AF.Exp, bias=nm1.t[:, 0:1], scale=1.0), r=[lg.b, nm1.b], w=[ex.b])
                em.do("dve", lambda e: e.tensor_tensor(out=ex.t[:], in0=ex.t[:], in1=sel.t[:], op=ALU.mult), r=[ex.b, sel.b], w=[ex.b])
                em.do("dve", lambda e: e.reduce_sum(out=den.t[:], in_=ex.t[:], axis=AX.X), r=[ex.b], w=[den.b])
                em.do("dve", lambda e: e.reciprocal(out=rden.t[:], in_=den.t[:]), r=[den.b], w=[rden.b])
                em.do("dve", lambda e: e.tensor_scalar(out=G[blk].t[:], in0=ex.t[:], scalar1=rden.t[:, 0:1], scalar2=None, op0=ALU.mult), r=[ex.b, rden.b], w=[G[blk].b])
            else:
                transposes_to_bf16(X1[blk], x1T.t[:, :, blk * 128:(blk + 1) * 128], x1T.b)
        if kind == "dense":
            nxt = load_w(wg_d[0], wu_d[0])
            for f in range(NF):
                cur = nxt
                if f + 1 < NF:
                    nxt = load_w(wg_d[f + 1], wu_d[f + 1])
                gate_up(cur[0], cur[1], FFC[f], hT.t[:FFC[f], f, :], hT.b)
            for blk in range(4):
                for half in range(2):
                    ps = PF[4 + half]
                    hs = slice(half * 512, (half + 1) * 512)
                    for f in range(NF):
                        cw = FFC[f]
                        em.do("pe", lambda e: e.matmul(ps.t[:], hT.t[:cw, f, blk * 128:(blk + 1) * 128], wd.t[:cw, f, hs], start=(f == 0), stop=(f == NF - 1)),
                              r=[hT.b, wd.b], w=[ps.b])
                    em.do("dve", lambda e: e.scalar_tensor_tensor(out=hbuf.t[:, hs], in0=X1[blk].t[:, hs], scalar=ALPHA, in1=ps.t[:], op0=ALU.mult, op1=ALU.add),
                          r=[X1[blk].b, ps.b], w=[hbuf.b])
                finish_block(it, blk)
        else:
            for ex_i in range(8):
                wdt = wdb[ex_i % 2]
                em.dma("pool", s_wd[ex_i % 2], wdt.t[:], wd_d[ex_i], w=[wdt.b])
                nxt = load_w(wg_d[ex_i, 0], wu_d[ex_i, 0])
                for f in range(NF):
                    cur = nxt
                    if f + 1 < NF:
                        nxt = load_w(wg_d[ex_i, f + 1], wu_d[ex_i, f + 1])
                    gate_up(cur[0], cur[1], FFC[f], hT.t[:FFC[f], f, :], hT.b)
                for blk in range(4):
                    for half in range(2):
                        ps = PF[4 + half]
                        hs = slice(half * 512, (half + 1) * 512)
                        for f in range(NF):
                            cw = FFC[f]
                            em.do("pe", lambda e: e.matmul(ps.t[:], hT.t[:cw, f, blk * 128:(blk + 1) * 128], wdt.t[:cw, f, hs], start=(f == 0), stop=(f == NF - 1)),
                                  r=[hT.b, wdt.b], w=[ps.b])
                        if ex_i == 0:
                            em.do("dve", lambda e: e.tensor_scalar(out=acc[blk].t[:, hs], in0=ps.t[:], scalar1=G[blk].t[:, 0:1], scalar2=None, op0=ALU.mult),
                                  r=[ps.b, G[blk].b], w=[acc[blk].b])
                        else:
                            em.do("dve", lambda e: e.scalar_tensor_tensor(out=acc[blk].t[:, hs], in0=ps.t[:], scalar=G[blk].t[:, ex_i:ex_i + 1], in1=acc[blk].t[:, hs],
                                                                          op0=ALU.mult, op1=ALU.add), r=[ps.b, G[blk].b, acc[blk].b], w=[acc[blk].b])
            for blk in range(4):
                em.do("dve", lambda e: e.scalar_tensor_tensor(out=hbuf.t[:], in0=X1[blk].t[:], scalar=ALPHA, in1=acc[blk].t[:], op0=ALU.mult, op1=ALU.add),
                      r=[X1[blk].b, acc[blk].b], w=[hbuf.b])
                finish_block(it, blk)

    if io is not None:
        _end_phase(nc, em)
        return nc, em
    em.finish(s_xo + s_xTo)
    em.close()
    return nc, em


def C_consts():
    p = np.arange(128)
    blk16 = ((p[:, None] // 16) == (p[None, :] // 16)).astype(np.float32) / np.float32(16.0)
    blk64 = ((p[:, None] // 64) == (p[None, :] // 64)).astype(np.float32) / np.float32(64.0)
    return dict(blk16=blk16, blk64=blk64, ident=np.eye(128, dtype=np.float32))


def _kchunk(w, ncols):
    K = w.shape[0] // 128
    return np.ascontiguousarray(w.reshape(K, 128, ncols).transpose(1, 0, 2))


def prep_C_weights(layer, inp, consts):
    d = dict(consts)
    d["wrg"] = _kchunk(inp["w_in"][layer][:, 1792:2304], 512)
    d["gluw"] = _kchunk(inp["ssm_glu_w"][layer], 256)
    d["wout"] = _kchunk(inp["w_out"][layer], 1024)
    vecs = np.zeros((128, 12), np.float32)
    vecs[:, 0:2] = inp["ssm_glu_b"][layer].reshape(2, 128).T
    vecs[:, 2:4] = inp["ssm_norm_w"][layer].reshape(2, 128).T
    vecs[:, 4:8] = inp["ret_gn_w"][layer].reshape(4, 128).T
    vecs[:, 8:12] = inp["ret_gn_b"][layer].reshape(4, 128).T
    d["vecs"] = vecs
    d["sbw"] = np.ascontiguousarray(inp["sb_norm_w"][layer].reshape(2, 128).T)
    rows = [inp["ln_mix_w"][layer], inp["ln_mix_b"][layer], inp["ln_ffn_w"][layer], inp["ln_ffn_b"][layer]]
    d["lnrows"] = np.ascontiguousarray(np.stack([np.broadcast_to(r[None, :], (128, 1024)) for r in rows]).astype(np.float32))
    li = layer // 2
    if layer % 2 == 0:
        def padc(w):
            o = np.zeros((1024, 2816), np.float32); o[:, :D_FF] = w; return o
        wgp = padc(inp["ffn_w_gate"][li]); wup = padc(inp["ffn_w_up"][li])
        d["wg"] = np.ascontiguousarray(wgp.reshape(8, 128, 22, 128).transpose(2, 1, 0, 3))
        d["wu"] = np.ascontiguousarray(wup.reshape(8, 128, 22, 128).transpose(2, 1, 0, 3))
        wdp = np.zeros((2816, 1024), np.float32); wdp[:D_FF] = inp["ffn_w_down"][li]
        d["wd"] = np.ascontiguousarray(wdp.reshape(22, 128, 1024).transpose(1, 0, 2))
    else:
        wg = np.zeros((8, 1024, 768), np.float32); wg[:, :, :D_FFE] = inp["moe_w_gate"][li]
        wu = np.zeros((8, 1024, 768), np.float32); wu[:, :, :D_FFE] = inp["moe_w_up"][li]
        d["wg"] = np.ascontiguousarray(wg.reshape(8, 8, 128, 6, 128).transpose(0, 3, 2, 1, 4))
        d["wu"] = np.ascontiguousarray(wu.reshape(8, 8, 128, 6, 128).transpose(0, 3, 2, 1, 4))
        wd = np.zeros((8, 768, 1024), np.float32); wd[:, :D_FFE] = inp["moe_w_down"][li]
        d["wd"] = np.ascontiguousarray(wd.reshape(8, 6, 128, 1024).transpose(0, 2, 1, 3))
        d["wr"] = _kchunk(inp["moe_router"][li], 8)
    return d


class _IO:
    def __init__(self, nc, S):
        self.nc = nc
        self.S = S
        self.TOK = S // 4
        self.layer = 0
        self.t = {}
        self.cur = {}

    def get(self, phase, name):
        return self.cur[(phase, name)]

    def xT_tile(self, i, k):
        r = (i * 512) // self.TOK
        off = i * 512 - r * self.TOK
        return self.t["xT_g"][k * 512 + r * 128:k * 512 + (r + 1) * 128, off:off + 512]

    def mix_out(self, i, row0, nrows):
        q = (i * 512) // self.TOK
        off = i * 512 - q * self.TOK
        return self.t["mix_loc"][q * 256 + row0:q * 256 + row0 + nrows, off:off + 512]

    def mix_in(self, which, idx, ts):
        gq = self.gq
        if which == "oret":
            return [(0, 64, gq[1 * 256 + idx * 64:1 * 256 + idx * 64 + 64, ts]),
                    (64, 64, gq[2 * 256 + idx * 64:2 * 256 + idx * 64 + 64, ts])]
        cb = 0 if which == "yss" else 3
        return [(0, 64, gq[cb * 256 + (2 * idx) * 64:cb * 256 + (2 * idx) * 64 + 64, ts]),
                (64, 64, gq[cb * 256 + (2 * idx + 1) * 64:cb * 256 + (2 * idx + 1) * 64 + 64, ts])]


_B_LAYER = ["wfm", "wtm", "s5p", "bt", "ct", "dmat"]
_B_CONST = ["cosF", "sinF", "cosT", "sinT", "maskT", "qdec", "kdec", "gC", "iota", "m01", "mneg", "ident"]
_C_LAYER = ["wrg", "gluw", "wout", "vecs", "sbw", "lnrows"]
_C_CONST = ["blk16", "blk64", "ident"]


def _collective(nc, kind, src, dst, nchunks):
    R = src.shape[0] // nchunks
    hs = []
    for c in range(nchunks):
        cs = nc.alloc_semaphore(name="cc_%d" % _next_uid())
        hs.append(cs)
        nc.gpsimd.collective_compute(kind, ALU.bypass, replica_groups=[[0, 1, 2, 3], [4, 5, 6, 7]],
                                     ins=[src[c * R:(c + 1) * R, :].opt()], outs=[dst[c * 4 * R:(c + 1) * 4 * R, :].opt()]).then_inc(cs, 1)
    for cs in hs:
        nc.gpsimd.wait_ge(cs, 1)
    nc.all_engine_barrier()
    nc.clear_and_free_semaphores(hs)
    nc.all_engine_barrier()


def _next_uid():
    _UID[0] += 1
    return _UID[0]


def build_fused(S, depth=DEPTH, upto=None):
    TOK = S // 4
    nc = bass.Bass("TRN2", target_bir_lowering=False)
    io = _IO(nc, S)
    ein = lambda name, shape, dt=F32: nc.dram_tensor(name, list(shape), dt, kind="ExternalInput").ap()
    t = io.t
    t["x"] = ein("x", [TOK, 1024])
    shp = dict(wfm=[128, 8, NFM], wtm=[128, 8, NTM], s5p=[128, 2, 3], bt=[128, 2, 2, 128], ct=[128, 2, 2, 64], dmat=[128, 64])
    for n in _B_LAYER:
        t["B_" + n] = ein("B_" + n, [depth] + shp[n])
    cshp = dict(cosF=[128, S], sinF=[128, S], cosT=[S, 128], sinT=[S, 128], maskT=[128, 2, 512], qdec=[128, 512], kdec=[128, 2], gC=[128, 1],
                iota=[128, 513], m01=[128, 128], mneg=[128, 128], ident=[128, 128])
    for n in _B_CONST:
        t["K_" + n] = ein("K_" + n, cshp[n])
    shp = dict(wrg=[128, 8, 512], gluw=[128, 2, 256], wout=[128, 8, 1024], vecs=[128, 12], sbw=[128, 2], lnrows=[4, 128, 1024])
    for n in _C_LAYER:
        t["C_" + n] = ein("C_" + n, [depth] + shp[n])
    t["K_blk16"] = ein("K_blk16", [128, 128]); t["K_blk64"] = ein("K_blk64", [128, 128])
    nd = (depth + 1) // 2
    nm = depth // 2
    t["D_wg"] = ein("D_wg", [nd, 22, 128, 8, 128]); t["D_wu"] = ein("D_wu", [nd, 22, 128, 8, 128]); t["D_wd"] = ein("D_wd", [nd, 128, 22, 1024])
    if nm:
        t["M_wg"] = ein("M_wg", [nm, 8, 6, 128, 8, 128]); t["M_wu"] = ein("M_wu", [nm, 8, 6, 128, 8, 128]); t["M_wd"] = ein("M_wd", [nm, 8, 128, 6, 1024])
        t["M_wr"] = ein("M_wr", [nm, 128, 8, 8])
    t["xo"] = nc.dram_tensor("xo", [TOK, 1024], F32, kind="ExternalOutput").ap()
    t["xT_loc"] = [nc.dram_tensor("xT_loc%d" % i, [1024, TOK], BF16).ap() for i in range(2)]
    t["xT_g"] = nc.dram_tensor("xT_g", [4 * 1024, TOK], BF16).ap()
    t["mix_loc"] = nc.dram_tensor("mix_loc", [1024, TOK], F32).ap()
    t["mix_g"] = nc.dram_tensor("mix_g", [4 * 1024 + 128, TOK], F32).ap()
    t["mix_q"] = nc.dram_tensor("mix_q", [1024, TOK], F32).ap()
    t["xbuf"] = [nc.dram_tensor("xbuf%d" % i, [TOK, 1024], F32).ap() for i in range(2)]

    io.cur = {("P", "x"): t["x"], ("P", "ident"): t["K_ident"], ("P", "xT"): t["xT_loc"][0]}
    build_P(TOK, io=io)
    if upto == "P":
        return nc, []
    _collective(nc, "AllGather", t["xT_loc"][0], t["xT_g"], 8)
    if upto == "AG0":
        return nc, []
    stats = []
    for layer in range(depth):
        io.layer = layer
        cur = {}
        for n in _B_LAYER:
            cur[("B", n)] = t["B_" + n][layer]
        for n in _B_CONST:
            cur[("B", n)] = t["K_" + n]
        io.cur = cur
        _, emB = build_B(S, io=io)
        if upto == "B%d" % layer:
            return nc, []
        _collective(nc, "AllGather", t["mix_loc"], t["mix_g"], 16)
        if upto == "AGm%d" % layer:
            return nc, []
        gs = nc.alloc_semaphore(name="gq_%d" % _next_uid())
        nc.sync.dma_start(out=t["mix_q"], in_=t["mix_g"][bass.DynSlice((nc.partition_id() % 4) * 1024, 1024), :]).then_inc(gs, 16)
        nc.sync.wait_ge(gs, 16)
        nc.all_engine_barrier()
        nc.clear_and_free_semaphores([gs])
        nc.all_engine_barrier()
        io.gq = t["mix_q"]
        if upto == "Q%d" % layer:
            return nc, []
        cur = {}
        for n in _C_LAYER:
            cur[("C", n)] = t["C_" + n][layer]
        for n in _C_CONST:
            cur[("C", n)] = t["K_" + n]
        li = layer // 2
        kind = "dense" if layer % 2 == 0 else "moe"
        pre = "D_" if kind == "dense" else "M_"
        cur[("C", "wg")] = t[pre + "wg"][li]; cur[("C", "wu")] = t[pre + "wu"][li]; cur[("C", "wd")] = t[pre + "wd"][li]
        if kind == "moe":
            cur[("C", "wr")] = t["M_wr"][li]
        cur[("C", "x")] = t["x"] if layer == 0 else t["xbuf"][(layer - 1) % 2]
        cur[("C", "xT")] = t["xT_loc"][layer % 2]
        cur[("C", "xo")] = t["xo"] if layer == depth - 1 else t["xbuf"][layer % 2]
        cur[("C", "xTo")] = t["xT_loc"][(layer + 1) % 2]
        io.cur = cur
        _, emC = build_C(TOK, kind, io=io)
        stats.append((emB.n_inst, emB.n_wait, emC.n_inst, emC.n_wait))
        if upto == "C%d" % layer:
            return nc, []
        if layer + 1 < depth:
            _collective(nc, "AllGather", t["xT_loc"][(layer + 1) % 2], t["xT_g"], 8)
            if upto == "AGx%d" % layer:
                return nc, []
    return nc, stats


_CACHE = {}


def _get(name, fn):
    if name not in _CACHE:
        _CACHE[name] = fn()
    return _CACHE[name]


def fused_inputs(inp, S, depth=DEPTH):
    TOK = S // 4
    bcon = B_consts(S)
    ccon = C_consts()
    x = np.ascontiguousarray(inp["x"], dtype=np.float32)
    shared = {}
    for n in _B_CONST:
        if n not in ("maskT", "qdec", "kdec", "gC"):
            shared["K_" + n] = bcon[n]
    shared["K_blk16"] = ccon["blk16"]; shared["K_blk64"] = ccon["blk64"]
    cw = [prep_C_weights(l, inp, {}) for l in range(depth)]
    for n in _C_LAYER:
        shared["C_" + n] = np.ascontiguousarray(np.stack([cw[l][n] for l in range(depth)]))
    dl = [l for l in range(depth) if l % 2 == 0]
    ml = [l for l in range(depth) if l % 2 == 1]
    for n in ("wg", "wu", "wd"):
        shared["D_" + n] = np.ascontiguousarray(np.stack([cw[l][n] for l in dl]))
        if ml:
            shared["M_" + n] = np.ascontiguousarray(np.stack([cw[l][n] for l in ml]))
    if ml:
        shared["M_wr"] = np.ascontiguousarray(np.stack([cw[l]["wr"] for l in ml]))
    del cw
    perj = []
    for j in range(4):
        bl = [prep_B_inputs(l, j, inp["w_in"], inp["ssm_lam_re"], inp["ssm_lam_im"], inp["ssm_log_dt"], inp["ssm_b_re"], inp["ssm_b_im"],
                            inp["ssm_c_re"], inp["ssm_c_im"], inp["ssm_d"], S, {}) for l in range(depth)]
        d = {}
        for n in _B_LAYER:
            d["B_" + n] = np.ascontiguousarray(np.stack([bl[l][n] for l in range(depth)]))
        for n in ("maskT", "qdec", "kdec", "gC"):
            d["K_" + n] = bl[0][n]
        perj.append(d)
    maps = []
    for core in range(8):
        b, j = core // 4, core % 4
        d = dict(shared)
        d.update(perj[j])
        d["x"] = np.ascontiguousarray(x[b, j * TOK:(j + 1) * TOK])
        maps.append(d)
    return maps


def kernel(**inputs):
    inp = {k: np.asarray(v) for k, v in inputs.items()}
    S = inp["x"].shape[1]
    TOK = S // 4
    nc = _get(("F", S), lambda: build_fused(S)[0])
    maps = fused_inputs(inp, S)
    res = run_bass_kernel_spmd(nc, maps, core_ids=list(range(8))).results
    out = np.stack([np.concatenate([res[b * 4 + c]["xo"] for c in range(4)], axis=0) for b in range(2)])
    return np.ascontiguousarray(out, dtype=np.float32)
```
